# Optimizing a Trainium2 kernel written in Bass

```python
import math
import jax, jax.numpy as jnp
from jax import lax
import numpy as np

D_MODEL = 1024
BATCH = 4
SEQ = 4096
DEPTH = 1

GM_WIDTH = D_MODEL // 2
GM_GROUPS = 8
GM_CHUNK = 128
DIL_PAIRS = ((128, 1), (512, 4), (2048, 16))
N_DIL = len(DIL_PAIRS)
HEADS_PER_GROUP = 8
HEAD_DIM = 64
ATT_WIDTH = HEADS_PER_GROUP * HEAD_DIM
N_ATT_HEADS = N_DIL * HEADS_PER_GROUP
ATT_BLOCK = 128
NEG_INF = -1e30
REL_BUCKETS = 32
REL_MAX_EXACT = 16
REL_MAX_DIST = 2048
N_EXPERTS = 32
TOP_K = 4
D_EXPERT = D_MODEL
SWIGLU_LIMIT = 7.0
SWIGLU_ALPHA = 1.702
MOE_BLOCK = 128
DN_ALPHA = (2 * DEPTH) ** 0.25
DN_BETA = (8 * DEPTH) ** -0.25
LN_EPS = 1e-5
UV_COLS = 2 * GM_WIDTH
QKV_COLS = N_DIL * 3 * ATT_WIDTH
GATE_COLS = 2 * D_MODEL
IN_COLS = UV_COLS + QKV_COLS + GATE_COLS

kernel_name = "hybrid_gmlp_dilated_attn_moe_deepnorm"


def layer_norm(x, g=None, b=None):
    xf = x.astype(jnp.float32)
    mu = xf.mean(-1, keepdims=True)
    var = jnp.square(xf - mu).mean(-1, keepdims=True)
    y = (xf - mu) * lax.rsqrt(var + LN_EPS)
    if g is not None:
        y = y * g + b
    return y.astype(x.dtype)


def t5_bucket(dist):
    d = dist.astype(jnp.float32)
    large = REL_MAX_EXACT + jnp.log(jnp.maximum(d, float(REL_MAX_EXACT)) / REL_MAX_EXACT) / math.log(REL_MAX_DIST / REL_MAX_EXACT) * (REL_BUCKETS - REL_MAX_EXACT)
    large = jnp.minimum(large.astype(jnp.int32), REL_BUCKETS - 1)
    return jnp.where(dist < REL_MAX_EXACT, dist, large)


def chunked_spatial_gating(u, v, ln_g, ln_b, w_s, b_s):
    B, S, _ = u.shape
    nc = S // GM_CHUNK
    v = layer_norm(v, ln_g, ln_b)
    vc = v.reshape(B, nc, GM_CHUNK, GM_GROUPS, GM_WIDTH // GM_GROUPS)
    causal = jnp.tril(jnp.ones((GM_CHUNK, GM_CHUNK), dtype=bool))
    ws = jnp.where(causal[None], w_s, 0)
    s = jnp.einsum('gts,bnsgc->bntgc', ws, vc) + b_s.T[:, :, None]
    return u * s.reshape(B, S, GM_WIDTH)


def dilated_group_attention(q, k, v, bias_table, window, dilation):
    B, S, H, Dh = q.shape
    span = window // dilation
    L = S // dilation
    nb = -(-L // ATT_BLOCK)
    Lp = nb * ATT_BLOCK

    def to_sub(t):
        return t.reshape(B, L, dilation, H, Dh).transpose(0, 2, 1, 3, 4)

    qs, ks, vs = to_sub(q), to_sub(k), to_sub(v)
    qb = jnp.pad(qs, ((0, 0), (0, 0), (0, Lp - L), (0, 0), (0, 0))).reshape(B, dilation, nb, ATT_BLOCK, H, Dh)

    def key_blocks(t):
        tp = jnp.pad(t, ((0, 0), (0, 0), (ATT_BLOCK, Lp - L), (0, 0), (0, 0)))
        prev = tp[:, :, :Lp].reshape(B, dilation, nb, ATT_BLOCK, H, Dh)
        cur = tp[:, :, ATT_BLOCK:].reshape(B, dilation, nb, ATT_BLOCK, H, Dh)
        return jnp.concatenate([prev, cur], axis=3)

    kb, vb = key_blocks(ks), key_blocks(vs)
    qi = jnp.arange(ATT_BLOCK)[:, None]
    ki = jnp.arange(2 * ATT_BLOCK)[None, :]
    didx = qi + ATT_BLOCK - ki
    band = (didx >= 0) & (didx <= span)
    first = (jnp.arange(nb) == 0)[:, None, None]
    valid = band[None] & ~(first & (ki < ATT_BLOCK)[None])
    bucket = t5_bucket(jnp.clip(didx, 0, None) * dilation)
    bias = bias_table[bucket].astype(jnp.float32).transpose(2, 0, 1)

    logits = jnp.einsum('brnqhd,brnkhd->brnhqk', qb, kb, preferred_element_type=jnp.float32) * (Dh ** -0.5) + bias
    logits = jnp.where(valid[None, None, :, None], logits, NEG_INF)
    m = logits.max(-1, keepdims=True)
    p = jnp.exp(logits - m)
    den = p.sum(-1, keepdims=True)
    o = jnp.einsum('brnhqk,brnkhd->brnqhd', (p / den).astype(v.dtype), vb)
    lse = (m + jnp.log(den))[..., 0]
    o = o.reshape(B, dilation, Lp, H, Dh)[:, :, :L].transpose(0, 2, 1, 3, 4).reshape(B, S, H, Dh)
    lse = lse.transpose(0, 1, 2, 4, 3).reshape(B, dilation, Lp, H)[:, :, :L].transpose(0, 2, 1, 3).reshape(B, S, H)
    return o, lse


def moe_ffn(h, w_router, b_router, w_gate, b_gate, w_up, b_up, w_down, b_down):
    B, S, D = h.shape
    T = B * S
    xt = h.reshape(T, D)
    logits = (xt @ w_router).astype(jnp.float32) + b_router
    top_v, top_e = lax.top_k(logits, TOP_K)
    top_w = jax.nn.softmax(top_v, axis=-1)
    A = T * TOP_K
    e_flat = top_e.reshape(A)
    tok_flat = jnp.arange(A, dtype=jnp.int32) // TOP_K
    w_flat = top_w.reshape(A)
    order = jnp.argsort(e_flat)
    e_sorted = e_flat[order]
    counts = jnp.bincount(e_flat, length=N_EXPERTS)
    starts = jnp.cumsum(counts) - counts
    pcounts = (counts + MOE_BLOCK - 1) // MOE_BLOCK * MOE_BLOCK
    pends = jnp.cumsum(pcounts)
    pstarts = pends - pcounts
    dest = pstarts[e_sorted] + jnp.arange(A, dtype=jnp.int32) - starts[e_sorted]
    nblk = A // MOE_BLOCK + N_EXPERTS
    R = nblk * MOE_BLOCK
    row_tok = jnp.zeros((R,), jnp.int32).at[dest].set(tok_flat[order])
    row_w = jnp.zeros((R,), jnp.float32).at[dest].set(w_flat[order])
    blk_e = jnp.minimum(jnp.searchsorted(pends, jnp.arange(nblk, dtype=jnp.int32) * MOE_BLOCK, side='right'), N_EXPERTS - 1)

    def expert_block(args):
        tok, e = args
        xb = xt[tok]
        g = xb @ w_gate[e] + b_gate[e]
        u = xb @ w_up[e] + b_up[e]
        g = jnp.minimum(g, SWIGLU_LIMIT)
        u = jnp.clip(u, -SWIGLU_LIMIT, SWIGLU_LIMIT)
        act = (u + 1) * (g * jax.nn.sigmoid(SWIGLU_ALPHA * g))
        return act @ w_down[e] + b_down[e]

    yb = lax.map(expert_block, (row_tok.reshape(nblk, MOE_BLOCK), blk_e))
    y = jnp.zeros((T, D), jnp.float32).at[row_tok].add(yb.reshape(R, D).astype(jnp.float32) * row_w[:, None])
    return y.astype(h.dtype).reshape(B, S, D)


def setup_inputs(seed: int = 0) -> dict:
    key = jax.random.key(seed)
    ks = jax.random.split(key, 32)
    nrm = jax.random.normal
    f32 = jnp.float32
    inp = {}
    inp['x'] = nrm(ks[0], (BATCH, SEQ, D_MODEL), f32)
    inp['c'] = nrm(ks[1], (BATCH, D_MODEL), f32)
    inp['w_ada'] = nrm(ks[2], (DEPTH, D_MODEL, 6 * D_MODEL), f32) * (0.5 * D_MODEL ** -0.5)
    inp['b_ada'] = nrm(ks[3], (DEPTH, 6 * D_MODEL), f32) * 0.02
    inp['w_in'] = nrm(ks[4], (DEPTH, D_MODEL, IN_COLS), f32) * D_MODEL ** -0.5
    inp['gm_ln_g'] = 1.0 + 0.02 * nrm(ks[5], (DEPTH, GM_WIDTH), f32)
    inp['gm_ln_b'] = 0.02 * nrm(ks[6], (DEPTH, GM_WIDTH), f32)
    inp['gm_w_s'] = nrm(ks[7], (DEPTH, GM_GROUPS, GM_CHUNK, GM_CHUNK), f32) * (0.5 * GM_CHUNK ** -0.5)
    inp['gm_b_s'] = 1.0 + 0.02 * nrm(ks[8], (DEPTH, GM_GROUPS, GM_CHUNK), f32)
    inp['w_branch_a'] = nrm(ks[9], (DEPTH, GM_WIDTH, D_MODEL), f32) * GM_WIDTH ** -0.5
    inp['w_branch_b'] = nrm(ks[10], (DEPTH, ATT_WIDTH, D_MODEL), f32) * ATT_WIDTH ** -0.5
    inp['w_out'] = nrm(ks[11], (DEPTH, D_MODEL, D_MODEL), f32) * (DN_BETA * D_MODEL ** -0.5)
    inp['rel_bias'] = 0.5 * nrm(ks[12], (REL_BUCKETS, N_ATT_HEADS), f32)
    inp['ln1_g'] = 1.0 + 0.02 * nrm(ks[13], (DEPTH, D_MODEL), f32)
    inp['ln1_b'] = 0.02 * nrm(ks[14], (DEPTH, D_MODEL), f32)
    inp['w_router'] = nrm(ks[15], (DEPTH, D_MODEL, N_EXPERTS), f32) * D_MODEL ** -0.5
    inp['b_router'] = 0.01 * nrm(ks[16], (DEPTH, N_EXPERTS), f32)
    inp['w_gate'] = nrm(ks[17], (DEPTH, N_EXPERTS, D_MODEL, D_EXPERT), f32) * D_MODEL ** -0.5
    inp['b_gate'] = 0.02 * nrm(ks[18], (DEPTH, N_EXPERTS, D_EXPERT), f32)
    inp['w_up'] = nrm(ks[19], (DEPTH, N_EXPERTS, D_MODEL, D_EXPERT), f32) * D_MODEL ** -0.5
    inp['b_up'] = 0.02 * nrm(ks[20], (DEPTH, N_EXPERTS, D_EXPERT), f32)
    inp['w_down'] = nrm(ks[21], (DEPTH, N_EXPERTS, D_EXPERT, D_MODEL), f32) * (DN_BETA * D_EXPERT ** -0.5)
    inp['b_down'] = 0.02 * nrm(ks[22], (DEPTH, N_EXPERTS, D_MODEL), f32)
    inp['ln2_g'] = 1.0 + 0.02 * nrm(ks[23], (DEPTH, D_MODEL), f32)
    inp['ln2_b'] = 0.02 * nrm(ks[24], (DEPTH, D_MODEL), f32)
    return inp


def reference(x, c, w_ada, b_ada, w_in, gm_ln_g, gm_ln_b, gm_w_s, gm_b_s, w_branch_a, w_branch_b, w_out, rel_bias, ln1_g, ln1_b, w_router, b_router, w_gate, b_gate, w_up, b_up, w_down, b_down, ln2_g, ln2_b):
    B, S, _ = x.shape
    for l in range(DEPTH):
        mod = jax.nn.silu(c) @ w_ada[l] + b_ada[l]
        sh1, sc1, g1, sh2, sc2, g2 = jnp.split(mod[:, None, :], 6, axis=-1)

        h = layer_norm(x) * (1 + sc1) + sh1
        proj = h @ w_in[l]
        uv, qkv, gates = jnp.split(proj, [UV_COLS, UV_COLS + QKV_COLS], axis=-1)
        u, v = jnp.split(jax.nn.gelu(uv), 2, axis=-1)
        y_a = chunked_spatial_gating(u, v, gm_ln_g[l], gm_ln_b[l], gm_w_s[l], gm_b_s[l])
        qkv = qkv.reshape(B, S, N_DIL, 3, HEADS_PER_GROUP, HEAD_DIM)
        outs, lses = [], []
        for g, (win, dil) in enumerate(DIL_PAIRS):
            o, lse = dilated_group_attention(qkv[:, :, g, 0], qkv[:, :, g, 1], qkv[:, :, g, 2],
                                             rel_bias[:, g * HEADS_PER_GROUP:(g + 1) * HEADS_PER_GROUP], win, dil)
            outs.append(o)
            lses.append(lse)
        wts = jax.nn.softmax(jnp.stack(lses, axis=0), axis=0)
        y_b = jnp.sum(wts[..., None] * jnp.stack(outs, axis=0).astype(jnp.float32), axis=0)
        y_b = y_b.astype(x.dtype).reshape(B, S, ATT_WIDTH)
        gate_a, gate_b = jnp.split(jax.nn.sigmoid(gates), 2, axis=-1)
        merged = gate_a * (y_a @ w_branch_a[l]) + gate_b * (y_b @ w_branch_b[l])
        mix = merged @ w_out[l]
        x = layer_norm(DN_ALPHA * x + g1 * mix, ln1_g[l], ln1_b[l])

        h = layer_norm(x) * (1 + sc2) + sh2
        ffn = moe_ffn(h, w_router[l], b_router[l], w_gate[l], b_gate[l], w_up[l], b_up[l], w_down[l], b_down[l])
        x = layer_norm(DN_ALPHA * x + g2 * ffn, ln2_g[l], ln2_b[l])
    return x
```

```python
import contextlib
import math
import numpy as np
import concourse.bass as bass
import concourse.mybir as mybir
from concourse.bass_utils import run_bass_kernel_spmd

F32 = mybir.dt.float32
BF16 = mybir.dt.bfloat16
I32 = mybir.dt.int32
U32 = mybir.dt.uint32
AF = mybir.ActivationFunctionType
ALU = mybir.AluOpType
AX = mybir.AxisListType

D = 1024
NTOK = 2048
NCORES = 8
NEXP = 32
DIL = (1, 4, 16)
ALPHA = 2.0 ** 0.25
EPS = 1e-5
IN_COLS = 7680
import os
PIPE_ATTN = os.environ.get('PIPE_ATTN', '1') == '1'
ZIP_STAGES = os.environ.get('ZIP_STAGES', '1') == '1'
KCAP = [((min(2048, 8192 // (r + 1)) + 127) // 128) * 128 for r in range(32)]
KBASE = [sum(KCAP[:r]) for r in range(32)]
NSLOT = sum(KCAP)

ENGINES = ("pe", "act", "dve", "pool", "sp")
COMPUTE = ("pe", "act", "dve", "pool")
HANDLES = {"pe": "tensor", "act": "scalar", "dve": "vector", "pool": "gpsimd", "sp": "sync"}


class Sched:
    NDMASEM = 12
    SAME_ENG_WINDOW = 3

    def __init__(self, nc):
        self.nc = nc
        self.ops = {e: [] for e in ENGINES}
        self.lastw = {}
        self.readers = {}
        self.capture = None

    def add(self, eng, fn, reads=(), writes=(), dma=False):
        if self.capture is not None:
            self.capture.append((eng, fn, tuple(reads), tuple(writes), dma))
            return None
        ops = self.ops[eng]
        idx = len(ops)
        deps = {}
        dmadeps = set()

        def anydep(p):
            if p == (eng, idx):
                return
            if self.ops[p[0]][p[1]]["dma"]:
                dmadeps.add(p)
            elif deps.get(p[0], -1) < p[1]:
                deps[p[0]] = p[1]

        for k in reads:
            w = self.lastw.get(k)
            if w is not None:
                anydep(w)
        for k in writes:
            w = self.lastw.get(k)
            if w is not None:
                anydep(w)
            for r in self.readers.get(k, ()):
                anydep(r)
        for k in reads:
            self.readers.setdefault(k, []).append((eng, idx))
        for k in writes:
            self.lastw[k] = (eng, idx)
            self.readers[k] = []
        if eng in deps and not dma:
            if eng == "pe" or deps[eng] < idx - self.SAME_ENG_WINDOW:
                del deps[eng]
        ops.append(dict(fn=fn, deps=deps, dmadeps=sorted(dmadeps), dma=dma, marked=False))
        return (eng, idx)

    def zipped(self, *stage_fns):
        if not ZIP_STAGES:
            for f in stage_fns:
                f()
            return
        lists = []
        for f in stage_fns:
            self.capture = []
            f()
            lists.append(self.capture)
            self.capture = None
        items = []
        for li, l in enumerate(lists):
            n = len(l)
            for i, op in enumerate(l):
                items.append(((i + 0.5) / max(n, 1), li, i, op))
        items.sort(key=lambda x: (x[0], x[1], x[2]))
        for _, _, _, op in items:
            self.add(*op)

    def barrier(self):
        last = {}
        for e in COMPUTE:
            for i in range(len(self.ops[e]) - 1, -1, -1):
                op = self.ops[e][i]
                if op["fn"] is not None and not op["dma"]:
                    last[e] = i
                    break
        dmas = []
        for e in ENGINES:
            seen = 0
            for i in range(len(self.ops[e]) - 1, -1, -1):
                if self.ops[e][i]["dma"]:
                    dmas.append((e, i))
                    seen += 1
                    if seen >= self.NDMASEM:
                        break
        for e in ENGINES:
            if not self.ops[e]:
                continue
            deps = {p: i for p, i in last.items() if p != e}
            self.ops[e].append(dict(fn=None, deps=deps, dmadeps=sorted(dmas), dma=False, marked=False))
        self.lastw = {}
        self.readers = {}

    def emit(self):
        nc = self.nc
        ops = self.ops
        for e in ENGINES:
            for op in ops[e]:
                for pe_, pi in op["deps"].items():
                    ops[pe_][pi]["marked"] = True
        for e in COMPUTE:
            c = 0
            for op in ops[e]:
                if op["dma"] or op["fn"] is None:
                    op["cnt"] = c
                    continue
                if op["marked"]:
                    c += 1
                op["cnt"] = c
        stack = contextlib.ExitStack()
        csem = {e: stack.enter_context(nc.semaphore("cs_" + e)) for e in COMPUTE}
        for e in ENGINES:
            n = 0
            ring = None
            for op in ops[e]:
                if not op["dma"]:
                    continue
                if ring is None:
                    ring = [stack.enter_context(nc.semaphore("ds_%s_%d" % (e, i))) for i in range(self.NDMASEM)]
                s = n % self.NDMASEM
                k = n // self.NDMASEM
                op["dsem"] = ring[s]
                op["dsem_id"] = (e, s)
                op["dtarget"] = 16 * (k + 1)
                op["dprev"] = 16 * k
                n += 1
        block = stack.enter_context(nc.Block())

        def make(e):
            def body(h):
                waited = {}

                def wait(key, sem, val):
                    if val <= 0 or waited.get(key, 0) >= val:
                        return
                    waited[key] = val
                    h.wait_ge(sem, val)

                for op in ops[e]:
                    for pe_, pi in op["deps"].items():
                        wait(("c", pe_), csem[pe_], ops[pe_][pi]["cnt"])
                    for (qe, qi) in op["dmadeps"]:
                        q = ops[qe][qi]
                        wait(("d",) + q["dsem_id"], q["dsem"], q["dtarget"])
                    if op["fn"] is None:
                        continue
                    if op["dma"]:
                        wait(("d",) + op["dsem_id"], op["dsem"], op["dprev"])
                        ins = op["fn"](h)
                        ins.then_inc(op["dsem"], 16)
                    else:
                        ins = op["fn"](h)
                        if op["marked"]:
                            ins.then_inc(csem[e], 1)
            return body

        for e in ENGINES:
            if ops[e]:
                getattr(block, HANDLES[e])(make(e))
        stack.close()


class Arena:
    def __init__(self, ap_f32, nbytes):
        self.ap = ap_f32
        self.nbytes = nbytes

    def view(self, off, shape, dtype, parts=128):
        esz = 2 if dtype == BF16 else 4
        n = int(np.prod(shape[1:]))
        assert off % 4 == 0 and off + n * esz <= self.nbytes, (off, shape, self.nbytes)
        nw = (n * esz + 3) // 4
        v = self.ap[0:parts, off // 4: off // 4 + nw]
        if dtype != F32:
            v = v.bitcast(dtype)
        if len(shape) == 3:
            v = v.rearrange("p (a b) -> p a b", a=shape[1])
        elif len(shape) == 4:
            v = v.rearrange("p (a b c) -> p a b c", a=shape[1], b=shape[2])
        return v


def build(stop_after=None, n_experts=NEXP, dbg=()):
    nc = bass.Bass("TRN2", target_bir_lowering=False)

    def din(name, shape, dt=F32):
        return nc.dram_tensor(name, list(shape), dt, kind="ExternalInput").ap()

    xo = din("xo", [NTOK, D])
    xc = din("xc", [NTOK, D])
    flag_d = din("flag", [128, 1])
    cT_d = din("cT", [128, 8])
    w_ada = din("w_ada", [D, 6 * D])
    b_adaT = din("b_adaT", [128, 48])
    w_in = din("w_in", [D, IN_COLS])
    gm_g = din("gm_g", [1, 512])
    gm_b = din("gm_b", [1, 512])
    wsT_d = din("wsT", [128, 8 * 128])
    bsp_d = din("bsp", [128, 4 * 128])
    wa_d = din("wa", [512, D])
    wb_d = din("wb", [512, D])
    wo_d = din("wo", [D, D])
    biasT_d = din("biasT", [128, 24 * 256])
    maskT_d = din("maskT", [128, 256])
    ln1g_d = din("ln1g", [1, D])
    ln1b_d = din("ln1b", [1, D])
    wr_d = din("wr", [D, NEXP])
    br_d = din("br", [1, NEXP])
    wg_d = din("wg", [n_experts, D, D])
    wu_d = din("wu", [n_experts, D, D])
    wd_d = din("wd", [n_experts, D, D])
    bg_d = din("bg", [NEXP, D])
    bu_d = din("bu", [NEXP, D])
    kbtab_d = din("kbtab", [128, NEXP])
    bdn_d = din("bdn", [NEXP, D])
    ln2g_d = din("ln2g", [1, D])
    ln2b_d = din("ln2b", [1, D])
    ident_d = din("ident", [128, 128])
    out_d = nc.dram_tensor("out", [NTOK, D], F32, kind="ExternalOutput").ap()
    x1s_d = nc.dram_tensor("x1s", [NTOK, D], F32, kind="Internal").ap()
    xs_scr = nc.dram_tensor("xs_scr", [NSLOT, D], BF16, kind="Internal").ap()
    ys_scr = nc.dram_tensor("ys_scr", [NSLOT, D], F32, kind="Internal").ap()
    dbg_out = {}

    w_in_v = w_in.rearrange("(kc p) n -> p kc n", p=128)
    w_ada_v = w_ada.rearrange("(kc p) n -> p kc n", p=128)

    st = contextlib.ExitStack()
    ARENA_BYTES = 207 * 1024
    arena_t = st.enter_context(nc.sbuf_tensor("arena", [128, ARENA_BYTES // 4], F32))
    A = Arena(arena_t, ARENA_BYTES)
    psum_t = st.enter_context(nc.psum_tensor("psum", [128, 4096], F32))

    def bank(i, shape=None, dtype=F32, half=None):
        v = psum_t[:, i * 512:(i + 1) * 512]
        if dtype != F32:
            v = v.bitcast(dtype)
        if shape is not None and len(shape) == 3:
            v = v.rearrange("p (a b) -> p a b", a=shape[1])
        return v

    S = Sched(nc)
    K = 1024

    off = [0]

    def palloc(shape, dtype, parts=128):
        esz = 2 if dtype == BF16 else 4
        n = int(np.prod(shape[1:])) * esz
        n = (n + 3) // 4 * 4
        v = A.view(off[0], shape, dtype, parts)
        off[0] += n
        return v

    ident_f = palloc([128, 128], F32)
    ident_b = palloc([128, 128], BF16)
    ones_f = palloc([128, 128], F32)
    ones_b = palloc([128, 64], BF16)
    onesf_b = palloc([128, 64], BF16)
    flag_s = palloc([128, 1], F32)
    cT_s = palloc([128, 8], F32)
    scT = palloc([128, 8], F32)
    scT_b = palloc([128, 8], BF16)
    badaT = palloc([128, 48], F32)
    modT = palloc([128, 48], F32)
    opsc1 = palloc([128, 8], F32)
    opsc2 = palloc([128, 8], F32)
    G1B = palloc([128, 1024], F32)
    G2B = palloc([128, 1024], F32)
    gates_all = palloc([128, 16, NEXP], F32)
    bgR = palloc([128, 8, NEXP], F32)
    bu1R = palloc([128, 8, NEXP], F32)
    E4f = palloc([128, 16, 4], F32)
    W4 = palloc([128, 16, 4], F32)
    destI = palloc([128, 4, 16], I32)
    idxW = palloc([128, NEXP, 8], I32)
    Mall = palloc([128, 16, NEXP], BF16)
    mi8_ = [palloc([128, 8], U32) for _ in range(2)]
    e4_ = [palloc([128, 4], F32) for _ in range(2)]
    maskT = palloc([128, 256], F32)
    stt_ = [palloc([128, 4, 2, 6], F32) for _ in range(2)]
    mv_ = [palloc([128, 4, 2], F32) for _ in range(2)]
    sd_ = [palloc([128, 4], F32) for _ in range(2)]
    rstd_ = [palloc([128, 4], F32) for _ in range(2)]
    nmr_ = [palloc([128, 4], F32) for _ in range(2)]
    mx8_ = [palloc([128, 8], F32) for _ in range(2)]
    negm_ = [palloc([128, 1], F32) for _ in range(2)]
    den_ = [palloc([128, 1], F32) for _ in range(2)]
    rden_ = [palloc([128, 1], F32) for _ in range(2)]
    lg_ = [palloc([128, NEXP], F32) for _ in range(2)]
    msk_ = [palloc([128, NEXP], F32) for _ in range(2)]
    ex_ = [palloc([128, NEXP], F32) for _ in range(2)]
    em_ = [palloc([128, NEXP], F32) for _ in range(2)]
    brB = palloc([128, NEXP], F32)
    R0_END = 32 * K
    assert off[0] <= R0_END, off[0]
    MOE_TMP = off[0]

    def dma(q, out, in_, reads=(), writes=()):
        return S.add(q, lambda h: h.dma_start(out=out, in_=in_), reads=reads, writes=writes, dma=True)

    def mm(out, lhsT, rhs, start, stop, reads, writes):
        return S.add("pe", lambda h: h.matmul(out, lhsT=lhsT, rhs=rhs, start=start, stop=stop), reads=reads, writes=writes)

    def tr(out, in_, ident, reads, writes):
        return S.add("pe", lambda h: h.transpose(out, in_, ident), reads=reads, writes=writes)

    def act(out, in_, func, reads, writes, bias=None, scale=None):
        kw = {}
        if bias is not None:
            kw["bias"] = bias
        if scale is not None:
            kw["scale"] = scale
        return S.add("act", lambda h: h.activation(out=out, in_=in_, func=func, **kw), reads=reads, writes=writes)

    def tt(eng, out, in0, in1, op, reads, writes):
        return S.add(eng, lambda h: h.tensor_tensor(out=out, in0=in0, in1=in1, op=op), reads=reads, writes=writes)

    def ts(eng, out, in0, s1, s2, op0, op1, reads, writes):
        if op1 is None:
            return S.add(eng, lambda h: h.tensor_scalar(out=out, in0=in0, scalar1=s1, scalar2=None, op0=op0), reads=reads, writes=writes)
        return S.add(eng, lambda h: h.tensor_scalar(out=out, in0=in0, scalar1=s1, scalar2=s2, op0=op0, op1=op1), reads=reads, writes=writes)

    def stt(out, in0, scalar, in1, op0, op1, reads, writes):
        return S.add("dve", lambda h: h.scalar_tensor_tensor(out=out, in0=in0, scalar=scalar, in1=in1, op0=op0, op1=op1), reads=reads, writes=writes)

    def cp(eng, out, in_, reads, writes):
        if eng == "act":
            return S.add("act", lambda h: h.activation(out=out, in_=in_, func=AF.Copy), reads=reads, writes=writes)
        return S.add(eng, lambda h: h.tensor_copy(out=out, in_=in_), reads=reads, writes=writes)

    def memset(eng, ap, val, writes):
        return S.add(eng, lambda h: h.memset(ap, val), writes=writes)

    def bcast(row_ap):
        return row_ap.partition_broadcast(128)

    def ln_stats(x_ap, slot, t, key_in, nhalf=2):
        for hh in range(nhalf):
            S.add("dve", lambda h, hh=hh: h.bn_stats(out=stt_[slot][:, t, hh, :], in_=x_ap[:, hh * 512:(hh + 1) * 512]),
                  reads=[key_in], writes=[("st", slot, t, hh)])
        src = stt_[slot][:, t, 0:nhalf, :].rearrange("p a b -> p (a b)")
        S.add("dve", lambda h: h.bn_aggr(out=mv_[slot][:, t, :], in_=src),
              reads=[("st", slot, t, hh) for hh in range(nhalf)], writes=[("mv", slot, t)])

    def ln_finish(slot, nt):
        act(sd_[slot][:, 0:nt], mv_[slot][:, 0:nt, 1], AF.Ln, reads=[("mv", slot, t) for t in range(nt)] + ["eps"],
            writes=[("sd", slot)], bias=EPS_AP[0], scale=1.0)
        act(rstd_[slot][:, 0:nt], sd_[slot][:, 0:nt], AF.Exp, reads=[("sd", slot)], writes=[("rstd", slot)], scale=-0.5)
        stt(nmr_[slot][:, 0:nt], mv_[slot][:, 0:nt, 0], -1.0, rstd_[slot][:, 0:nt], ALU.mult, ALU.mult,
            reads=[("mv", slot, t) for t in range(nt)] + [("rstd", slot)], writes=[("nmr", slot)])

    EPS_AP = [None]
    eps_t = A.view(MOE_TMP, [128, 1], F32)
    EPS_AP[0] = eps_t
    MOE_TMP += 4

    dma("sp", ident_f, ident_d, writes=["ident_f"])
    dma("sp", flag_s, flag_d, writes=["flag"])
    dma("sp", cT_s, cT_d, writes=["cT"])
    dma("sp", badaT, b_adaT, writes=["badaT"])
    dma("sp", maskT, maskT_d, writes=["maskT"])
    memset("dve", eps_t, EPS, writes=["eps"])
    memset("dve", ones_f, 1.0, writes=["ones_f"])
    memset("dve", ones_b, 1.0, writes=["ones_b"])
    cp("dve", ident_b, ident_f, reads=["ident_f"], writes=["ident_b"])
    ts("dve", onesf_b, ones_b, flag_s[:, 0:1], None, ALU.mult, None, reads=["flag", "ones_b"], writes=["onesf_b"])
    act(scT, cT_s, AF.Silu, reads=["cT"], writes=["scT"])
    cp("dve", scT_b, scT, reads=["scT"], writes=["scT_b"])

    R5 = 144 * K
    wad = [A.view(R5 + i * 8 * K, [128, 8, 512], BF16) for i in range(4)]
    psM = bank(7)
    for blk in range(12):
        wbuf = wad[blk % 4]
        dma("pool", wbuf, w_ada_v[:, :, blk * 512:(blk + 1) * 512], writes=[("wad", blk % 4)])
        for jj in range(4):
            j = blk * 4 + jj
            for kc in range(8):
                mm(psM[:, j:j + 1], wbuf[:, kc, jj * 128:(jj + 1) * 128], scT_b[:, kc:kc + 1], kc == 0, kc == 7,
                   reads=[("wad", blk % 4), "scT_b"], writes=[("ps", 7)])
    tt("dve", modT, psM[:, 0:48], badaT, ALU.add, reads=[("ps", 7), "badaT"], writes=["modT"])
    ts("dve", opsc1, modT[:, 8:16], 1.0, None, ALU.add, None, reads=["modT"], writes=["opsc1"])
    ts("dve", opsc2, modT[:, 32:40], 1.0, None, ALU.add, None, reads=["modT"], writes=["opsc2"])
    dg = [A.view(R5 + 32 * K + i * 512, [128, 128], F32) for i in range(2)]
    for gi, (GB, col0) in enumerate(((G1B, 16), (G2B, 40))):
        for c in range(8):
            d_ = dg[c % 2]
            ts("dve", d_, ident_f, modT[:, col0 + c:col0 + c + 1], None, ALU.mult, None,
               reads=["ident_f", "modT"], writes=[("dg", c % 2)])
            pb = bank(gi * 2 + c // 4)
            mm(pb[:, (c % 4) * 128:(c % 4 + 1) * 128], ones_f, d_, True, True,
               reads=["ones_f", ("dg", c % 2)], writes=[("ps", gi * 2 + c // 4)])
        for hh in range(2):
            cp("dve", GB[:, hh * 512:(hh + 1) * 512], bank(gi * 2 + hh), reads=[("ps", gi * 2 + hh)], writes=[("GB", gi)])
    sh1 = modT[:, 0:8]
    sh2 = modT[:, 24:32]

    R1 = 32 * K
    R2 = 64 * K
    hT = A.view(R2, [128, 8, 4096], BF16)
    R3 = R2 + 64 * K
    uT = A.view(R3, [128, 4, NTOK], BF16)
    R4 = R3 + 16 * K
    ybT = A.view(R4, [128, 4, NTOK], BF16)
    R5 = R4 + 16 * K
    xs = [A.view(R1 + i * 4 * K, [128, 1024], F32) for i in range(8)]
    xn = [A.view(180 * K + i * 2 * K, [128, 1024], BF16) for i in range(4)]

    def x_tile(tt_):
        return xc[tt_ * 128:(tt_ + 1) * 128, :] if tt_ < 16 else xo[(tt_ - 16) * 128:(tt_ - 15) * 128, :]

    evac_flip = [0]

    def evac_mod(out, in_, scale_ap, bias_ap, reads, writes, eng=None):
        if eng is None:
            evac_flip[0] ^= 1
            eng = "act" if evac_flip[0] else "dve"
        if eng == "act":
            act(out, in_, AF.Identity, reads=reads, writes=writes, bias=bias_ap, scale=scale_ap)
        else:
            ts("dve", out, in_, scale_ap, bias_ap, ALU.mult, ALU.add, reads=reads, writes=writes)

    for b in range(8):
        slot = b % 2
        xo_ = (b % 2) * 4
        for t in range(4):
            tile = b * 4 + t
            dma("sp", xs[xo_ + t], x_tile(tile), writes=[("xs", xo_ + t)])
            ln_stats(xs[xo_ + t], slot, t, ("xs", xo_ + t))
        ln_finish(slot, 4)
        for hb in range(2):
            b0 = 2 * (hb + 2 * (b % 2))
            pT = psum_t[:, b0 * 512:(b0 + 2) * 512].bitcast(BF16).rearrange("p (a b) -> p a b", a=8)
            pkey = ("ps", b0)
            for t2 in range(2):
                t = hb * 2 + t2
                act(xn[t], xs[xo_ + t], AF.Identity, reads=[("xs", xo_ + t), ("rstd", slot), ("nmr", slot)], writes=[("xn", t)],
                    bias=nmr_[slot][:, t:t + 1], scale=rstd_[slot][:, t:t + 1])
                for kc in range(8):
                    tr(pT[:, kc, t2 * 128:(t2 + 1) * 128], xn[t][:, kc * 128:(kc + 1) * 128], ident_b,
                       reads=[("xn", t), "ident_b"], writes=[pkey, ("ps", b0 + 1)])
            tok0 = b * 512 + hb * 256
            for kc in range(8):
                evac_mod(hT[:, kc, tok0:tok0 + 256], pT[:, kc, :], opsc1[:, kc:kc + 1], sh1[:, kc:kc + 1],
                         reads=[pkey, ("ps", b0 + 1), "opsc1", "modT"], writes=[("hT", b, kc)], eng=("act" if kc < 4 else "dve"))

    if "hT" in dbg:
        dbg_out["hT"] = (hT, [128, 8 * 4096], BF16, [("hT", b, kc) for b in range(8) for kc in range(8)])

    NWR = 7
    wring = [A.view(R5 + i * 2 * K, [128, 8, 128], BF16) for i in range(NWR)]
    wr_n = [0]

    def load_chunk(src_v, col0, ncols=128):
        i = wr_n[0] % NWR
        wr_n[0] += 1
        dma("pool", wring[i][:, :, 0:ncols], src_v[:, :, col0:col0 + ncols], writes=[("wring", i)])
        return wring[i], ("wring", i)

    ps_n = [0]

    def next_bank(lo=0, n=2):
        i = lo + ps_n[0] % n
        ps_n[0] += 1
        return i

    def proj_T(wbuf, wkey, tb, nk=8, src=None, srckeys=None, bank_lo=0, bank_n=2):
        bi = next_bank(bank_lo, bank_n)
        pb = bank(bi)
        for kc in range(nk):
            mm(pb, wbuf[:, kc, :], hT[:, kc, tb * 512:(tb + 1) * 512], kc == 0, kc == nk - 1,
               reads=[wkey, ("hT", tb, kc)], writes=[("ps", bi)])
        return pb, ("ps", bi)

    if stop_after != "hT":
        GM = R1
        gmgB = A.view(GM, [128, 512], F32)
        gmbB = A.view(GM + 2 * K, [128, 512], F32)
        BSp = A.view(GM + 4 * K, [128, 4, 128], F32)
        wsT_f = A.view(GM + 6 * K, [128, 8, 128], F32)
        wsT_b = A.view(GM + 10 * K, [128, 8, 128], BF16)
        vg = [A.view(GM + 12 * K + i * 2 * K, [128, 512], F32) for i in range(2)]
        zz = [A.view(GM + 16 * K + i * 2 * K, [128, 512], F32) for i in range(2)]
        vn = [A.view(GM + 20 * K + i * K, [128, 512], BF16) for i in range(2)]
        tmpg = [A.view(GM + 22 * K + i * 2 * K, [128, 4, 128], F32) for i in range(2)]
        Wvg = A.view(R5 + 14 * K, [128, 8, 512], BF16)
        S.barrier()
        dma("sp", gmgB, bcast(gm_g[0:1, :]), writes=["gmgB"])
        dma("sp", gmbB, bcast(gm_b[0:1, :]), writes=["gmbB"])
        dma("sp", BSp, bsp_d.rearrange("p (a b) -> p a b", a=4), writes=["BSp"])
        dma("sp", wsT_f, wsT_d.rearrange("p (a b) -> p a b", a=8), writes=["wsT_f"])
        S.add("pool", lambda h: h.affine_select(out=wsT_f, in_=wsT_f, pattern=[[0, 8], [1, 128]], compare_op=ALU.is_ge,
                                                 fill=0.0, base=0, channel_multiplier=-1),
              reads=["wsT_f"], writes=["wsT_f"])
        cp("pool", wsT_b, wsT_f, reads=["wsT_f"], writes=["wsT_b"])
        dma("pool", Wvg, w_in_v[:, :, 512:1024], writes=["Wvg"])
        for c in range(4):
            wbuf, wkey = load_chunk(w_in_v, c * 128)
            for tb in range(4, 8):
                pb, pkey = proj_T(wbuf, wkey, tb)
                act(uT[:, c, (tb - 4) * 512:(tb - 3) * 512], pb, AF.Gelu_apprx_tanh, reads=[pkey], writes=[("uT", tb - 4)])
        def gmlpA(ti):
            tile = 16 + ti
            sl = ti % 2
            bi = next_bank(0, 2)
            pb = bank(bi)
            for kc in range(8):
                mm(pb, hT[:, kc, tile * 128:(tile + 1) * 128], Wvg[:, kc, :], kc == 0, kc == 7,
                   reads=[("hT", tile // 4, kc), "Wvg"], writes=[("ps", bi)])
            act(vg[sl], pb, AF.Gelu_apprx_tanh, reads=[("ps", bi)], writes=[("vg", sl)])
            ln_stats(vg[sl], sl, 0, ("vg", sl), nhalf=1)
            ln_finish(sl, 1)
            act(zz[sl], vg[sl], AF.Identity, reads=[("vg", sl), ("rstd", sl), ("nmr", sl)], writes=[("zz", sl)],
                bias=nmr_[sl][:, 0:1], scale=rstd_[sl][:, 0:1])
            tt("dve", zz[sl], zz[sl], gmgB, ALU.mult, reads=[("zz", sl), "gmgB"], writes=[("zz", sl)])
            tt("dve", vn[sl], zz[sl], gmbB, ALU.add, reads=[("zz", sl), "gmbB"], writes=[("vn", sl)])

        def gmlpB(ti):
            sl = ti % 2
            bs_i = 2 + ti % 2
            pS = bank(bs_i, [128, 4, 128])
            for gp in range(4):
                for hh in range(2):
                    g_ = 2 * gp + hh
                    mm(pS[hh * 64:(hh + 1) * 64, gp, :], vn[sl][:, g_ * 64:(g_ + 1) * 64], wsT_b[:, g_, :], True, True,
                       reads=[("vn", sl), "wsT_b"], writes=[("ps", bs_i)])
            tt("dve", tmpg[sl], pS, BSp, ALU.add, reads=[("ps", bs_i), "BSp"], writes=[("tmpg", sl)])
            tt("dve", uT[:, :, ti * 128:(ti + 1) * 128], tmpg[sl], uT[:, :, ti * 128:(ti + 1) * 128], ALU.mult,
               reads=[("tmpg", sl), ("uT", ti // 4)], writes=[("uT", ti // 4)])

        gmlpA(0)
        for ti in range(16):
            if ti + 1 < 16:
                S.zipped(lambda: gmlpA(ti + 1), lambda: gmlpB(ti))
            else:
                gmlpB(ti)
        if "yaT" in dbg:
            dbg_out["yaT"] = (uT, [128, 4 * NTOK], BF16, [("uT", i) for i in range(4)])

    if stop_after not in ("hT", "gmlp"):
        AT = R1
        accND = A.view(AT, [128, 2, NTOK], F32)
        sbS = [A.view(AT + 16 * K + i * K, [128, 256], F32) for i in range(4)]
        ptS = [A.view(AT + 20 * K + i * 512, [128, 256], BF16) for i in range(4)]
        braw = [A.view(AT + 22 * K + i * 2 * K, [128, 2, 256], F32) for i in range(2)]
        biasm = [A.view(AT + 26 * K + i * 2 * K, [128, 2, 256], F32) for i in range(2)]
        QKV0 = R5 + 14 * K
        QT = [A.view(QKV0, [128, NTOK], BF16), A.view(QKV0 + 20 * K, [128, NTOK], BF16)]
        KT = [A.view(QKV0 + 4 * K, [128, 4096], BF16)] * 2
        VV = [A.view(QKV0 + 12 * K, [128, 32, 128], BF16), A.view(QKV0 + 24 * K, [128, 32, 128], BF16)]
        assert QKV0 + 32 * K <= ARENA_BYTES
        S.barrier()
        ztile = A.view(MOE_TMP, [128, 1024], BF16)
        memset("pool", ztile, 0.0, writes=["ztile"])
        ZR = 1024
        zlist = list(range(0, NSLOT, ZR))

        def zero_fill(cnt):
            for _ in range(cnt):
                if not zlist:
                    return
                z0 = zlist.pop(0)
                zn_ = min(ZR, NSLOT - z0)
                dma("sp", xs_scr[z0:z0 + zn_, :].rearrange("(t p) d -> p t d", p=128),
                    ztile.unsqueeze(1).broadcast_to([128, zn_ // 128, 1024]), reads=["ztile"], writes=[("xs_zero", z0)])
        unit_n = 0
        for j in range(4):
            for g in range(3):
                r = DIL[g]
                nb_rho = 16 // r
                sl = unit_n % 2
                unit_n += 1
                kq = ("QT", sl)
                kk = ("KT", 0)
                col = 1024 + g * 1536 + j * 128
                wq, wqk = load_chunk(w_in_v, col)
                wk, wkk = load_chunk(w_in_v, col + 512)
                wv, wvk = load_chunk(w_in_v, col + 1024)
                dma("sp", braw[sl], biasT_d.rearrange("p (a b) -> p a b", a=24)[:, g * 8 + 2 * j:g * 8 + 2 * j + 2, :],
                    writes=[("braw", sl)])
                zero_fill(3)
                for hh in range(2):
                    tt("pool", biasm[sl][:, hh, :], braw[sl][:, hh, :], maskT, ALU.add,
                       reads=[("braw", sl), "maskT"], writes=[("biasm", sl)])
                for tb in range(4, 8):
                    pb, pkey = proj_T(wq, wqk, tb)
                    act(QT[sl][:, (tb - 4) * 512:(tb - 3) * 512], pb, AF.Identity, reads=[pkey], writes=[kq], scale=0.125)
                tb_lo = 0 if g == 2 else 3
                for tb in range(tb_lo, 8):
                    pb, pkey = proj_T(wk, wkk, tb)
                    cp("dve", KT[sl][:, tb * 512:(tb + 1) * 512], pb, reads=[pkey], writes=[kk])
                for rho in range(r):
                    for ib in range(2 * nb_rho):
                        ctx = ib < nb_rho
                        if ctx and g < 2 and ib != nb_rho - 1:
                            continue
                        vt = (0 if ctx else 16) + rho * nb_rho + ib % nb_rho
                        bi = 2 + (vt % 2)
                        pv = bank(bi)[:, 0:128]
                        start = rho + r * 128 * ib
                        for kc in range(8):
                            mm(pv, hT[:, kc, start:start + 127 * r + 1:r], wv[:, kc, :], kc == 0, kc == 7,
                               reads=[("hT", b_, kc) for b_ in range(start // 512, (start + 128 * r - 1) // 512 + 1)] + [wvk],
                               writes=[("ps", bi)])
                        if ctx:
                            act(VV[sl][:, vt, :], pv, AF.Identity, reads=[("ps", bi), "flag"], writes=[("V", sl)], scale=flag_s[:, 0:1])
                        else:
                            cp("dve", VV[sl][:, vt, :], pv, reads=[("ps", bi)], writes=[("V", sl)])
                def unit_geom(qb):
                    rho = qb // nb_rho
                    ib_cur = nb_rho + qb % nb_rho
                    qcol0 = rho + r * 128 * ib_cur - NTOK
                    q_sl = slice(qcol0, qcol0 + 127 * r + 1, r)
                    ktile = []
                    for ib in (ib_cur - 1, ib_cur):
                        ctx = ib < nb_rho
                        vt = (0 if ctx else 16) + rho * nb_rho + ib % nb_rho
                        kst = rho + r * 128 * ib
                        ktile.append((slice(kst, kst + 127 * r + 1, r), vt, ctx))
                    return q_sl, ktile

                def unit_S(qb):
                    q_sl, ktile = unit_geom(qb)
                    u = qb % 2
                    for hh in range(2):
                        bS = 2 * hh + u
                        pS = bank(bS)[:, 0:256]
                        pk = ("ps", bS)
                        p0, p1 = hh * 64, hh * 64 + 64
                        for kb in range(2):
                            mm(pS[:, kb * 128:(kb + 1) * 128], KT[sl][p0:p1, ktile[kb][0]], QT[sl][p0:p1, q_sl], True, True,
                               reads=[kk, kq], writes=[pk])
                        si = u * 2 + hh
                        stt(sbS[si], pS, 60.0, biasm[sl][:, hh, :], ALU.min, ALU.add,
                            reads=[pk, ("biasm", sl)], writes=[("sbS", si)])
                        act(ptS[si], sbS[si], AF.Exp, reads=[("sbS", si)], writes=[("ptS", si)])

                def unit_PV(qb):
                    q_sl, ktile = unit_geom(qb)
                    u = qb % 2
                    pND = bank(4 + u)[:, 0:256].rearrange("p (a b) -> p a b", a=2)
                    pkN = ("ps", 4 + u)
                    for which in range(2):
                        for hh in range(2):
                            si = u * 2 + hh
                            p0, p1 = hh * 64, hh * 64 + 64
                            for kb in range(2):
                                _, vt, ctx = ktile[kb]
                                if which == 0:
                                    lhs = VV[sl][:, vt, p0:p1]
                                    rk = [("V", sl)]
                                else:
                                    lhs = onesf_b if ctx else ones_b
                                    rk = ["onesf_b", "ones_b"]
                                mm(pND[p0:p1, which, :], lhs, ptS[si][:, kb * 128:(kb + 1) * 128], kb == 0, kb == 1,
                                   reads=rk + [("ptS", si)], writes=[pkN])
                    dst = accND[:, :, q_sl]
                    if g == 0:
                        cp("dve", dst, pND, reads=[pkN], writes=["accND"])
                    else:
                        tt("dve", dst, pND, dst, ALU.add, reads=[pkN, "accND"], writes=["accND"])

                if PIPE_ATTN:
                    unit_S(0)
                    for qb in range(16):
                        if qb + 1 < 16:
                            unit_S(qb + 1)
                        unit_PV(qb)
                else:
                    for qb in range(16):
                        unit_S(qb)
                        unit_PV(qb)
            S.add("dve", lambda h: h.reciprocal(out=accND[:, 1, :], in_=accND[:, 1, :]), reads=["accND"], writes=["accND"])
            tt("dve", ybT[:, j, :], accND[:, 0, :], accND[:, 1, :], ALU.mult, reads=["accND"], writes=[("ybT", j)])
        if "ybT" in dbg:
            dbg_out["ybT"] = (ybT, [128, 4 * NTOK], BF16, [("ybT", i) for i in range(4)])

    if stop_after not in ("hT", "gmlp", "attn"):
        S.barrier()
        mT = A.view(R1, [128, 8, NTOK], BF16)
        Wa = A.view(R5 + 14 * K, [128, 4, 1024], BF16)
        Wb = A.view(R5 + 22 * K, [128, 4, 1024], BF16)
        gab = [A.view(R5 + 30 * K + i * 2 * K, [128, 512], F32) for i in range(4)]
        t12 = [A.view(R5 + 38 * K + i * 2 * K, [128, 512], F32) for i in range(4)]
        dma("pool", Wa, wa_d.rearrange("(kc p) n -> p kc n", p=128), writes=["Wa"])
        dma("pool", Wb, wb_d.rearrange("(kc p) n -> p kc n", p=128), writes=["Wb"])
        it = 0
        for m in range(8):
            wga, wgak = load_chunk(w_in_v, 5632 + m * 128)
            wgb, wgbk = load_chunk(w_in_v, 6656 + m * 128)
            for tb in range(4):
                s2 = it % 2
                it += 1
                pa = bank(0 + s2 * 4)
                pbb = bank(1 + s2 * 4)
                for kc in range(4):
                    mm(pa, Wa[:, kc, m * 128:(m + 1) * 128], uT[:, kc, tb * 512:(tb + 1) * 512], kc == 0, kc == 3,
                       reads=["Wa", ("uT", tb)], writes=[("ps", 0 + s2 * 4)])
                for kc in range(4):
                    mm(pbb, Wb[:, kc, m * 128:(m + 1) * 128], ybT[:, kc, tb * 512:(tb + 1) * 512], kc == 0, kc == 3,
                       reads=["Wb"] + [("ybT", jj) for jj in range(4)], writes=[("ps", 1 + s2 * 4)])
                pga, pgak = proj_T(wga, wgak, 4 + tb, bank_lo=2 + s2 * 4, bank_n=1)
                pgb, pgbk = proj_T(wgb, wgbk, 4 + tb, bank_lo=3 + s2 * 4, bank_n=1)
                act(gab[s2 * 2], pga, AF.Sigmoid, reads=[pgak], writes=[("gab", s2 * 2)])
                act(gab[s2 * 2 + 1], pgb, AF.Sigmoid, reads=[pgbk], writes=[("gab", s2 * 2 + 1)])
                tt("dve", t12[s2 * 2], pa, gab[s2 * 2], ALU.mult, reads=[("ps", 0 + s2 * 4), ("gab", s2 * 2)], writes=[("t12", s2 * 2)])
                tt("dve", t12[s2 * 2 + 1], pbb, gab[s2 * 2 + 1], ALU.mult, reads=[("ps", 1 + s2 * 4), ("gab", s2 * 2 + 1)], writes=[("t12", s2 * 2 + 1)])
                tt("pool", mT[:, m, tb * 512:(tb + 1) * 512], t12[s2 * 2], t12[s2 * 2 + 1], ALU.add,
                   reads=[("t12", s2 * 2), ("t12", s2 * 2 + 1)], writes=[("mT", tb)])
        if "mT" in dbg:
            dbg_out["mT"] = (mT, [128, 8 * NTOK], BF16, [("mT", i) for i in range(4)])

        S.barrier()
        H2 = R2
        xnb = A.view(H2, [128, 16, 1024], BF16)
        O5 = H2 + 32 * K
        Wo = A.view(O5, [128, 8, 1024], BF16)
        ln1gB = A.view(O5 + 16 * K, [128, 1024], F32)
        ln1bB = A.view(O5 + 20 * K, [128, 1024], F32)
        xs5 = [A.view(O5 + 24 * K + i * 4 * K, [128, 1024], F32) for i in range(2)]
        zt = [A.view(O5 + 32 * K + i * 4 * K, [128, 1024], F32) for i in range(2)]
        x1t = [A.view(O5 + 40 * K + i * 4 * K, [128, 1024], F32) for i in range(2)]
        wr_s = A.view(MOE_TMP, [128, 8, NEXP], F32)
        h2Tf = A.view(MOE_TMP + 6 * K, [128, 8, 128], F32)
        assert MOE_TMP + 10 * K + 512 <= R0_END
        dma("pool", Wo, wo_d.rearrange("(kc p) n -> p kc n", p=128), writes=["Wo"])
        dma("sp", ln1gB, bcast(ln1g_d[0:1, :]), writes=["ln1gB"])
        dma("sp", ln1bB, bcast(ln1b_d[0:1, :]), writes=["ln1bB"])
        dma("sp", wr_s, wr_d.rearrange("(kc p) n -> p kc n", p=128), writes=["wr_s"])
        dma("sp", brB, bcast(br_d[0:1, :]), writes=["brB"])
        def stage5A(ti):
            s2 = ti % 2
            dma("sp", xs5[s2], xo[ti * 128:(ti + 1) * 128, :], writes=[("xs5", s2)])
            for hh in range(2):
                bi = hh + 2 * s2
                pb = bank(bi)
                for kc in range(8):
                    mm(pb, mT[:, kc, ti * 128:(ti + 1) * 128], Wo[:, kc, hh * 512:(hh + 1) * 512], kc == 0, kc == 7,
                       reads=[("mT", ti // 4), "Wo"], writes=[("ps", bi)])
                tt("dve", zt[s2][:, hh * 512:(hh + 1) * 512], pb, G1B[:, hh * 512:(hh + 1) * 512], ALU.mult,
                   reads=[("ps", bi), ("GB", 0)], writes=[("zt", s2)])
            stt(zt[s2], xs5[s2], ALPHA, zt[s2], ALU.mult, ALU.add, reads=[("xs5", s2), ("zt", s2)], writes=[("zt", s2)])
            ln_stats(zt[s2], s2, 0, ("zt", s2))
            ln_finish(s2, 1)
            act(zt[s2], zt[s2], AF.Identity, reads=[("zt", s2), ("rstd", s2), ("nmr", s2)], writes=[("zt", s2)],
                bias=nmr_[s2][:, 0:1], scale=rstd_[s2][:, 0:1])
            tt("dve", zt[s2], zt[s2], ln1gB, ALU.mult, reads=[("zt", s2), "ln1gB"], writes=[("zt", s2)])
            tt("dve", x1t[s2], zt[s2], ln1bB, ALU.add, reads=[("zt", s2), "ln1bB"], writes=[("x1t", s2)])
            dma("sp", x1s_d[ti * 128:(ti + 1) * 128, :], x1t[s2], reads=[("x1t", s2)], writes=[("x1s", ti)])
            ln_stats(x1t[s2], s2, 1, ("x1t", s2))
            act(sd_[s2][:, 1:2], mv_[s2][:, 1:2, 1], AF.Ln, reads=[("mv", s2, 1)], writes=[("sd2", s2)], bias=eps_t, scale=1.0)
            act(rstd_[s2][:, 1:2], sd_[s2][:, 1:2], AF.Exp, reads=[("sd2", s2)], writes=[("rstd2", s2)], scale=-0.5)
            stt(nmr_[s2][:, 1:2], mv_[s2][:, 1:2, 0], -1.0, rstd_[s2][:, 1:2], ALU.mult, ALU.mult,
                reads=[("mv", s2, 1), ("rstd2", s2)], writes=[("nmr2", s2)])
            act(zt[s2], x1t[s2], AF.Identity, reads=[("x1t", s2), ("rstd2", s2), ("nmr2", s2)], writes=[("zt", s2)],
                bias=nmr_[s2][:, 1:2], scale=rstd_[s2][:, 1:2])
            cp("act", xnb[:, ti, :], zt[s2], reads=[("zt", s2)], writes=[("xnb", ti)])

        def stage5B(ti):
            s2 = ti % 2
            for hb in range(2):
                bi = 4 + hb
                pT = bank(bi, [128, 4, 128])
                for k4 in range(4):
                    kc = hb * 4 + k4
                    tr(pT[:, k4, :], zt[s2][:, kc * 128:(kc + 1) * 128], ident_f, reads=[("zt", s2), "ident_f"], writes=[("ps", bi)])
                for k4 in range(4):
                    kc = hb * 4 + k4
                    evac_mod(h2Tf[:, kc, :], pT[:, k4, :], opsc2[:, kc:kc + 1], sh2[:, kc:kc + 1],
                             reads=[("ps", bi), "opsc2", "modT"], writes=[("h2Tf", kc)], eng=("act" if hb == 0 else "dve"))
            pR = bank(6)[:, 0:NEXP]
            for kc in range(8):
                mm(pR, h2Tf[:, kc, :], wr_s[:, kc, :], kc == 0, kc == 7, reads=[("h2Tf", kc), "wr_s"], writes=[("ps", 6)])
            tt("dve", lg_[s2], pR, brB, ALU.add, reads=[("ps", 6), "brB"], writes=[("lg", s2)])
            S.add("dve", lambda h, s2=s2: h.max(out=mx8_[s2], in_=lg_[s2]), reads=[("lg", s2)], writes=[("mx8", s2)])
            ts("dve", msk_[s2], lg_[s2], mx8_[s2][:, 3:4], None, ALU.is_ge, None, reads=[("lg", s2), ("mx8", s2)], writes=[("msk", s2)])
            ts("dve", negm_[s2], mx8_[s2][:, 0:1], -1.0, None, ALU.mult, None, reads=[("mx8", s2)], writes=[("negm", s2)])
            act(ex_[s2], lg_[s2], AF.Exp, reads=[("lg", s2), ("negm", s2)], writes=[("ex", s2)], bias=negm_[s2][:, 0:1], scale=1.0)
            tt("dve", em_[s2], ex_[s2], msk_[s2], ALU.mult, reads=[("ex", s2), ("msk", s2)], writes=[("em", s2)])
            S.add("dve", lambda h, s2=s2: h.reduce_sum(out=den_[s2], in_=em_[s2], axis=AX.X), reads=[("em", s2)], writes=[("den", s2)])
            S.add("dve", lambda h, s2=s2: h.reciprocal(out=rden_[s2], in_=den_[s2]), reads=[("den", s2)], writes=[("rden", s2)])
            ts("dve", gates_all[:, ti, :], em_[s2], rden_[s2][:, 0:1], None, ALU.mult, None, reads=[("em", s2), ("rden", s2)], writes=[("gates", ti)])
            S.add("dve", lambda h, s2=s2: h.max_index(out=mi8_[s2], in_max=mx8_[s2], in_values=lg_[s2]), reads=[("lg", s2), ("mx8", s2)], writes=[("mi8", s2)])
            cp("dve", E4f[:, ti, :], mi8_[s2][:, 0:4], reads=[("mi8", s2)], writes=[("E4f", ti)])
            act(e4_[s2], mx8_[s2][:, 0:4], AF.Exp, reads=[("mx8", s2), ("negm", s2)], writes=[("e4", s2)], bias=negm_[s2][:, 0:1], scale=1.0)
            ts("dve", W4[:, ti, :], e4_[s2], rden_[s2][:, 0:1], None, ALU.mult, None, reads=[("e4", s2), ("rden", s2)], writes=[("W4", ti)])
            cp("pool", Mall[:, ti, :], msk_[s2], reads=[("msk", s2)], writes=[("Mall", ti)])

        stage5A(0)
        for ti in range(16):
            if ti + 1 < 16:
                S.zipped(lambda: stage5A(ti + 1), lambda: stage5B(ti))
            else:
                stage5B(ti)
        if "x1" in dbg:
            dbg_out["x1"] = None
        if "gates" in dbg:
            dbg_out["gates"] = (gates_all, [128, 16 * NEXP], F32, [("gates", i) for i in range(16)])

    if stop_after in (None, "moe", "route"):
        S.barrier()
        RT0 = R5
        within = A.view(RT0, [128, 16, NEXP], F32)
        tot = A.view(RT0 + 2 * K, [128, 16, NEXP], F32)
        base = A.view(RT0 + 4 * K, [128, 16, NEXP], F32)
        dall = A.view(RT0 + 6 * K, [128, 16, NEXP], F32)
        big1 = A.view(RT0 + 8 * K, [128, NEXP, NEXP], F32)
        big2 = A.view(RT0 + 12 * K, [128, NEXP, NEXP], F32)
        LTm = A.view(RT0 + 16 * K, [128, NEXP, NEXP], F32)
        oh = A.view(RT0 + 20 * K, [128, 16, NEXP], F32)
        U_f = A.view(RT0 + 22 * K, [128, 128], F32)
        U_b = A.view(RT0 + 22 * K + 512, [128, 128], BF16)
        on_b = A.view(RT0 + 22 * K + 768, [128, 128], BF16)
        iotaE = A.view(RT0 + 23 * K, [128, NEXP], F32)
        cnt = A.view(RT0 + 23 * K + 128, [128, NEXP], F32)
        rank = A.view(RT0 + 23 * K + 256, [128, NEXP], F32)
        kbv = A.view(RT0 + 23 * K + 384, [128, NEXP], F32)
        sig = A.view(RT0 + 23 * K + 512, [128, NEXP], F32)
        kbtab = A.view(RT0 + 23 * K + 640, [128, NEXP], F32)
        pk = A.view(RT0 + 23 * K + 768, [128, 8], F32)
        destF = A.view(RT0 + 24 * K, [128, 4, 16], F32)
        idxWf = A.view(RT0 + 24 * K + 256, [128, NEXP, 8], F32)
        rankT = A.view(RT0 + 26 * K, [NEXP, 1], F32, parts=NEXP)
        RTm = A.view(RT0 + 26 * K + 64, [NEXP, NEXP], F32, parts=NEXP)
        bgrow = A.view(RT0 + 27 * K, [NEXP, 1024], F32, parts=NEXP)
        burow = A.view(RT0 + 31 * K, [NEXP, 1024], F32, parts=NEXP)
        dma("sp", kbtab, kbtab_d, writes=["kbtab"])
        dma("sp", bgrow, bg_d, writes=["bgrow"])
        dma("sp", burow, bu_d, writes=["burow"])
        memset("pool", U_f, 1.0, writes=["U_f"])
        S.add("pool", lambda h: h.affine_select(out=U_f, in_=U_f, pattern=[[1, 128]], compare_op=ALU.is_gt, fill=0.0, base=0, channel_multiplier=-1),
              reads=["U_f"], writes=["U_f"])
        cp("pool", U_b, U_f, reads=["U_f"], writes=["U_b"])
        memset("pool", on_b, 1.0, writes=["on_b"])
        S.add("pool", lambda h: h.iota(iotaE, pattern=[[1, NEXP]], base=0, channel_multiplier=0, allow_small_or_imprecise_dtypes=True), writes=["iotaE"])
        S.add("pool", lambda h: h.iota(pk, pattern=[[128, 8]], base=0, channel_multiplier=1, allow_small_or_imprecise_dtypes=True), writes=["pk"])
        memset("pool", LTm, 1.0, writes=["LTm"])
        S.add("pool", lambda h: h.affine_select(out=LTm, in_=LTm, pattern=[[1, NEXP], [-1, NEXP]], compare_op=ALU.is_gt, fill=0.0, base=0, channel_multiplier=0),
              reads=["LTm"], writes=["LTm"])
        Mkeys = [("Mall", i) for i in range(16)]
        Mflat = Mall.rearrange("p a b -> p (a b)")
        mm(bank(0), U_b, Mflat, True, True, reads=["U_b"] + Mkeys, writes=[("ps", 0)])
        mm(bank(1), on_b, Mflat, True, True, reads=["on_b"] + Mkeys, writes=[("ps", 1)])
        cp("dve", within.rearrange("p a b -> p (a b)"), bank(0), reads=[("ps", 0)], writes=["within"])
        cp("act", tot.rearrange("p a b -> p (a b)"), bank(1), reads=[("ps", 1)], writes=["tot"])
        memset("dve", base[:, 0, :], 0.0, writes=["base"])
        for ti in range(1, 16):
            tt("dve", base[:, ti, :], base[:, ti - 1, :], tot[:, ti - 1, :], ALU.add, reads=["base", "tot"], writes=["base"])
        tt("dve", cnt, base[:, 15, :], tot[:, 15, :], ALU.add, reads=["base", "tot"], writes=["cnt"])
        cA = cnt.unsqueeze(1).broadcast_to([128, NEXP, NEXP])
        cB = cnt.unsqueeze(2).broadcast_to([128, NEXP, NEXP])
        tt("dve", big1, cA, cB, ALU.is_gt, reads=["cnt"], writes=["big1"])
        tt("dve", big2, cA, cB, ALU.is_equal, reads=["cnt"], writes=["big2"])
        tt("dve", big2, big2, LTm, ALU.mult, reads=["big2", "LTm"], writes=["big2"])
        tt("dve", big1, big1, big2, ALU.add, reads=["big1", "big2"], writes=["big1"])
        S.add("dve", lambda h: h.reduce_sum(out=rank, in_=big1, axis=AX.X), reads=["big1"], writes=["rank"])
        rA = rank.unsqueeze(2).broadcast_to([128, NEXP, NEXP])
        iB = iotaE.unsqueeze(1).broadcast_to([128, NEXP, NEXP])
        tt("dve", big1, rA, iB, ALU.is_equal, reads=["rank", "iotaE"], writes=["big1"])
        tt("dve", big2, big1, kbtab.unsqueeze(1).broadcast_to([128, NEXP, NEXP]), ALU.mult, reads=["big1", "kbtab"], writes=["big2"])
        S.add("dve", lambda h: h.reduce_sum(out=kbv, in_=big2, axis=AX.X), reads=["big2"], writes=["kbv"])
        tt("dve", big1, rank.unsqueeze(1).broadcast_to([128, NEXP, NEXP]), iotaE.unsqueeze(2).broadcast_to([128, NEXP, NEXP]), ALU.is_equal,
           reads=["rank", "iotaE"], writes=["big1"])
        tt("dve", big2, big1, iB, ALU.mult, reads=["big1", "iotaE"], writes=["big2"])
        S.add("dve", lambda h: h.reduce_sum(out=sig, in_=big2, axis=AX.X), reads=["big2"], writes=["sig"])
        tt("dve", dall, base, within, ALU.add, reads=["base", "within"], writes=["dall"])
        tt("dve", dall, dall, kbv.unsqueeze(1).broadcast_to([128, 16, NEXP]), ALU.add, reads=["dall", "kbv"], writes=["dall"])
        E4keys = [("E4f", i) for i in range(16)]
        for k in range(4):
            tt("dve", oh, iotaE.unsqueeze(1).broadcast_to([128, 16, NEXP]), E4f[:, :, k:k + 1].broadcast_to([128, 16, NEXP]), ALU.is_equal,
               reads=["iotaE"] + E4keys, writes=["oh"])
            tt("dve", oh, oh, dall, ALU.mult, reads=["oh", "dall"], writes=["oh"])
            S.add("dve", lambda h, k=k: h.reduce_sum(out=destF[:, k, :], in_=oh, axis=AX.X), reads=["oh"], writes=["destF"])
        cp("dve", destI, destF, reads=["destF"], writes=["destI"])
        ts("dve", sig, sig, 1024.0, None, ALU.mult, None, reads=["sig"], writes=["sig"])
        tt("dve", idxWf, sig.unsqueeze(2).broadcast_to([128, NEXP, 8]), pk.unsqueeze(1).broadcast_to([128, NEXP, 8]), ALU.add,
           reads=["sig", "pk"], writes=["idxWf"])
        cp("dve", idxW, idxWf, reads=["idxWf"], writes=["idxW"])
        pRk = bank(2)[0:NEXP, 0:128]
        tr(pRk, rank, ident_f, reads=["rank", "ident_f"], writes=[("ps", 2)])
        cp("act", rankT, pRk[:, 0:1], reads=[("ps", 2)], writes=["rankT"])
        ts("dve", RTm, iotaE[0:NEXP, :], rankT[:, 0:1], None, ALU.is_equal, None, reads=["iotaE", "rankT"], writes=["RTm"])
        for bi_, (rows, rkey, dstR, dkey) in enumerate(((bgrow, "bgrow", bgR, "bgR"), (burow, "burow", bu1R, "bu1R"))):
            pbk = bank(3 + bi_)
            for c in range(8):
                mm(pbk[:, c * NEXP:(c + 1) * NEXP], rows[:, c * 128:(c + 1) * 128], RTm, True, True, reads=[rkey, "RTm"], writes=[("ps", 3 + bi_)])
            cp("act", dstR.rearrange("p a b -> p (a b)"), pbk[:, 0:8 * NEXP], reads=[("ps", 3 + bi_)], writes=[dkey])
        ts("pool", bu1R, bu1R, 1.0, None, ALU.add, None, reads=["bu1R"], writes=["bu1R"])
        if "route" in dbg:
            dbg_out["gates"] = (gates_all, [128, 16 * NEXP], F32, [("gates", i) for i in range(16)])
            dbg_out["destI"] = (destI, [128, 64], I32, ["destI"])
            dbg_out["idxW"] = (idxW, [128, NEXP * 8], I32, ["idxW"])
            dbg_out["rank"] = (rank, [128, NEXP], F32, ["rank"])
            dbg_out["W4"] = (W4, [128, 64], F32, [("W4", i) for i in range(16)])
            dbg_out["bgR"] = (bgR, [128, 8 * NEXP], F32, ["bgR"])

    if stop_after is None or stop_after == "moe":
        for ti in range(16):
            for k in range(4):
                S.add("pool", lambda h, ti=ti, k=k: h.indirect_dma_start(
                    out=xs_scr[:, :], out_offset=bass.IndirectOffsetOnAxis(ap=destI[:, k, ti:ti + 1], axis=0),
                    in_=xnb[:, ti, :], in_offset=None), reads=["destI", ("xnb", ti)], writes=[("xs_scr", ti, k)], dma=True)
        S.barrier()
        xrow = [A.view(R5 + i * 8 * K, [128, 4, 1024], BF16) for i in range(2)]
        yrow = [A.view(R5 + 16 * K + i * 4 * K, [128, 1024], F32) for i in range(2)]
        xeT = [A.view(R1 + i * 8 * K, [128, 8, 512], BF16) for i in range(2)] + [A.view(R5 + 24 * K, [128, 8, 512], BF16)]
        actT = [A.view(R1 + 16 * K + i * 8 * K, [128, 8, 512], BF16) for i in range(2)]
        NWB = 6
        wbufs = [A.view(R2 + i * 16 * K, [128, 8, 1024], BF16) for i in range(NWB)]
        tmp_g = [A.view(MOE_TMP + i * 2 * K, [128, 512], F32) for i in range(2)]
        tmp_s = [A.view(MOE_TMP + 4 * K + i * 2 * K, [128, 512], F32) for i in range(2)]
        tmp_u = [A.view(MOE_TMP + 8 * K + i * 2 * K, [128, 512], F32) for i in range(2)]
        assert MOE_TMP + 12 * K <= R0_END, (MOE_TMP, R0_END)
        wn = [0]

        def load_w(src, r):
            i = wn[0] % NWB
            wn[0] += 1
            rows = src.rearrange("e k n -> (e k) n")
            for kc in range(8):
                S.add("pool", lambda h, i=i, kc=kc: h.indirect_dma_start(
                    out=wbufs[i][:, kc, :], out_offset=None, in_=rows[:, :],
                    in_offset=bass.IndirectOffsetOnAxis(ap=idxW[:, r, kc:kc + 1], axis=0)),
                    reads=["idxW"], writes=[("wb", i, kc)], dma=True)
            return wbufs[i], i

        def load_rank(r):
            return (load_w(wg_d, r), load_w(wu_d, r), load_w(wd_d, r))

        itc = [0]
        ytile_n = [0]

        def blk_load(blk):
            r, sb_, bidx, W = blk
            n = min(512, KCAP[r] - sb_ * 512)
            nt = n // 128
            row0 = KBASE[r] + sb_ * 512
            xr = bidx % 2
            dma("sp", xrow[xr][:, 0:nt, :], xs_scr[row0:row0 + n, :].rearrange("(t p) d -> p t d", p=128),
                reads=["xs_scr"], writes=[("xrow", xr)])

        def blk_T(blk):
            r, sb_, bidx, W = blk
            n = min(512, KCAP[r] - sb_ * 512)
            nt = n // 128
            xr, x3 = bidx % 2, bidx % 3
            for kg in range(2):
                pT = psum_t[:, 4 * 512:6 * 512].bitcast(BF16).rearrange("p (a b) -> p a b", a=4)
                for t in range(nt):
                    for k4 in range(4):
                        kc = kg * 4 + k4
                        tr(pT[:, k4, t * 128:(t + 1) * 128], xrow[xr][:, t, kc * 128:(kc + 1) * 128], ident_b,
                           reads=[("xrow", xr), "ident_b"], writes=[("ps", 4), ("ps", 5)])
                for k4 in range(4):
                    kc = kg * 4 + k4
                    evac_mod(xeT[x3][:, kc, 0:n], pT[:, k4, 0:n], opsc2[:, kc:kc + 1], sh2[:, kc:kc + 1],
                             reads=[("ps", 4), ("ps", 5), "opsc2", "modT"], writes=[("xeT", x3, kc)], eng=("act" if k4 < 2 else "dve"))

        def blk_GU(blk):
            r, sb_, bidx, W = blk
            (Wg_, Wgk), (Wu_, Wuk), (Wd_, Wdk) = W
            n = min(512, KCAP[r] - sb_ * 512)
            x3, xs2 = bidx % 3, bidx % 2
            for c in range(8):
                s2 = itc[0] % 2
                itc[0] += 1
                bg_i, bu_i = 0 + s2, 2 + s2
                pg, pu = bank(bg_i)[:, 0:n], bank(bu_i)[:, 0:n]
                for kc in range(8):
                    mm(pg, Wg_[:, kc, c * 128:(c + 1) * 128], xeT[x3][:, kc, 0:n], kc == 0, kc == 7,
                       reads=[("wb", Wgk, kc), ("xeT", x3, kc)], writes=[("ps", bg_i)])
                for kc in range(8):
                    mm(pu, Wu_[:, kc, c * 128:(c + 1) * 128], xeT[x3][:, kc, 0:n], kc == 0, kc == 7,
                       reads=[("wb", Wuk, kc), ("xeT", x3, kc)], writes=[("ps", bu_i)])
                tg, tsg, tu = tmp_g[s2][:, 0:n], tmp_s[s2][:, 0:n], tmp_u[s2][:, 0:n]
                ts("dve", tg, pg, bgR[:, c, r:r + 1], 7.0, ALU.add, ALU.min, reads=[("ps", bg_i), "bgR"], writes=[("tg", s2)])
                act(tsg, tg, AF.Sigmoid, reads=[("tg", s2)], writes=[("tsg", s2)], scale=1.702)
                act(tu, pu, AF.Identity, reads=[("ps", bu_i), "bu1R"], writes=[("tu", s2)], bias=bu1R[:, c, r:r + 1], scale=1.0)
                ts("dve", tu, tu, 8.0, -6.0, ALU.min, ALU.max, reads=[("tu", s2)], writes=[("tu", s2)])
                tt("dve", tsg, tg, tsg, ALU.mult, reads=[("tg", s2), ("tsg", s2)], writes=[("tsg", s2)])
                tt("dve", actT[xs2][:, c, 0:n], tsg, tu, ALU.mult, reads=[("tsg", s2), ("tu", s2)], writes=[("actT", xs2)])

        def blk_back(blk):
            r, sb_, bidx, W = blk
            xs2 = bidx % 2
            (Wg_, Wgk), (Wu_, Wuk), (Wd_, Wdk) = W
            n = min(512, KCAP[r] - sb_ * 512)
            nt = n // 128
            row0 = KBASE[r] + sb_ * 512
            for t in range(nt):
                ys2 = ytile_n[0] % 2
                ytile_n[0] += 1
                for hh in range(2):
                    bi = 6 + hh
                    py = bank(bi)
                    for c in range(8):
                        mm(py, actT[xs2][:, c, t * 128:(t + 1) * 128], Wd_[:, c, hh * 512:(hh + 1) * 512], c == 0, c == 7,
                           reads=[("actT", xs2), ("wb", Wdk, c)], writes=[("ps", bi)])
                    cp("act" if hh == 0 else "dve", yrow[ys2][:, hh * 512:(hh + 1) * 512], py, reads=[("ps", bi)], writes=[("yrow", ys2, hh)])
                dma("sp", ys_scr[row0 + t * 128:row0 + (t + 1) * 128, :], yrow[ys2], reads=[("yrow", ys2, 0), ("yrow", ys2, 1)], writes=[("ys_scr", ytile_n[0])])

        blist = []
        for r in range(n_experts):
            for sb_ in range((KCAP[r] + 511) // 512):
                blist.append([r, sb_, len(blist), None])
        NBLK = len(blist)
        Wsets = {0: load_rank(0)}

        def attach_w(bi_):
            r = blist[bi_][0]
            blist[bi_][3] = Wsets[r]

        blk_load(blist[0])
        if NBLK > 1:
            blk_load(blist[1])
        blk_T(blist[0])
        if NBLK > 2:
            blk_load(blist[2])
        if NBLK > 1:
            blk_T(blist[1])
        attach_w(0)
        blk_GU(blist[0])
        if 1 < n_experts:
            Wsets[1] = load_rank(1)
        for bi_ in range(NBLK):
            if bi_ + 3 < NBLK:
                blk_load(blist[bi_ + 3])
            if bi_ + 2 < NBLK:
                blk_T(blist[bi_ + 2])
            if bi_ + 1 < NBLK:
                attach_w(bi_ + 1)
                blk_GU(blist[bi_ + 1])
            blk_back(blist[bi_])
            if bi_ + 1 < NBLK and blist[bi_ + 1][1] == 0:
                r1 = blist[bi_ + 1][0]
                if r1 + 1 < n_experts and (r1 + 1) not in Wsets:
                    Wsets[r1 + 1] = load_rank(r1 + 1)
        S.barrier()
        F7 = R2
        ln2gB = A.view(F7, [128, 1024], F32)
        ln2bB = A.view(F7 + 4 * K, [128, 1024], F32)
        x1r = [A.view(F7 + 8 * K + i * 4 * K, [128, 1024], F32) for i in range(2)]
        z2 = [A.view(F7 + 16 * K + i * 4 * K, [128, 1024], F32) for i in range(2)]
        ot = [A.view(F7 + 24 * K + i * 4 * K, [128, 1024], F32) for i in range(2)]
        acct = [A.view(F7 + 32 * K + i * 4 * K, [128, 1024], F32) for i in range(2)]
        ygt = [A.view(F7 + 40 * K + i * 4 * K, [128, 1024], F32) for i in range(4)]
        bdn_s = A.view(F7 + 56 * K, [NEXP, 1024], F32, parts=NEXP)
        gT_s = [A.view(F7 + 60 * K + i * 512, [NEXP, 128], F32, parts=NEXP) for i in range(2)]
        dma("sp", ln2gB, bcast(ln2g_d[0:1, :]), writes=["ln2gB"])
        dma("sp", ln2bB, bcast(ln2b_d[0:1, :]), writes=["ln2bB"])
        dma("sp", bdn_s, bdn_d, writes=["bdn_s"])
        outs = []
        gi = 0
        gi_ = [0]
        def fin_A(ti):
            s2 = ti % 2
            dma("sp", x1r[s2], x1s_d[ti * 128:(ti + 1) * 128, :], reads=[("x1s", ti)], writes=[("x1r", s2)])
            pG = bank(4 + s2)[0:NEXP, 0:128]
            tr(pG, gates_all[:, ti, :], ident_f, reads=[("gates", ti), "ident_f"], writes=[("ps", 4 + s2)])
            cp("act", gT_s[s2], pG, reads=[("ps", 4 + s2)], writes=[("gT_s", s2)])
            for hh in range(2):
                bi = hh + 2 * s2
                pb = bank(bi)
                mm(pb, gT_s[s2], bdn_s[:, hh * 512:(hh + 1) * 512], True, True, reads=[("gT_s", s2), "bdn_s"], writes=[("ps", bi)])
                cp("act", acct[s2][:, hh * 512:(hh + 1) * 512], pb, reads=[("ps", bi)], writes=[("acct", s2)])
            for k in range(4):
                g4 = gi_[0] % 4
                gi_[0] += 1
                S.add("pool", lambda h, ti=ti, k=k, g4=g4: h.indirect_dma_start(
                    out=ygt[g4][:, :], out_offset=None, in_=ys_scr[:, :],
                    in_offset=bass.IndirectOffsetOnAxis(ap=destI[:, k, ti:ti + 1], axis=0)),
                    reads=["destI"], writes=[("ygt", g4)], dma=True)
                stt(acct[s2], ygt[g4], W4[:, ti, k:k + 1], acct[s2], ALU.mult, ALU.add,
                    reads=[("ygt", g4), ("W4", ti), ("acct", s2)], writes=[("acct", s2)])

        def fin_B(ti):
            s2 = ti % 2
            tt("dve", z2[s2], acct[s2], G2B, ALU.mult, reads=[("acct", s2), ("GB", 1)], writes=[("z2", s2)])
            stt(z2[s2], x1r[s2], ALPHA, z2[s2], ALU.mult, ALU.add, reads=[("x1r", s2), ("z2", s2)], writes=[("z2", s2)])
            ln_stats(z2[s2], s2, 0, ("z2", s2))
            ln_finish(s2, 1)
            act(z2[s2], z2[s2], AF.Identity, reads=[("z2", s2), ("rstd", s2), ("nmr", s2)], writes=[("z2", s2)],
                bias=nmr_[s2][:, 0:1], scale=rstd_[s2][:, 0:1])
            tt("dve", z2[s2], z2[s2], ln2gB, ALU.mult, reads=[("z2", s2), "ln2gB"], writes=[("z2", s2)])
            tt("dve", ot[s2], z2[s2], ln2bB, ALU.add, reads=[("z2", s2), "ln2bB"], writes=[("ot", s2)])
            dma("sp", out_d[ti * 128:(ti + 1) * 128, :], ot[s2], reads=[("ot", s2)], writes=[("out", ti)])
            outs.append(("out", ti))

        fin_A(0)
        for ti in range(16):
            if ti + 1 < 16:
                S.zipped(lambda: fin_A(ti + 1), lambda: fin_B(ti))
            else:
                fin_B(ti)
        S.add("sp", None, reads=outs)

    dkeys = []
    for name, spec in dbg_out.items():
        if spec is None:
            continue
        ap, shape, dt, keys = spec
        dd = nc.dram_tensor("dbg_" + name, shape, dt, kind="ExternalOutput").ap()
        flat = ap
        if len(ap.shape) == 3:
            flat = ap.rearrange("p a b -> p (a b)")
        dma("sp", dd, flat, reads=keys, writes=[("dbg", name)])
        dkeys.append(("dbg", name))
    if dkeys:
        S.add("sp", None, reads=dkeys)
    S.emit()
    st.close()
    return nc


def t5_bucket_np(dist):
    d = dist.astype(np.float32)
    large = np.float32(16) + np.log(np.maximum(d, np.float32(16.0)) / np.float32(16)) / np.float32(math.log(2048 / 16)) * np.float32(16)
    large = np.minimum(large.astype(np.int32), 31)
    return np.where(dist < 16, dist, large)


def host_layout(inp):
    f = np.float32
    x = np.asarray(inp["x"], f)
    shared = {}
    shared["w_ada"] = np.ascontiguousarray(np.asarray(inp["w_ada"], f)[0])
    shared["b_adaT"] = np.ascontiguousarray(np.asarray(inp["b_ada"], f)[0].reshape(48, 128).T)
    shared["w_in"] = np.ascontiguousarray(np.asarray(inp["w_in"], f)[0])
    shared["gm_g"] = np.asarray(inp["gm_ln_g"], f).reshape(1, 512)
    shared["gm_b"] = np.asarray(inp["gm_ln_b"], f).reshape(1, 512)
    ws = np.asarray(inp["gm_w_s"], f)[0]
    shared["wsT"] = np.ascontiguousarray(ws.transpose(2, 0, 1).reshape(128, 8 * 128))
    bs = np.asarray(inp["gm_b_s"], f)[0]
    bsp = np.empty((128, 4, 128), f)
    for gp in range(4):
        bsp[0:64, gp, :] = bs[2 * gp][None, :]
        bsp[64:128, gp, :] = bs[2 * gp + 1][None, :]
    shared["bsp"] = bsp.reshape(128, 512)
    shared["wa"] = np.ascontiguousarray(np.asarray(inp["w_branch_a"], f)[0])
    shared["wb"] = np.ascontiguousarray(np.asarray(inp["w_branch_b"], f)[0])
    shared["wo"] = np.ascontiguousarray(np.asarray(inp["w_out"], f)[0])
    rel = np.asarray(inp["rel_bias"], f)
    kk = np.arange(128)[:, None]
    qq = np.arange(128)[None, :]
    d_prev = qq + 128 - kk
    d_cur = qq - kk
    biasT = np.empty((128, 24, 2, 128), f)
    for g in range(3):
        bp = t5_bucket_np(np.clip(d_prev, 0, None).astype(np.int32) * DIL[g])
        bc = t5_bucket_np(np.clip(d_cur, 0, None).astype(np.int32) * DIL[g])
        for h in range(8):
            biasT[:, g * 8 + h, 0, :] = rel[bp, g * 8 + h]
            biasT[:, g * 8 + h, 1, :] = rel[bc, g * 8 + h]
    shared["biasT"] = biasT.reshape(128, 24 * 256)
    maskT = np.zeros((128, 2, 128), f)
    maskT[:, 0, :][d_prev > 128] = -1e30
    maskT[:, 1, :][d_cur < 0] = -1e30
    shared["maskT"] = maskT.reshape(128, 256)
    shared["ln1g"] = np.asarray(inp["ln1_g"], f).reshape(1, D)
    shared["ln1b"] = np.asarray(inp["ln1_b"], f).reshape(1, D)
    shared["wr"] = np.ascontiguousarray(np.asarray(inp["w_router"], f)[0])
    shared["br"] = np.asarray(inp["b_router"], f).reshape(1, NEXP)
    shared["wg"] = np.ascontiguousarray(np.asarray(inp["w_gate"], f)[0])
    shared["wu"] = np.ascontiguousarray(np.asarray(inp["w_up"], f)[0])
    shared["wd"] = np.ascontiguousarray(np.asarray(inp["w_down"], f)[0])
    shared["bg"] = np.ascontiguousarray(np.asarray(inp["b_gate"], f)[0])
    shared["bu"] = np.ascontiguousarray(np.asarray(inp["b_up"], f)[0])
    shared["kbtab"] = np.broadcast_to(np.asarray(KBASE, f)[None, :], (128, NEXP)).copy()
    shared["bdn"] = np.ascontiguousarray(np.asarray(inp["b_down"], f)[0])
    shared["ln2g"] = np.asarray(inp["ln2_g"], f).reshape(1, D)
    shared["ln2b"] = np.asarray(inp["ln2_b"], f).reshape(1, D)
    shared["ident"] = np.eye(128, dtype=f)
    c = np.asarray(inp["c"], f)
    in_maps = []
    for k in range(NCORES):
        b, hf = k // 2, k % 2
        m = dict(shared)
        m["xo"] = np.ascontiguousarray(x[b, hf * NTOK:(hf + 1) * NTOK])
        m["xc"] = np.ascontiguousarray(x[b, 0:NTOK]) if hf == 1 else np.zeros((NTOK, D), f)
        m["flag"] = np.full((128, 1), float(hf), f)
        m["cT"] = np.ascontiguousarray(c[b].reshape(8, 128).T)
        in_maps.append(m)
    return in_maps


_NC_CACHE = {}


def kernel(**inputs):
    in_maps = host_layout(inputs)
    if "nc" not in _NC_CACHE:
        _NC_CACHE["nc"] = build()
    nc = _NC_CACHE["nc"]
    res = run_bass_kernel_spmd(nc, in_maps, core_ids=list(range(NCORES)))
    out = np.empty((4, 4096, D), np.float32)
    for k in range(NCORES):
        b, hf = k // 2, k % 2
        out[b, hf * NTOK:(hf + 1) * NTOK] = np.asarray(res.results[k]["out"], np.float32)
    return out
```

```python
import contextlib
import math
import numpy as np
import concourse.bass as bass
import concourse.mybir as mybir
from concourse.bass_utils import run_bass_kernel_spmd

F32 = mybir.dt.float32
BF16 = mybir.dt.bfloat16
I32 = mybir.dt.int32
U32 = mybir.dt.uint32
AF = mybir.ActivationFunctionType
ALU = mybir.AluOpType
AX = mybir.AxisListType

D = 1024
NTOK = 2048
NCORES = 8
NEXP = 32
DIL = (1, 4, 16)
ALPHA = 2.0 ** 0.25
EPS = 1e-5
IN_COLS = 7680
import os
PIPE_ATTN = os.environ.get('PIPE_ATTN', '1') == '1'
ZIP_STAGES = os.environ.get('ZIP_STAGES', '1') == '1'
KCAP = [((min(2048, 8192 // (r + 1)) + 127) // 128) * 128 for r in range(32)]
KBASE = [sum(KCAP[:r]) for r in range(32)]
NSLOT = sum(KCAP)

ENGINES = ("pe", "act", "dve", "pool", "sp")
COMPUTE = ("pe", "act", "dve", "pool")
HANDLES = {"pe": "tensor", "act": "scalar", "dve": "vector", "pool": "gpsimd", "sp": "sync"}


class Sched:
    NDMASEM = 12
    SAME_ENG_WINDOW = 3

    def __init__(self, nc):
        self.nc = nc
        self.ops = {e: [] for e in ENGINES}
        self.lastw = {}
        self.readers = {}
        self.capture = None

    def add(self, eng, fn, reads=(), writes=(), dma=False):
        if self.capture is not None:
            self.capture.append((eng, fn, tuple(reads), tuple(writes), dma))
            return None
        ops = self.ops[eng]
        idx = len(ops)
        deps = {}
        dmadeps = set()

        def anydep(p):
            if p == (eng, idx):
                return
            if self.ops[p[0]][p[1]]["dma"]:
                dmadeps.add(p)
            elif deps.get(p[0], -1) < p[1]:
                deps[p[0]] = p[1]

        for k in reads:
            w = self.lastw.get(k)
            if w is not None:
                anydep(w)
        for k in writes:
            w = self.lastw.get(k)
            if w is not None:
                anydep(w)
            for r in self.readers.get(k, ()):
                anydep(r)
        for k in reads:
            self.readers.setdefault(k, []).append((eng, idx))
        for k in writes:
            self.lastw[k] = (eng, idx)
            self.readers[k] = []
        if eng in deps and not dma:
            if eng == "pe" or deps[eng] < idx - self.SAME_ENG_WINDOW:
                del deps[eng]
        ops.append(dict(fn=fn, deps=deps, dmadeps=sorted(dmadeps), dma=dma, marked=False))
        return (eng, idx)

    def zipped(self, *stage_fns):
        if not ZIP_STAGES:
            for f in stage_fns:
                f()
            return
        lists = []
        for f in stage_fns:
            self.capture = []
            f()
            lists.append(self.capture)
            self.capture = None
        items = []
        for li, l in enumerate(lists):
            n = len(l)
            for i, op in enumerate(l):
                items.append(((i + 0.5) / max(n, 1), li, i, op))
        items.sort(key=lambda x: (x[0], x[1], x[2]))
        for _, _, _, op in items:
            self.add(*op)

    def barrier(self):
        last = {}
        for e in COMPUTE:
            for i in range(len(self.ops[e]) - 1, -1, -1):
                op = self.ops[e][i]
                if op["fn"] is not None and not op["dma"]:
                    last[e] = i
                    break
        dmas = []
        for e in ENGINES:
            seen = 0
            for i in range(len(self.ops[e]) - 1, -1, -1):
                if self.ops[e][i]["dma"]:
                    dmas.append((e, i))
                    seen += 1
                    if seen >= self.NDMASEM:
                        break
        for e in ENGINES:
            if not self.ops[e]:
                continue
            deps = {p: i for p, i in last.items() if p != e}
            self.ops[e].append(dict(fn=None, deps=deps, dmadeps=sorted(dmas), dma=False, marked=False))
        self.lastw = {}
        self.readers = {}

    def emit(self):
        nc = self.nc
        ops = self.ops
        for e in ENGINES:
            for op in ops[e]:
                for pe_, pi in op["deps"].items():
                    ops[pe_][pi]["marked"] = True
        for e in COMPUTE:
            c = 0
            for op in ops[e]:
                if op["dma"] or op["fn"] is None:
                    op["cnt"] = c
                    continue
                if op["marked"]:
                    c += 1
                op["cnt"] = c
        stack = contextlib.ExitStack()
        csem = {e: stack.enter_context(nc.semaphore("cs_" + e)) for e in COMPUTE}
        for e in ENGINES:
            n = 0
            ring = None
            for op in ops[e]:
                if not op["dma"]:
                    continue
                if ring is None:
                    ring = [stack.enter_context(nc.semaphore("ds_%s_%d" % (e, i))) for i in range(self.NDMASEM)]
                s = n % self.NDMASEM
                k = n // self.NDMASEM
                op["dsem"] = ring[s]
                op["dsem_id"] = (e, s)
                op["dtarget"] = 16 * (k + 1)
                op["dprev"] = 16 * k
                n += 1
        block = stack.enter_context(nc.Block())

        def make(e):
            def body(h):
                waited = {}

                def wait(key, sem, val):
                    if val <= 0 or waited.get(key, 0) >= val:
                        return
                    waited[key] = val
                    h.wait_ge(sem, val)

                for op in ops[e]:
                    for pe_, pi in op["deps"].items():
                        wait(("c", pe_), csem[pe_], ops[pe_][pi]["cnt"])
                    for (qe, qi) in op["dmadeps"]:
                        q = ops[qe][qi]
                        wait(("d",) + q["dsem_id"], q["dsem"], q["dtarget"])
                    if op["fn"] is None:
                        continue
                    if op["dma"]:
                        wait(("d",) + op["dsem_id"], op["dsem"], op["dprev"])
                        ins = op["fn"](h)
                        ins.then_inc(op["dsem"], 16)
                    else:
                        ins = op["fn"](h)
                        if op["marked"]:
                            ins.then_inc(csem[e], 1)
            return body

        for e in ENGINES:
            if ops[e]:
                getattr(block, HANDLES[e])(make(e))
        stack.close()


class Arena:
    def __init__(self, ap_f32, nbytes):
        self.ap = ap_f32
        self.nbytes = nbytes

    def view(self, off, shape, dtype, parts=128):
        esz = 2 if dtype == BF16 else 4
        n = int(np.prod(shape[1:]))
        assert off % 4 == 0 and off + n * esz <= self.nbytes, (off, shape, self.nbytes)
        nw = (n * esz + 3) // 4
        v = self.ap[0:parts, off // 4: off // 4 + nw]
        if dtype != F32:
            v = v.bitcast(dtype)
        if len(shape) == 3:
            v = v.rearrange("p (a b) -> p a b", a=shape[1])
        elif len(shape) == 4:
            v = v.rearrange("p (a b c) -> p a b c", a=shape[1], b=shape[2])
        return v


def build(stop_after=None, n_experts=NEXP, dbg=()):
    nc = bass.Bass("TRN2", target_bir_lowering=False)

    def din(name, shape, dt=F32):
        return nc.dram_tensor(name, list(shape), dt, kind="ExternalInput").ap()

    xo = din("xo", [NTOK, D])
    xc = din("xc", [NTOK, D])
    flag_d = din("flag", [128, 1])
    cT_d = din("cT", [128, 8])
    w_ada = din("w_ada", [D, 6 * D])
    b_adaT = din("b_adaT", [128, 48])
    w_in = din("w_in", [D, IN_COLS])
    gm_g = din("gm_g", [1, 512])
    gm_b = din("gm_b", [1, 512])
    wsT_d = din("wsT", [128, 8 * 128])
    bsp_d = din("bsp", [128, 4 * 128])
    wa_d = din("wa", [512, D])
    wb_d = din("wb", [512, D])
    wo_d = din("wo", [D, D])
    biasT_d = din("biasT", [128, 24 * 256])
    maskT_d = din("maskT", [128, 256])
    ln1g_d = din("ln1g", [1, D])
    ln1b_d = din("ln1b", [1, D])
    wr_d = din("wr", [D, NEXP])
    br_d = din("br", [1, NEXP])
    wg_d = din("wg", [n_experts, D, D])
    wu_d = din("wu", [n_experts, D, D])
    wd_d = din("wd", [n_experts, D, D])
    bg_d = din("bg", [NEXP, D])
    bu_d = din("bu", [NEXP, D])
    kbtab_d = din("kbtab", [128, NEXP])
    bdn_d = din("bdn", [NEXP, D])
    ln2g_d = din("ln2g", [1, D])
    ln2b_d = din("ln2b", [1, D])
    ident_d = din("ident", [128, 128])
    out_d = nc.dram_tensor("out", [NTOK, D], F32, kind="ExternalOutput").ap()
    x1s_d = nc.dram_tensor("x1s", [NTOK, D], F32, kind="Internal").ap()
    xs_scr = nc.dram_tensor("xs_scr", [NSLOT, D], BF16, kind="Internal").ap()
    ys_scr = nc.dram_tensor("ys_scr", [NSLOT, D], F32, kind="Internal").ap()
    dbg_out = {}

    w_in_v = w_in.rearrange("(kc p) n -> p kc n", p=128)
    w_ada_v = w_ada.rearrange("(kc p) n -> p kc n", p=128)

    st = contextlib.ExitStack()
    ARENA_BYTES = 207 * 1024
    arena_t = st.enter_context(nc.sbuf_tensor("arena", [128, ARENA_BYTES // 4], F32))
    A = Arena(arena_t, ARENA_BYTES)
    psum_t = st.enter_context(nc.psum_tensor("psum", [128, 4096], F32))

    def bank(i, shape=None, dtype=F32, half=None):
        v = psum_t[:, i * 512:(i + 1) * 512]
        if dtype != F32:
            v = v.bitcast(dtype)
        if shape is not None and len(shape) == 3:
            v = v.rearrange("p (a b) -> p a b", a=shape[1])
        return v

    S = Sched(nc)
    K = 1024

    off = [0]

    def palloc(shape, dtype, parts=128):
        esz = 2 if dtype == BF16 else 4
        n = int(np.prod(shape[1:])) * esz
        n = (n + 3) // 4 * 4
        v = A.view(off[0], shape, dtype, parts)
        off[0] += n
        return v

    ident_f = palloc([128, 128], F32)
    ident_b = palloc([128, 128], BF16)
    ones_f = palloc([128, 128], F32)
    ones_b = palloc([128, 64], BF16)
    onesf_b = palloc([128, 64], BF16)
    flag_s = palloc([128, 1], F32)
    cT_s = palloc([128, 8], F32)
    scT = palloc([128, 8], F32)
    scT_b = palloc([128, 8], BF16)
    badaT = palloc([128, 48], F32)
    modT = palloc([128, 48], F32)
    opsc1 = palloc([128, 8], F32)
    opsc2 = palloc([128, 8], F32)
    G1B = palloc([128, 1024], F32)
    G2B = palloc([128, 1024], F32)
    gates_all = palloc([128, 16, NEXP], F32)
    bgR = palloc([128, 8, NEXP], F32)
    bu1R = palloc([128, 8, NEXP], F32)
    E4f = palloc([128, 16, 4], F32)
    W4 = palloc([128, 16, 4], F32)
    destI = palloc([128, 4, 16], I32)
    idxW = palloc([128, NEXP, 8], I32)
    Mall = palloc([128, 16, NEXP], BF16)
    mi8_ = [palloc([128, 8], U32) for _ in range(2)]
    e4_ = [palloc([128, 4], F32) for _ in range(2)]
    maskT = palloc([128, 256], F32)
    stt_ = [palloc([128, 4, 2, 6], F32) for _ in range(2)]
    mv_ = [palloc([128, 4, 2], F32) for _ in range(2)]
    sd_ = [palloc([128, 4], F32) for _ in range(2)]
    rstd_ = [palloc([128, 4], F32) for _ in range(2)]
    nmr_ = [palloc([128, 4], F32) for _ in range(2)]
    mx8_ = [palloc([128, 8], F32) for _ in range(2)]
    negm_ = [palloc([128, 1], F32) for _ in range(2)]
    den_ = [palloc([128, 1], F32) for _ in range(2)]
    rden_ = [palloc([128, 1], F32) for _ in range(2)]
    lg_ = [palloc([128, NEXP], F32) for _ in range(2)]
    msk_ = [palloc([128, NEXP], F32) for _ in range(2)]
    ex_ = [palloc([128, NEXP], F32) for _ in range(2)]
    em_ = [palloc([128, NEXP], F32) for _ in range(2)]
    brB = palloc([128, NEXP], F32)
    R0_END = 32 * K
    assert off[0] <= R0_END, off[0]
    MOE_TMP = off[0]

    def dma(q, out, in_, reads=(), writes=()):
        return S.add(q, lambda h: h.dma_start(out=out, in_=in_), reads=reads, writes=writes, dma=True)

    def mm(out, lhsT, rhs, start, stop, reads, writes):
        return S.add("pe", lambda h: h.matmul(out, lhsT=lhsT, rhs=rhs, start=start, stop=stop), reads=reads, writes=writes)

    def tr(out, in_, ident, reads, writes):
        return S.add("pe", lambda h: h.transpose(out, in_, ident), reads=reads, writes=writes)

    def act(out, in_, func, reads, writes, bias=None, scale=None):
        kw = {}
        if bias is not None:
            kw["bias"] = bias
        if scale is not None:
            kw["scale"] = scale
        return S.add("act", lambda h: h.activation(out=out, in_=in_, func=func, **kw), reads=reads, writes=writes)

    def tt(eng, out, in0, in1, op, reads, writes):
        return S.add(eng, lambda h: h.tensor_tensor(out=out, in0=in0, in1=in1, op=op), reads=reads, writes=writes)

    def ts(eng, out, in0, s1, s2, op0, op1, reads, writes):
        if op1 is None:
            return S.add(eng, lambda h: h.tensor_scalar(out=out, in0=in0, scalar1=s1, scalar2=None, op0=op0), reads=reads, writes=writes)
        return S.add(eng, lambda h: h.tensor_scalar(out=out, in0=in0, scalar1=s1, scalar2=s2, op0=op0, op1=op1), reads=reads, writes=writes)

    def stt(out, in0, scalar, in1, op0, op1, reads, writes):
        return S.add("dve", lambda h: h.scalar_tensor_tensor(out=out, in0=in0, scalar=scalar, in1=in1, op0=op0, op1=op1), reads=reads, writes=writes)

    def cp(eng, out, in_, reads, writes):
        if eng == "act":
            return S.add("act", lambda h: h.activation(out=out, in_=in_, func=AF.Copy), reads=reads, writes=writes)
        return S.add(eng, lambda h: h.tensor_copy(out=out, in_=in_), reads=reads, writes=writes)

    def memset(eng, ap, val, writes):
        return S.add(eng, lambda h: h.memset(ap, val), writes=writes)

    def bcast(row_ap):
        return row_ap.partition_broadcast(128)

    def ln_stats(x_ap, slot, t, key_in, nhalf=2):
        for hh in range(nhalf):
            S.add("dve", lambda h, hh=hh: h.bn_stats(out=stt_[slot][:, t, hh, :], in_=x_ap[:, hh * 512:(hh + 1) * 512]),
                  reads=[key_in], writes=[("st", slot, t, hh)])
        src = stt_[slot][:, t, 0:nhalf, :].rearrange("p a b -> p (a b)")
        S.add("dve", lambda h: h.bn_aggr(out=mv_[slot][:, t, :], in_=src),
              reads=[("st", slot, t, hh) for hh in range(nhalf)], writes=[("mv", slot, t)])

    def ln_finish(slot, nt):
        act(sd_[slot][:, 0:nt], mv_[slot][:, 0:nt, 1], AF.Ln, reads=[("mv", slot, t) for t in range(nt)] + ["eps"],
            writes=[("sd", slot)], bias=EPS_AP[0], scale=1.0)
        act(rstd_[slot][:, 0:nt], sd_[slot][:, 0:nt], AF.Exp, reads=[("sd", slot)], writes=[("rstd", slot)], scale=-0.5)
        stt(nmr_[slot][:, 0:nt], mv_[slot][:, 0:nt, 0], -1.0, rstd_[slot][:, 0:nt], ALU.mult, ALU.mult,
            reads=[("mv", slot, t) for t in range(nt)] + [("rstd", slot)], writes=[("nmr", slot)])

    EPS_AP = [None]
    eps_t = A.view(MOE_TMP, [128, 1], F32)
    EPS_AP[0] = eps_t
    MOE_TMP += 4

    dma("sp", ident_f, ident_d, writes=["ident_f"])
    dma("sp", flag_s, flag_d, writes=["flag"])
    dma("sp", cT_s, cT_d, writes=["cT"])
    dma("sp", badaT, b_adaT, writes=["badaT"])
    dma("sp", maskT, maskT_d, writes=["maskT"])
    memset("dve", eps_t, EPS, writes=["eps"])
    memset("dve", ones_f, 1.0, writes=["ones_f"])
    memset("dve", ones_b, 1.0, writes=["ones_b"])
    cp("dve", ident_b, ident_f, reads=["ident_f"], writes=["ident_b"])
    ts("dve", onesf_b, ones_b, flag_s[:, 0:1], None, ALU.mult, None, reads=["flag", "ones_b"], writes=["onesf_b"])
    act(scT, cT_s, AF.Silu, reads=["cT"], writes=["scT"])
    cp("dve", scT_b, scT, reads=["scT"], writes=["scT_b"])

    R5 = 144 * K
    wad = [A.view(R5 + i * 8 * K, [128, 8, 512], BF16) for i in range(4)]
    psM = bank(7)
    for blk in range(12):
        wbuf = wad[blk % 4]
        dma("pool", wbuf, w_ada_v[:, :, blk * 512:(blk + 1) * 512], writes=[("wad", blk % 4)])
        for jj in range(4):
            j = blk * 4 + jj
            for kc in range(8):
                mm(psM[:, j:j + 1], wbuf[:, kc, jj * 128:(jj + 1) * 128], scT_b[:, kc:kc + 1], kc == 0, kc == 7,
                   reads=[("wad", blk % 4), "scT_b"], writes=[("ps", 7)])
    tt("dve", modT, psM[:, 0:48], badaT, ALU.add, reads=[("ps", 7), "badaT"], writes=["modT"])
    ts("dve", opsc1, modT[:, 8:16], 1.0, None, ALU.add, None, reads=["modT"], writes=["opsc1"])
    ts("dve", opsc2, modT[:, 32:40], 1.0, None, ALU.add, None, reads=["modT"], writes=["opsc2"])
    dg = [A.view(R5 + 32 * K + i * 512, [128, 128], F32) for i in range(2)]
    for gi, (GB, col0) in enumerate(((G1B, 16), (G2B, 40))):
        for c in range(8):
            d_ = dg[c % 2]
            ts("dve", d_, ident_f, modT[:, col0 + c:col0 + c + 1], None, ALU.mult, None,
               reads=["ident_f", "modT"], writes=[("dg", c % 2)])
            pb = bank(gi * 2 + c // 4)
            mm(pb[:, (c % 4) * 128:(c % 4 + 1) * 128], ones_f, d_, True, True,
               reads=["ones_f", ("dg", c % 2)], writes=[("ps", gi * 2 + c // 4)])
        for hh in range(2):
            cp("dve", GB[:, hh * 512:(hh + 1) * 512], bank(gi * 2 + hh), reads=[("ps", gi * 2 + hh)], writes=[("GB", gi)])
    sh1 = modT[:, 0:8]
    sh2 = modT[:, 24:32]

    R1 = 32 * K
    R2 = 64 * K
    hT = A.view(R2, [128, 8, 4096], BF16)
    R3 = R2 + 64 * K
    uT = A.view(R3, [128, 4, NTOK], BF16)
    R4 = R3 + 16 * K
    ybT = A.view(R4, [128, 4, NTOK], BF16)
    R5 = R4 + 16 * K
    xs = [A.view(R1 + i * 4 * K, [128, 1024], F32) for i in range(8)]
    xn = [A.view(180 * K + i * 2 * K, [128, 1024], BF16) for i in range(4)]

    def x_tile(tt_):
        return xc[tt_ * 128:(tt_ + 1) * 128, :] if tt_ < 16 else xo[(tt_ - 16) * 128:(tt_ - 15) * 128, :]

    evac_flip = [0]

    def evac_mod(out, in_, scale_ap, bias_ap, reads, writes, eng=None):
        if eng is None:
            evac_flip[0] ^= 1
            eng = "act" if evac_flip[0] else "dve"
        if eng == "act":
            act(out, in_, AF.Identity, reads=reads, writes=writes, bias=bias_ap, scale=scale_ap)
        else:
            ts("dve", out, in_, scale_ap, bias_ap, ALU.mult, ALU.add, reads=reads, writes=writes)

    def ph1_A(b):
        slot = b % 2
        xo_ = (b % 2) * 4
        for t in range(4):
            tile = b * 4 + t
            dma("sp", xs[xo_ + t], x_tile(tile), writes=[("xs", xo_ + t)])
            ln_stats(xs[xo_ + t], slot, t, ("xs", xo_ + t))
        ln_finish(slot, 4)

    def ph1_B(b):
        slot = b % 2
        xo_ = (b % 2) * 4
        for hb in range(2):
            b0 = 2 * (hb + 2 * (b % 2))
            pT = psum_t[:, b0 * 512:(b0 + 2) * 512].bitcast(BF16).rearrange("p (a b) -> p a b", a=8)
            pkey = ("ps", b0)
            for t2 in range(2):
                t = hb * 2 + t2
                act(xn[t], xs[xo_ + t], AF.Identity, reads=[("xs", xo_ + t), ("rstd", slot), ("nmr", slot)], writes=[("xn", t)],
                    bias=nmr_[slot][:, t:t + 1], scale=rstd_[slot][:, t:t + 1])
                for kc in range(8):
                    tr(pT[:, kc, t2 * 128:(t2 + 1) * 128], xn[t][:, kc * 128:(kc + 1) * 128], ident_b,
                       reads=[("xn", t), "ident_b"], writes=[pkey, ("ps", b0 + 1)])
            tok0 = b * 512 + hb * 256
            for kc in range(8):
                evac_mod(hT[:, kc, tok0:tok0 + 256], pT[:, kc, :], opsc1[:, kc:kc + 1], sh1[:, kc:kc + 1],
                         reads=[pkey, ("ps", b0 + 1), "opsc1", "modT"], writes=[("hT", b, kc)], eng=("act" if kc < 4 else "dve"))


    ph1_A(0)
    for b in range(8):
        if b + 1 < 8:
            ph1_A(b + 1)
        ph1_B(b)

    if "hT" in dbg:
        dbg_out["hT"] = (hT, [128, 8 * 4096], BF16, [("hT", b, kc) for b in range(8) for kc in range(8)])

    NWR = 7
    wring = [A.view(R5 + i * 2 * K, [128, 8, 128], BF16) for i in range(NWR)]
    wr_n = [0]

    def load_chunk(src_v, col0, ncols=128):
        i = wr_n[0] % NWR
        wr_n[0] += 1
        dma("pool", wring[i][:, :, 0:ncols], src_v[:, :, col0:col0 + ncols], writes=[("wring", i)])
        return wring[i], ("wring", i)

    ps_n = [0]

    def next_bank(lo=0, n=2):
        i = lo + ps_n[0] % n
        ps_n[0] += 1
        return i

    def proj_T(wbuf, wkey, tb, nk=8, src=None, srckeys=None, bank_lo=0, bank_n=2):
        bi = next_bank(bank_lo, bank_n)
        pb = bank(bi)
        for kc in range(nk):
            mm(pb, wbuf[:, kc, :], hT[:, kc, tb * 512:(tb + 1) * 512], kc == 0, kc == nk - 1,
               reads=[wkey, ("hT", tb, kc)], writes=[("ps", bi)])
        return pb, ("ps", bi)

    if stop_after != "hT":
        GM = R1
        gmgB = A.view(GM, [128, 512], F32)
        gmbB = A.view(GM + 2 * K, [128, 512], F32)
        BSp = A.view(GM + 4 * K, [128, 4, 128], F32)
        wsT_f = A.view(GM + 6 * K, [128, 8, 128], F32)
        wsT_b = A.view(GM + 10 * K, [128, 8, 128], BF16)
        vg = [A.view(GM + 12 * K + i * 2 * K, [128, 512], F32) for i in range(2)]
        zz = [A.view(GM + 16 * K + i * 2 * K, [128, 512], F32) for i in range(2)]
        vn = [A.view(GM + 20 * K + i * K, [128, 512], BF16) for i in range(2)]
        tmpg = [A.view(GM + 22 * K + i * 2 * K, [128, 4, 128], F32) for i in range(2)]
        Wvg = A.view(R5 + 14 * K, [128, 8, 512], BF16)
        S.barrier()
        dma("sp", gmgB, bcast(gm_g[0:1, :]), writes=["gmgB"])
        dma("sp", gmbB, bcast(gm_b[0:1, :]), writes=["gmbB"])
        dma("sp", BSp, bsp_d.rearrange("p (a b) -> p a b", a=4), writes=["BSp"])
        dma("sp", wsT_f, wsT_d.rearrange("p (a b) -> p a b", a=8), writes=["wsT_f"])
        S.add("pool", lambda h: h.affine_select(out=wsT_f, in_=wsT_f, pattern=[[0, 8], [1, 128]], compare_op=ALU.is_ge,
                                                 fill=0.0, base=0, channel_multiplier=-1),
              reads=["wsT_f"], writes=["wsT_f"])
        cp("pool", wsT_b, wsT_f, reads=["wsT_f"], writes=["wsT_b"])
        dma("pool", Wvg, w_in_v[:, :, 512:1024], writes=["Wvg"])
        for c in range(4):
            wbuf, wkey = load_chunk(w_in_v, c * 128)
            for tb in range(4, 8):
                pb, pkey = proj_T(wbuf, wkey, tb)
                act(uT[:, c, (tb - 4) * 512:(tb - 3) * 512], pb, AF.Gelu_apprx_tanh, reads=[pkey], writes=[("uT", tb - 4)])
        def gmlpA(ti):
            tile = 16 + ti
            sl = ti % 2
            bi = next_bank(0, 2)
            pb = bank(bi)
            for kc in range(8):
                mm(pb, hT[:, kc, tile * 128:(tile + 1) * 128], Wvg[:, kc, :], kc == 0, kc == 7,
                   reads=[("hT", tile // 4, kc), "Wvg"], writes=[("ps", bi)])
            act(vg[sl], pb, AF.Gelu_apprx_tanh, reads=[("ps", bi)], writes=[("vg", sl)])
            ln_stats(vg[sl], sl, 0, ("vg", sl), nhalf=1)
            ln_finish(sl, 1)
            act(zz[sl], vg[sl], AF.Identity, reads=[("vg", sl), ("rstd", sl), ("nmr", sl)], writes=[("zz", sl)],
                bias=nmr_[sl][:, 0:1], scale=rstd_[sl][:, 0:1])
            tt("dve", zz[sl], zz[sl], gmgB, ALU.mult, reads=[("zz", sl), "gmgB"], writes=[("zz", sl)])
            tt("dve", vn[sl], zz[sl], gmbB, ALU.add, reads=[("zz", sl), "gmbB"], writes=[("vn", sl)])

        def gmlpB(ti):
            sl = ti % 2
            bs_i = 2 + ti % 2
            pS = bank(bs_i, [128, 4, 128])
            for gp in range(4):
                for hh in range(2):
                    g_ = 2 * gp + hh
                    mm(pS[hh * 64:(hh + 1) * 64, gp, :], vn[sl][:, g_ * 64:(g_ + 1) * 64], wsT_b[:, g_, :], True, True,
                       reads=[("vn", sl), "wsT_b"], writes=[("ps", bs_i)])
            tt("dve", tmpg[sl], pS, BSp, ALU.add, reads=[("ps", bs_i), "BSp"], writes=[("tmpg", sl)])
            tt("dve", uT[:, :, ti * 128:(ti + 1) * 128], tmpg[sl], uT[:, :, ti * 128:(ti + 1) * 128], ALU.mult,
               reads=[("tmpg", sl), ("uT", ti // 4)], writes=[("uT", ti // 4)])

        gmlpA(0)
        for ti in range(16):
            if ti + 1 < 16:
                S.zipped(lambda: gmlpA(ti + 1), lambda: gmlpB(ti))
            else:
                gmlpB(ti)
        if "yaT" in dbg:
            dbg_out["yaT"] = (uT, [128, 4 * NTOK], BF16, [("uT", i) for i in range(4)])

    if stop_after not in ("hT", "gmlp"):
        AT = R1
        accND = A.view(AT, [128, 2, NTOK], F32)
        sbS = [A.view(AT + 16 * K + i * K, [128, 256], F32) for i in range(4)]
        ptS = [A.view(AT + 20 * K + i * 512, [128, 256], BF16) for i in range(4)]
        braw = [A.view(AT + 22 * K + i * 2 * K, [128, 2, 256], F32) for i in range(2)]
        biasm = [A.view(AT + 26 * K + i * 2 * K, [128, 2, 256], F32) for i in range(2)]
        QKV0 = R5 + 14 * K
        QT = [A.view(QKV0, [128, NTOK], BF16), A.view(QKV0 + 20 * K, [128, NTOK], BF16)]
        KT = [A.view(QKV0 + 4 * K, [128, 4096], BF16)] * 2
        VV = [A.view(QKV0 + 12 * K, [128, 32, 128], BF16), A.view(QKV0 + 24 * K, [128, 32, 128], BF16)]
        assert QKV0 + 32 * K <= ARENA_BYTES
        S.barrier()
        ztile = A.view(MOE_TMP, [128, 1024], BF16)
        memset("pool", ztile, 0.0, writes=["ztile"])
        ZR = 1024
        zlist = list(range(0, NSLOT, ZR))

        def zero_fill(cnt):
            for _ in range(cnt):
                if not zlist:
                    return
                z0 = zlist.pop(0)
                zn_ = min(ZR, NSLOT - z0)
                dma("sp", xs_scr[z0:z0 + zn_, :].rearrange("(t p) d -> p t d", p=128),
                    ztile.unsqueeze(1).broadcast_to([128, zn_ // 128, 1024]), reads=["ztile"], writes=[("xs_zero", z0)])
        unit_n = 0
        for j in range(4):
            for g in range(3):
                r = DIL[g]
                nb_rho = 16 // r
                sl = unit_n % 2
                unit_n += 1
                kq = ("QT", sl)
                kk = ("KT", 0)
                col = 1024 + g * 1536 + j * 128
                wq, wqk = load_chunk(w_in_v, col)
                wk, wkk = load_chunk(w_in_v, col + 512)
                wv, wvk = load_chunk(w_in_v, col + 1024)
                dma("sp", braw[sl], biasT_d.rearrange("p (a b) -> p a b", a=24)[:, g * 8 + 2 * j:g * 8 + 2 * j + 2, :],
                    writes=[("braw", sl)])
                zero_fill(3)
                for hh in range(2):
                    tt("pool", biasm[sl][:, hh, :], braw[sl][:, hh, :], maskT, ALU.add,
                       reads=[("braw", sl), "maskT"], writes=[("biasm", sl)])
                for tb in range(4, 8):
                    pb, pkey = proj_T(wq, wqk, tb)
                    act(QT[sl][:, (tb - 4) * 512:(tb - 3) * 512], pb, AF.Identity, reads=[pkey], writes=[kq], scale=0.125)
                tb_lo = 0 if g == 2 else 3
                for tb in range(tb_lo, 8):
                    pb, pkey = proj_T(wk, wkk, tb)
                    cp("dve", KT[sl][:, tb * 512:(tb + 1) * 512], pb, reads=[pkey], writes=[kk])
                for rho in range(r):
                    for ib in range(2 * nb_rho):
                        ctx = ib < nb_rho
                        if ctx and g < 2 and ib != nb_rho - 1:
                            continue
                        vt = (0 if ctx else 16) + rho * nb_rho + ib % nb_rho
                        bi = 2 + (vt % 2)
                        pv = bank(bi)[:, 0:128]
                        start = rho + r * 128 * ib
                        for kc in range(8):
                            mm(pv, hT[:, kc, start:start + 127 * r + 1:r], wv[:, kc, :], kc == 0, kc == 7,
                               reads=[("hT", b_, kc) for b_ in range(start // 512, (start + 128 * r - 1) // 512 + 1)] + [wvk],
                               writes=[("ps", bi)])
                        if ctx:
                            act(VV[sl][:, vt, :], pv, AF.Identity, reads=[("ps", bi), "flag"], writes=[("V", sl)], scale=flag_s[:, 0:1])
                        else:
                            cp("dve", VV[sl][:, vt, :], pv, reads=[("ps", bi)], writes=[("V", sl)])
                def unit_geom(qb):
                    rho = qb // nb_rho
                    ib_cur = nb_rho + qb % nb_rho
                    qcol0 = rho + r * 128 * ib_cur - NTOK
                    q_sl = slice(qcol0, qcol0 + 127 * r + 1, r)
                    ktile = []
                    for ib in (ib_cur - 1, ib_cur):
                        ctx = ib < nb_rho
                        vt = (0 if ctx else 16) + rho * nb_rho + ib % nb_rho
                        kst = rho + r * 128 * ib
                        ktile.append((slice(kst, kst + 127 * r + 1, r), vt, ctx))
                    return q_sl, ktile

                def unit_S(qb):
                    q_sl, ktile = unit_geom(qb)
                    u = qb % 2
                    for hh in range(2):
                        bS = 2 * hh + u
                        pS = bank(bS)[:, 0:256]
                        pk = ("ps", bS)
                        p0, p1 = hh * 64, hh * 64 + 64
                        for kb in range(2):
                            mm(pS[:, kb * 128:(kb + 1) * 128], KT[sl][p0:p1, ktile[kb][0]], QT[sl][p0:p1, q_sl], True, True,
                               reads=[kk, kq], writes=[pk])
                        si = u * 2 + hh
                        stt(sbS[si], pS, 60.0, biasm[sl][:, hh, :], ALU.min, ALU.add,
                            reads=[pk, ("biasm", sl)], writes=[("sbS", si)])
                        act(ptS[si], sbS[si], AF.Exp, reads=[("sbS", si)], writes=[("ptS", si)])

                def unit_PV(qb):
                    q_sl, ktile = unit_geom(qb)
                    u = qb % 2
                    pND = bank(4 + u)[:, 0:256].rearrange("p (a b) -> p a b", a=2)
                    pkN = ("ps", 4 + u)
                    for which in range(2):
                        for hh in range(2):
                            si = u * 2 + hh
                            p0, p1 = hh * 64, hh * 64 + 64
                            for kb in range(2):
                                _, vt, ctx = ktile[kb]
                                if which == 0:
                                    lhs = VV[sl][:, vt, p0:p1]
                                    rk = [("V", sl)]
                                else:
                                    lhs = onesf_b if ctx else ones_b
                                    rk = ["onesf_b", "ones_b"]
                                mm(pND[p0:p1, which, :], lhs, ptS[si][:, kb * 128:(kb + 1) * 128], kb == 0, kb == 1,
                                   reads=rk + [("ptS", si)], writes=[pkN])
                    dst = accND[:, :, q_sl]
                    if g == 0:
                        cp("dve", dst, pND, reads=[pkN], writes=["accND"])
                    else:
                        tt("dve", dst, pND, dst, ALU.add, reads=[pkN, "accND"], writes=["accND"])

                if PIPE_ATTN:
                    unit_S(0)
                    for qb in range(16):
                        if qb + 1 < 16:
                            unit_S(qb + 1)
                        unit_PV(qb)
                else:
                    for qb in range(16):
                        unit_S(qb)
                        unit_PV(qb)
            S.add("dve", lambda h: h.reciprocal(out=accND[:, 1, :], in_=accND[:, 1, :]), reads=["accND"], writes=["accND"])
            tt("dve", ybT[:, j, :], accND[:, 0, :], accND[:, 1, :], ALU.mult, reads=["accND"], writes=[("ybT", j)])
        if "ybT" in dbg:
            dbg_out["ybT"] = (ybT, [128, 4 * NTOK], BF16, [("ybT", i) for i in range(4)])

    if stop_after not in ("hT", "gmlp", "attn"):
        S.barrier()
        mT = A.view(R1, [128, 8, NTOK], BF16)
        Wa = A.view(R5 + 14 * K, [128, 4, 1024], BF16)
        Wb = A.view(R5 + 22 * K, [128, 4, 1024], BF16)
        gab = [A.view(R5 + 30 * K + i * 2 * K, [128, 512], F32) for i in range(4)]
        t12 = [A.view(R5 + 38 * K + i * 2 * K, [128, 512], F32) for i in range(4)]
        dma("pool", Wa, wa_d.rearrange("(kc p) n -> p kc n", p=128), writes=["Wa"])
        dma("pool", Wb, wb_d.rearrange("(kc p) n -> p kc n", p=128), writes=["Wb"])
        it = 0
        for m in range(8):
            wga, wgak = load_chunk(w_in_v, 5632 + m * 128)
            wgb, wgbk = load_chunk(w_in_v, 6656 + m * 128)
            for tb in range(4):
                s2 = it % 2
                it += 1
                pa = bank(0 + s2 * 4)
                pbb = bank(1 + s2 * 4)
                for kc in range(4):
                    mm(pa, Wa[:, kc, m * 128:(m + 1) * 128], uT[:, kc, tb * 512:(tb + 1) * 512], kc == 0, kc == 3,
                       reads=["Wa", ("uT", tb)], writes=[("ps", 0 + s2 * 4)])
                for kc in range(4):
                    mm(pbb, Wb[:, kc, m * 128:(m + 1) * 128], ybT[:, kc, tb * 512:(tb + 1) * 512], kc == 0, kc == 3,
                       reads=["Wb"] + [("ybT", jj) for jj in range(4)], writes=[("ps", 1 + s2 * 4)])
                pga, pgak = proj_T(wga, wgak, 4 + tb, bank_lo=2 + s2 * 4, bank_n=1)
                pgb, pgbk = proj_T(wgb, wgbk, 4 + tb, bank_lo=3 + s2 * 4, bank_n=1)
                act(gab[s2 * 2], pga, AF.Sigmoid, reads=[pgak], writes=[("gab", s2 * 2)])
                act(gab[s2 * 2 + 1], pgb, AF.Sigmoid, reads=[pgbk], writes=[("gab", s2 * 2 + 1)])
                tt("dve", t12[s2 * 2], pa, gab[s2 * 2], ALU.mult, reads=[("ps", 0 + s2 * 4), ("gab", s2 * 2)], writes=[("t12", s2 * 2)])
                tt("dve", t12[s2 * 2 + 1], pbb, gab[s2 * 2 + 1], ALU.mult, reads=[("ps", 1 + s2 * 4), ("gab", s2 * 2 + 1)], writes=[("t12", s2 * 2 + 1)])
                tt("pool", mT[:, m, tb * 512:(tb + 1) * 512], t12[s2 * 2], t12[s2 * 2 + 1], ALU.add,
                   reads=[("t12", s2 * 2), ("t12", s2 * 2 + 1)], writes=[("mT", tb)])
        if "mT" in dbg:
            dbg_out["mT"] = (mT, [128, 8 * NTOK], BF16, [("mT", i) for i in range(4)])

        S.barrier()
        H2 = R2
        xnb = A.view(H2, [128, 16, 1024], BF16)
        O5 = H2 + 32 * K
        Wo = A.view(O5, [128, 8, 1024], BF16)
        ln1gB = A.view(O5 + 16 * K, [128, 1024], F32)
        ln1bB = A.view(O5 + 20 * K, [128, 1024], F32)
        xs5 = [A.view(O5 + 24 * K + i * 4 * K, [128, 1024], F32) for i in range(2)]
        zt = [A.view(O5 + 32 * K + i * 4 * K, [128, 1024], F32) for i in range(2)]
        x1t = [A.view(O5 + 40 * K + i * 4 * K, [128, 1024], F32) for i in range(2)]
        wr_s = A.view(MOE_TMP, [128, 8, NEXP], F32)
        h2Tf = A.view(MOE_TMP + 6 * K, [128, 8, 128], F32)
        assert MOE_TMP + 10 * K + 512 <= R0_END
        dma("pool", Wo, wo_d.rearrange("(kc p) n -> p kc n", p=128), writes=["Wo"])
        dma("sp", ln1gB, bcast(ln1g_d[0:1, :]), writes=["ln1gB"])
        dma("sp", ln1bB, bcast(ln1b_d[0:1, :]), writes=["ln1bB"])
        dma("sp", wr_s, wr_d.rearrange("(kc p) n -> p kc n", p=128), writes=["wr_s"])
        dma("sp", brB, bcast(br_d[0:1, :]), writes=["brB"])
        def stage5A(ti):
            s2 = ti % 2
            dma("sp", xs5[s2], xo[ti * 128:(ti + 1) * 128, :], writes=[("xs5", s2)])
            for hh in range(2):
                bi = hh + 2 * s2
                pb = bank(bi)
                for kc in range(8):
                    mm(pb, mT[:, kc, ti * 128:(ti + 1) * 128], Wo[:, kc, hh * 512:(hh + 1) * 512], kc == 0, kc == 7,
                       reads=[("mT", ti // 4), "Wo"], writes=[("ps", bi)])
                tt("dve", zt[s2][:, hh * 512:(hh + 1) * 512], pb, G1B[:, hh * 512:(hh + 1) * 512], ALU.mult,
                   reads=[("ps", bi), ("GB", 0)], writes=[("zt", s2)])
            stt(zt[s2], xs5[s2], ALPHA, zt[s2], ALU.mult, ALU.add, reads=[("xs5", s2), ("zt", s2)], writes=[("zt", s2)])
            ln_stats(zt[s2], s2, 0, ("zt", s2))
            ln_finish(s2, 1)
            act(zt[s2], zt[s2], AF.Identity, reads=[("zt", s2), ("rstd", s2), ("nmr", s2)], writes=[("zt", s2)],
                bias=nmr_[s2][:, 0:1], scale=rstd_[s2][:, 0:1])
            tt("dve", zt[s2], zt[s2], ln1gB, ALU.mult, reads=[("zt", s2), "ln1gB"], writes=[("zt", s2)])
            tt("dve", x1t[s2], zt[s2], ln1bB, ALU.add, reads=[("zt", s2), "ln1bB"], writes=[("x1t", s2)])
            dma("sp", x1s_d[ti * 128:(ti + 1) * 128, :], x1t[s2], reads=[("x1t", s2)], writes=[("x1s", ti)])
            ln_stats(x1t[s2], s2, 1, ("x1t", s2))
            act(sd_[s2][:, 1:2], mv_[s2][:, 1:2, 1], AF.Ln, reads=[("mv", s2, 1)], writes=[("sd2", s2)], bias=eps_t, scale=1.0)
            act(rstd_[s2][:, 1:2], sd_[s2][:, 1:2], AF.Exp, reads=[("sd2", s2)], writes=[("rstd2", s2)], scale=-0.5)
            stt(nmr_[s2][:, 1:2], mv_[s2][:, 1:2, 0], -1.0, rstd_[s2][:, 1:2], ALU.mult, ALU.mult,
                reads=[("mv", s2, 1), ("rstd2", s2)], writes=[("nmr2", s2)])
            act(zt[s2], x1t[s2], AF.Identity, reads=[("x1t", s2), ("rstd2", s2), ("nmr2", s2)], writes=[("zt", s2)],
                bias=nmr_[s2][:, 1:2], scale=rstd_[s2][:, 1:2])
            cp("act", xnb[:, ti, :], zt[s2], reads=[("zt", s2)], writes=[("xnb", ti)])

        def stage5B(ti):
            s2 = ti % 2
            for hb in range(2):
                bi = 4 + hb
                pT = bank(bi, [128, 4, 128])
                for k4 in range(4):
                    kc = hb * 4 + k4
                    tr(pT[:, k4, :], zt[s2][:, kc * 128:(kc + 1) * 128], ident_f, reads=[("zt", s2), "ident_f"], writes=[("ps", bi)])
                for k4 in range(4):
                    kc = hb * 4 + k4
                    evac_mod(h2Tf[:, kc, :], pT[:, k4, :], opsc2[:, kc:kc + 1], sh2[:, kc:kc + 1],
                             reads=[("ps", bi), "opsc2", "modT"], writes=[("h2Tf", kc)], eng=("act" if hb == 0 else "dve"))
            pR = bank(6)[:, 0:NEXP]
            for kc in range(8):
                mm(pR, h2Tf[:, kc, :], wr_s[:, kc, :], kc == 0, kc == 7, reads=[("h2Tf", kc), "wr_s"], writes=[("ps", 6)])
            tt("dve", lg_[s2], pR, brB, ALU.add, reads=[("ps", 6), "brB"], writes=[("lg", s2)])
            S.add("dve", lambda h, s2=s2: h.max(out=mx8_[s2], in_=lg_[s2]), reads=[("lg", s2)], writes=[("mx8", s2)])
            ts("dve", msk_[s2], lg_[s2], mx8_[s2][:, 3:4], None, ALU.is_ge, None, reads=[("lg", s2), ("mx8", s2)], writes=[("msk", s2)])
            ts("dve", negm_[s2], mx8_[s2][:, 0:1], -1.0, None, ALU.mult, None, reads=[("mx8", s2)], writes=[("negm", s2)])
            act(ex_[s2], lg_[s2], AF.Exp, reads=[("lg", s2), ("negm", s2)], writes=[("ex", s2)], bias=negm_[s2][:, 0:1], scale=1.0)
            tt("dve", em_[s2], ex_[s2], msk_[s2], ALU.mult, reads=[("ex", s2), ("msk", s2)], writes=[("em", s2)])
            S.add("dve", lambda h, s2=s2: h.reduce_sum(out=den_[s2], in_=em_[s2], axis=AX.X), reads=[("em", s2)], writes=[("den", s2)])
            S.add("dve", lambda h, s2=s2: h.reciprocal(out=rden_[s2], in_=den_[s2]), reads=[("den", s2)], writes=[("rden", s2)])
            ts("dve", gates_all[:, ti, :], em_[s2], rden_[s2][:, 0:1], None, ALU.mult, None, reads=[("em", s2), ("rden", s2)], writes=[("gates", ti)])
            S.add("dve", lambda h, s2=s2: h.max_index(out=mi8_[s2], in_max=mx8_[s2], in_values=lg_[s2]), reads=[("lg", s2), ("mx8", s2)], writes=[("mi8", s2)])
            cp("dve", E4f[:, ti, :], mi8_[s2][:, 0:4], reads=[("mi8", s2)], writes=[("E4f", ti)])
            act(e4_[s2], mx8_[s2][:, 0:4], AF.Exp, reads=[("mx8", s2), ("negm", s2)], writes=[("e4", s2)], bias=negm_[s2][:, 0:1], scale=1.0)
            ts("dve", W4[:, ti, :], e4_[s2], rden_[s2][:, 0:1], None, ALU.mult, None, reads=[("e4", s2), ("rden", s2)], writes=[("W4", ti)])
            cp("pool", Mall[:, ti, :], msk_[s2], reads=[("msk", s2)], writes=[("Mall", ti)])

        stage5A(0)
        for ti in range(16):
            if ti + 1 < 16:
                S.zipped(lambda: stage5A(ti + 1), lambda: stage5B(ti))
            else:
                stage5B(ti)
        if "x1" in dbg:
            dbg_out["x1"] = None
        if "gates" in dbg:
            dbg_out["gates"] = (gates_all, [128, 16 * NEXP], F32, [("gates", i) for i in range(16)])

    if stop_after in (None, "moe", "route"):
        S.barrier()
        RT0 = R5
        within = A.view(RT0, [128, 16, NEXP], F32)
        tot = A.view(RT0 + 2 * K, [128, 16, NEXP], F32)
        base = A.view(RT0 + 4 * K, [128, 16, NEXP], F32)
        dall = A.view(RT0 + 6 * K, [128, 16, NEXP], F32)
        big1 = A.view(RT0 + 8 * K, [128, NEXP, NEXP], F32)
        big2 = A.view(RT0 + 12 * K, [128, NEXP, NEXP], F32)
        LTm = A.view(RT0 + 16 * K, [128, NEXP, NEXP], F32)
        oh = A.view(RT0 + 20 * K, [128, 16, NEXP], F32)
        U_f = A.view(RT0 + 22 * K, [128, 128], F32)
        U_b = A.view(RT0 + 22 * K + 512, [128, 128], BF16)
        on_b = A.view(RT0 + 22 * K + 768, [128, 128], BF16)
        iotaE = A.view(RT0 + 23 * K, [128, NEXP], F32)
        cnt = A.view(RT0 + 23 * K + 128, [128, NEXP], F32)
        rank = A.view(RT0 + 23 * K + 256, [128, NEXP], F32)
        kbv = A.view(RT0 + 23 * K + 384, [128, NEXP], F32)
        sig = A.view(RT0 + 23 * K + 512, [128, NEXP], F32)
        kbtab = A.view(RT0 + 23 * K + 640, [128, NEXP], F32)
        pk = A.view(RT0 + 23 * K + 768, [128, 8], F32)
        destF = A.view(RT0 + 24 * K, [128, 4, 16], F32)
        idxWf = A.view(RT0 + 24 * K + 256, [128, NEXP, 8], F32)
        rankT = A.view(RT0 + 26 * K, [NEXP, 1], F32, parts=NEXP)
        RTm = A.view(RT0 + 26 * K + 64, [NEXP, NEXP], F32, parts=NEXP)
        bgrow = A.view(RT0 + 27 * K, [NEXP, 1024], F32, parts=NEXP)
        burow = A.view(RT0 + 31 * K, [NEXP, 1024], F32, parts=NEXP)
        dma("sp", kbtab, kbtab_d, writes=["kbtab"])
        dma("sp", bgrow, bg_d, writes=["bgrow"])
        dma("sp", burow, bu_d, writes=["burow"])
        memset("pool", U_f, 1.0, writes=["U_f"])
        S.add("pool", lambda h: h.affine_select(out=U_f, in_=U_f, pattern=[[1, 128]], compare_op=ALU.is_gt, fill=0.0, base=0, channel_multiplier=-1),
              reads=["U_f"], writes=["U_f"])
        cp("pool", U_b, U_f, reads=["U_f"], writes=["U_b"])
        memset("pool", on_b, 1.0, writes=["on_b"])
        S.add("pool", lambda h: h.iota(iotaE, pattern=[[1, NEXP]], base=0, channel_multiplier=0, allow_small_or_imprecise_dtypes=True), writes=["iotaE"])
        S.add("pool", lambda h: h.iota(pk, pattern=[[128, 8]], base=0, channel_multiplier=1, allow_small_or_imprecise_dtypes=True), writes=["pk"])
        memset("pool", LTm, 1.0, writes=["LTm"])
        S.add("pool", lambda h: h.affine_select(out=LTm, in_=LTm, pattern=[[1, NEXP], [-1, NEXP]], compare_op=ALU.is_gt, fill=0.0, base=0, channel_multiplier=0),
              reads=["LTm"], writes=["LTm"])
        Mkeys = [("Mall", i) for i in range(16)]
        Mflat = Mall.rearrange("p a b -> p (a b)")
        mm(bank(0), U_b, Mflat, True, True, reads=["U_b"] + Mkeys, writes=[("ps", 0)])
        mm(bank(1), on_b, Mflat, True, True, reads=["on_b"] + Mkeys, writes=[("ps", 1)])
        cp("dve", within.rearrange("p a b -> p (a b)"), bank(0), reads=[("ps", 0)], writes=["within"])
        cp("act", tot.rearrange("p a b -> p (a b)"), bank(1), reads=[("ps", 1)], writes=["tot"])
        memset("dve", base[:, 0, :], 0.0, writes=["base"])
        for ti in range(1, 16):
            tt("dve", base[:, ti, :], base[:, ti - 1, :], tot[:, ti - 1, :], ALU.add, reads=["base", "tot"], writes=["base"])
        tt("dve", cnt, base[:, 15, :], tot[:, 15, :], ALU.add, reads=["base", "tot"], writes=["cnt"])
        cA = cnt.unsqueeze(1).broadcast_to([128, NEXP, NEXP])
        cB = cnt.unsqueeze(2).broadcast_to([128, NEXP, NEXP])
        tt("dve", big1, cA, cB, ALU.is_gt, reads=["cnt"], writes=["big1"])
        tt("dve", big2, cA, cB, ALU.is_equal, reads=["cnt"], writes=["big2"])
        tt("dve", big2, big2, LTm, ALU.mult, reads=["big2", "LTm"], writes=["big2"])
        tt("dve", big1, big1, big2, ALU.add, reads=["big1", "big2"], writes=["big1"])
        S.add("dve", lambda h: h.reduce_sum(out=rank, in_=big1, axis=AX.X), reads=["big1"], writes=["rank"])
        rA = rank.unsqueeze(2).broadcast_to([128, NEXP, NEXP])
        iB = iotaE.unsqueeze(1).broadcast_to([128, NEXP, NEXP])
        tt("dve", big1, rA, iB, ALU.is_equal, reads=["rank", "iotaE"], writes=["big1"])
        tt("dve", big2, big1, kbtab.unsqueeze(1).broadcast_to([128, NEXP, NEXP]), ALU.mult, reads=["big1", "kbtab"], writes=["big2"])
        S.add("dve", lambda h: h.reduce_sum(out=kbv, in_=big2, axis=AX.X), reads=["big2"], writes=["kbv"])
        tt("dve", big1, rank.unsqueeze(1).broadcast_to([128, NEXP, NEXP]), iotaE.unsqueeze(2).broadcast_to([128, NEXP, NEXP]), ALU.is_equal,
           reads=["rank", "iotaE"], writes=["big1"])
        tt("dve", big2, big1, iB, ALU.mult, reads=["big1", "iotaE"], writes=["big2"])
        S.add("dve", lambda h: h.reduce_sum(out=sig, in_=big2, axis=AX.X), reads=["big2"], writes=["sig"])
        tt("dve", dall, base, within, ALU.add, reads=["base", "within"], writes=["dall"])
        tt("dve", dall, dall, kbv.unsqueeze(1).broadcast_to([128, 16, NEXP]), ALU.add, reads=["dall", "kbv"], writes=["dall"])
        E4keys = [("E4f", i) for i in range(16)]
        for k in range(4):
            tt("dve", oh, iotaE.unsqueeze(1).broadcast_to([128, 16, NEXP]), E4f[:, :, k:k + 1].broadcast_to([128, 16, NEXP]), ALU.is_equal,
               reads=["iotaE"] + E4keys, writes=["oh"])
            tt("dve", oh, oh, dall, ALU.mult, reads=["oh", "dall"], writes=["oh"])
            S.add("dve", lambda h, k=k: h.reduce_sum(out=destF[:, k, :], in_=oh, axis=AX.X), reads=["oh"], writes=["destF"])
        cp("dve", destI, destF, reads=["destF"], writes=["destI"])
        ts("dve", sig, sig, 1024.0, None, ALU.mult, None, reads=["sig"], writes=["sig"])
        tt("dve", idxWf, sig.unsqueeze(2).broadcast_to([128, NEXP, 8]), pk.unsqueeze(1).broadcast_to([128, NEXP, 8]), ALU.add,
           reads=["sig", "pk"], writes=["idxWf"])
        cp("dve", idxW, idxWf, reads=["idxWf"], writes=["idxW"])
        pRk = bank(2)[0:NEXP, 0:128]
        tr(pRk, rank, ident_f, reads=["rank", "ident_f"], writes=[("ps", 2)])
        cp("act", rankT, pRk[:, 0:1], reads=[("ps", 2)], writes=["rankT"])
        ts("dve", RTm, iotaE[0:NEXP, :], rankT[:, 0:1], None, ALU.is_equal, None, reads=["iotaE", "rankT"], writes=["RTm"])
        for bi_, (rows, rkey, dstR, dkey) in enumerate(((bgrow, "bgrow", bgR, "bgR"), (burow, "burow", bu1R, "bu1R"))):
            pbk = bank(3 + bi_)
            for c in range(8):
                mm(pbk[:, c * NEXP:(c + 1) * NEXP], rows[:, c * 128:(c + 1) * 128], RTm, True, True, reads=[rkey, "RTm"], writes=[("ps", 3 + bi_)])
            cp("act", dstR.rearrange("p a b -> p (a b)"), pbk[:, 0:8 * NEXP], reads=[("ps", 3 + bi_)], writes=[dkey])
        ts("pool", bu1R, bu1R, 1.0, None, ALU.add, None, reads=["bu1R"], writes=["bu1R"])
        if "route" in dbg:
            dbg_out["gates"] = (gates_all, [128, 16 * NEXP], F32, [("gates", i) for i in range(16)])
            dbg_out["destI"] = (destI, [128, 64], I32, ["destI"])
            dbg_out["idxW"] = (idxW, [128, NEXP * 8], I32, ["idxW"])
            dbg_out["rank"] = (rank, [128, NEXP], F32, ["rank"])
            dbg_out["W4"] = (W4, [128, 64], F32, [("W4", i) for i in range(16)])
            dbg_out["bgR"] = (bgR, [128, 8 * NEXP], F32, ["bgR"])

    if stop_after is None or stop_after == "moe":
        NWB = 6
        wbufs = [A.view(R2 + i * 16 * K, [128, 8, 1024], BF16) for i in range(NWB)]
        tmp_g = [A.view(MOE_TMP + i * 2 * K, [128, 512], F32) for i in range(2)]
        tmp_s = [A.view(MOE_TMP + 4 * K + i * 2 * K, [128, 512], F32) for i in range(2)]
        tmp_u = [A.view(MOE_TMP + 8 * K + i * 2 * K, [128, 512], F32) for i in range(2)]
        assert MOE_TMP + 12 * K <= R0_END, (MOE_TMP, R0_END)
        wn = [2]

        def load_w(src, r):
            i = wn[0] % NWB
            wn[0] += 1
            rows = src.rearrange("e k n -> (e k) n")
            for kc in range(8):
                S.add("pool", lambda h, i=i, kc=kc: h.indirect_dma_start(
                    out=wbufs[i][:, kc, :], out_offset=None, in_=rows[:, :],
                    in_offset=bass.IndirectOffsetOnAxis(ap=idxW[:, r, kc:kc + 1], axis=0)),
                    reads=["idxW"], writes=[("wb", i, kc)], dma=True)
            return wbufs[i], i

        def load_rank(r):
            return (load_w(wg_d, r), load_w(wu_d, r), load_w(wd_d, r))

        W_rank0 = load_rank(0)
        for ti in range(16):
            for k in range(4):
                S.add("pool", lambda h, ti=ti, k=k: h.indirect_dma_start(
                    out=xs_scr[:, :], out_offset=bass.IndirectOffsetOnAxis(ap=destI[:, k, ti:ti + 1], axis=0),
                    in_=xnb[:, ti, :], in_offset=None), reads=["destI", ("xnb", ti)], writes=[("xs_scr", ti, k)], dma=True)
        S.barrier()
        xrow = [A.view(R5 + i * 8 * K, [128, 4, 1024], BF16) for i in range(2)]
        yrow = [A.view(R5 + 16 * K + i * 4 * K, [128, 1024], F32) for i in range(2)]
        xeT = [A.view(R1 + i * 8 * K, [128, 8, 512], BF16) for i in range(2)] + [A.view(R5 + 24 * K, [128, 8, 512], BF16)]
        actT = [A.view(R1 + 16 * K + i * 8 * K, [128, 8, 512], BF16) for i in range(2)]
        itc = [0]
        ytile_n = [0]

        def blk_load(blk):
            r, sb_, bidx, W = blk
            n = min(512, KCAP[r] - sb_ * 512)
            nt = n // 128
            row0 = KBASE[r] + sb_ * 512
            xr = bidx % 2
            dma("sp", xrow[xr][:, 0:nt, :], xs_scr[row0:row0 + n, :].rearrange("(t p) d -> p t d", p=128),
                reads=["xs_scr"], writes=[("xrow", xr)])

        def blk_T(blk):
            r, sb_, bidx, W = blk
            n = min(512, KCAP[r] - sb_ * 512)
            nt = n // 128
            xr, x3 = bidx % 2, bidx % 3
            for kg in range(2):
                pT = psum_t[:, 4 * 512:6 * 512].bitcast(BF16).rearrange("p (a b) -> p a b", a=4)
                for t in range(nt):
                    for k4 in range(4):
                        kc = kg * 4 + k4
                        tr(pT[:, k4, t * 128:(t + 1) * 128], xrow[xr][:, t, kc * 128:(kc + 1) * 128], ident_b,
                           reads=[("xrow", xr), "ident_b"], writes=[("ps", 4), ("ps", 5)])
                for k4 in range(4):
                    kc = kg * 4 + k4
                    evac_mod(xeT[x3][:, kc, 0:n], pT[:, k4, 0:n], opsc2[:, kc:kc + 1], sh2[:, kc:kc + 1],
                             reads=[("ps", 4), ("ps", 5), "opsc2", "modT"], writes=[("xeT", x3, kc)], eng=("act" if k4 < 2 else "dve"))

        def blk_GU(blk):
            r, sb_, bidx, W = blk
            (Wg_, Wgk), (Wu_, Wuk), (Wd_, Wdk) = W
            n = min(512, KCAP[r] - sb_ * 512)
            x3, xs2 = bidx % 3, bidx % 2
            for c in range(8):
                s2 = itc[0] % 2
                itc[0] += 1
                bg_i, bu_i = 0 + s2, 2 + s2
                pg, pu = bank(bg_i)[:, 0:n], bank(bu_i)[:, 0:n]
                for kc in range(8):
                    mm(pg, Wg_[:, kc, c * 128:(c + 1) * 128], xeT[x3][:, kc, 0:n], kc == 0, kc == 7,
                       reads=[("wb", Wgk, kc), ("xeT", x3, kc)], writes=[("ps", bg_i)])
                for kc in range(8):
                    mm(pu, Wu_[:, kc, c * 128:(c + 1) * 128], xeT[x3][:, kc, 0:n], kc == 0, kc == 7,
                       reads=[("wb", Wuk, kc), ("xeT", x3, kc)], writes=[("ps", bu_i)])
                tg, tsg, tu = tmp_g[s2][:, 0:n], tmp_s[s2][:, 0:n], tmp_u[s2][:, 0:n]
                ts("dve", tg, pg, bgR[:, c, r:r + 1], 7.0, ALU.add, ALU.min, reads=[("ps", bg_i), "bgR"], writes=[("tg", s2)])
                act(tsg, tg, AF.Sigmoid, reads=[("tg", s2)], writes=[("tsg", s2)], scale=1.702)
                act(tu, pu, AF.Identity, reads=[("ps", bu_i), "bu1R"], writes=[("tu", s2)], bias=bu1R[:, c, r:r + 1], scale=1.0)
                ts("dve", tu, tu, 8.0, -6.0, ALU.min, ALU.max, reads=[("tu", s2)], writes=[("tu", s2)])
                tt("dve", tsg, tg, tsg, ALU.mult, reads=[("tg", s2), ("tsg", s2)], writes=[("tsg", s2)])
                tt("dve", actT[xs2][:, c, 0:n], tsg, tu, ALU.mult, reads=[("tsg", s2), ("tu", s2)], writes=[("actT", xs2)])

        def blk_back(blk):
            r, sb_, bidx, W = blk
            xs2 = bidx % 2
            (Wg_, Wgk), (Wu_, Wuk), (Wd_, Wdk) = W
            n = min(512, KCAP[r] - sb_ * 512)
            nt = n // 128
            row0 = KBASE[r] + sb_ * 512
            for t in range(nt):
                ys2 = ytile_n[0] % 2
                ytile_n[0] += 1
                for hh in range(2):
                    bi = 6 + hh
                    py = bank(bi)
                    for c in range(8):
                        mm(py, actT[xs2][:, c, t * 128:(t + 1) * 128], Wd_[:, c, hh * 512:(hh + 1) * 512], c == 0, c == 7,
                           reads=[("actT", xs2), ("wb", Wdk, c)], writes=[("ps", bi)])
                    cp("act" if hh == 0 else "dve", yrow[ys2][:, hh * 512:(hh + 1) * 512], py, reads=[("ps", bi)], writes=[("yrow", ys2, hh)])
                dma("sp", ys_scr[row0 + t * 128:row0 + (t + 1) * 128, :], yrow[ys2], reads=[("yrow", ys2, 0), ("yrow", ys2, 1)], writes=[("ys_scr", ytile_n[0])])

        blist = []
        for r in range(n_experts):
            for sb_ in range((KCAP[r] + 511) // 512):
                blist.append([r, sb_, len(blist), None])
        NBLK = len(blist)
        Wsets = {0: W_rank0}

        def attach_w(bi_):
            r = blist[bi_][0]
            blist[bi_][3] = Wsets[r]

        blk_load(blist[0])
        if NBLK > 1:
            blk_load(blist[1])
        blk_T(blist[0])
        if NBLK > 2:
            blk_load(blist[2])
        if NBLK > 1:
            blk_T(blist[1])
        attach_w(0)
        blk_GU(blist[0])
        if 1 < n_experts:
            Wsets[1] = load_rank(1)
        for bi_ in range(NBLK):
            if bi_ + 3 < NBLK:
                blk_load(blist[bi_ + 3])
            if bi_ + 2 < NBLK:
                blk_T(blist[bi_ + 2])
            if bi_ + 1 < NBLK:
                attach_w(bi_ + 1)
                blk_GU(blist[bi_ + 1])
            blk_back(blist[bi_])
            if bi_ + 1 < NBLK and blist[bi_ + 1][1] == 0:
                r1 = blist[bi_ + 1][0]
                if r1 + 1 < n_experts and (r1 + 1) not in Wsets:
                    Wsets[r1 + 1] = load_rank(r1 + 1)
        S.barrier()
        F7 = R2
        ln2gB = A.view(F7, [128, 1024], F32)
        ln2bB = A.view(F7 + 4 * K, [128, 1024], F32)
        x1r = [A.view(F7 + 8 * K + i * 4 * K, [128, 1024], F32) for i in range(2)]
        z2 = [A.view(F7 + 16 * K + i * 4 * K, [128, 1024], F32) for i in range(2)]
        ot = [A.view(F7 + 24 * K + i * 4 * K, [128, 1024], F32) for i in range(2)]
        acct = [A.view(F7 + 32 * K + i * 4 * K, [128, 1024], F32) for i in range(2)]
        ygt = [A.view(F7 + 40 * K + i * 4 * K, [128, 1024], F32) for i in range(4)]
        bdn_s = A.view(F7 + 56 * K, [NEXP, 1024], F32, parts=NEXP)
        gT_s = [A.view(F7 + 60 * K + i * 512, [NEXP, 128], F32, parts=NEXP) for i in range(2)]
        dma("sp", ln2gB, bcast(ln2g_d[0:1, :]), writes=["ln2gB"])
        dma("sp", ln2bB, bcast(ln2b_d[0:1, :]), writes=["ln2bB"])
        dma("sp", bdn_s, bdn_d, writes=["bdn_s"])
        outs = []
        gi = 0
        gi_ = [0]
        def fin_A(ti):
            s2 = ti % 2
            dma("sp", x1r[s2], x1s_d[ti * 128:(ti + 1) * 128, :], reads=[("x1s", ti)], writes=[("x1r", s2)])
            pG = bank(4 + s2)[0:NEXP, 0:128]
            tr(pG, gates_all[:, ti, :], ident_f, reads=[("gates", ti), "ident_f"], writes=[("ps", 4 + s2)])
            cp("act", gT_s[s2], pG, reads=[("ps", 4 + s2)], writes=[("gT_s", s2)])
            for hh in range(2):
                bi = hh + 2 * s2
                pb = bank(bi)
                mm(pb, gT_s[s2], bdn_s[:, hh * 512:(hh + 1) * 512], True, True, reads=[("gT_s", s2), "bdn_s"], writes=[("ps", bi)])
                cp("act", acct[s2][:, hh * 512:(hh + 1) * 512], pb, reads=[("ps", bi)], writes=[("acct", s2)])
            for k in range(4):
                g4 = gi_[0] % 4
                gi_[0] += 1
                S.add("pool", lambda h, ti=ti, k=k, g4=g4: h.indirect_dma_start(
                    out=ygt[g4][:, :], out_offset=None, in_=ys_scr[:, :],
                    in_offset=bass.IndirectOffsetOnAxis(ap=destI[:, k, ti:ti + 1], axis=0)),
                    reads=["destI"], writes=[("ygt", g4)], dma=True)
                stt(acct[s2], ygt[g4], W4[:, ti, k:k + 1], acct[s2], ALU.mult, ALU.add,
                    reads=[("ygt", g4), ("W4", ti), ("acct", s2)], writes=[("acct", s2)])

        def fin_B(ti):
            s2 = ti % 2
            tt("dve", z2[s2], acct[s2], G2B, ALU.mult, reads=[("acct", s2), ("GB", 1)], writes=[("z2", s2)])
            stt(z2[s2], x1r[s2], ALPHA, z2[s2], ALU.mult, ALU.add, reads=[("x1r", s2), ("z2", s2)], writes=[("z2", s2)])
            ln_stats(z2[s2], s2, 0, ("z2", s2))
            ln_finish(s2, 1)
            act(z2[s2], z2[s2], AF.Identity, reads=[("z2", s2), ("rstd", s2), ("nmr", s2)], writes=[("z2", s2)],
                bias=nmr_[s2][:, 0:1], scale=rstd_[s2][:, 0:1])
            tt("dve", z2[s2], z2[s2], ln2gB, ALU.mult, reads=[("z2", s2), "ln2gB"], writes=[("z2", s2)])
            tt("dve", ot[s2], z2[s2], ln2bB, ALU.add, reads=[("z2", s2), "ln2bB"], writes=[("ot", s2)])
            dma("sp", out_d[ti * 128:(ti + 1) * 128, :], ot[s2], reads=[("ot", s2)], writes=[("out", ti)])
            outs.append(("out", ti))

        fin_A(0)
        for ti in range(16):
            if ti + 1 < 16:
                S.zipped(lambda: fin_A(ti + 1), lambda: fin_B(ti))
            else:
                fin_B(ti)
        S.add("sp", None, reads=outs)

    dkeys = []
    for name, spec in dbg_out.items():
        if spec is None:
            continue
        ap, shape, dt, keys = spec
        dd = nc.dram_tensor("dbg_" + name, shape, dt, kind="ExternalOutput").ap()
        flat = ap
        if len(ap.shape) == 3:
            flat = ap.rearrange("p a b -> p (a b)")
        dma("sp", dd, flat, reads=keys, writes=[("dbg", name)])
        dkeys.append(("dbg", name))
    if dkeys:
        S.add("sp", None, reads=dkeys)
    S.emit()
    st.close()
    return nc


def t5_bucket_np(dist):
    d = dist.astype(np.float32)
    large = np.float32(16) + np.log(np.maximum(d, np.float32(16.0)) / np.float32(16)) / np.float32(math.log(2048 / 16)) * np.float32(16)
    large = np.minimum(large.astype(np.int32), 31)
    return np.where(dist < 16, dist, large)


def host_layout(inp):
    f = np.float32
    x = np.asarray(inp["x"], f)
    shared = {}
    shared["w_ada"] = np.ascontiguousarray(np.asarray(inp["w_ada"], f)[0])
    shared["b_adaT"] = np.ascontiguousarray(np.asarray(inp["b_ada"], f)[0].reshape(48, 128).T)
    shared["w_in"] = np.ascontiguousarray(np.asarray(inp["w_in"], f)[0])
    shared["gm_g"] = np.asarray(inp["gm_ln_g"], f).reshape(1, 512)
    shared["gm_b"] = np.asarray(inp["gm_ln_b"], f).reshape(1, 512)
    ws = np.asarray(inp["gm_w_s"], f)[0]
    shared["wsT"] = np.ascontiguousarray(ws.transpose(2, 0, 1).reshape(128, 8 * 128))
    bs = np.asarray(inp["gm_b_s"], f)[0]
    bsp = np.empty((128, 4, 128), f)
    for gp in range(4):
        bsp[0:64, gp, :] = bs[2 * gp][None, :]
        bsp[64:128, gp, :] = bs[2 * gp + 1][None, :]
    shared["bsp"] = bsp.reshape(128, 512)
    shared["wa"] = np.ascontiguousarray(np.asarray(inp["w_branch_a"], f)[0])
    shared["wb"] = np.ascontiguousarray(np.asarray(inp["w_branch_b"], f)[0])
    shared["wo"] = np.ascontiguousarray(np.asarray(inp["w_out"], f)[0])
    rel = np.asarray(inp["rel_bias"], f)
    kk = np.arange(128)[:, None]
    qq = np.arange(128)[None, :]
    d_prev = qq + 128 - kk
    d_cur = qq - kk
    biasT = np.empty((128, 24, 2, 128), f)
    for g in range(3):
        bp = t5_bucket_np(np.clip(d_prev, 0, None).astype(np.int32) * DIL[g])
        bc = t5_bucket_np(np.clip(d_cur, 0, None).astype(np.int32) * DIL[g])
        for h in range(8):
            biasT[:, g * 8 + h, 0, :] = rel[bp, g * 8 + h]
            biasT[:, g * 8 + h, 1, :] = rel[bc, g * 8 + h]
    shared["biasT"] = biasT.reshape(128, 24 * 256)
    maskT = np.zeros((128, 2, 128), f)
    maskT[:, 0, :][d_prev > 128] = -1e30
    maskT[:, 1, :][d_cur < 0] = -1e30
    shared["maskT"] = maskT.reshape(128, 256)
    shared["ln1g"] = np.asarray(inp["ln1_g"], f).reshape(1, D)
    shared["ln1b"] = np.asarray(inp["ln1_b"], f).reshape(1, D)
    shared["wr"] = np.ascontiguousarray(np.asarray(inp["w_router"], f)[0])
    shared["br"] = np.asarray(inp["b_router"], f).reshape(1, NEXP)
    shared["wg"] = np.ascontiguousarray(np.asarray(inp["w_gate"], f)[0])
    shared["wu"] = np.ascontiguousarray(np.asarray(inp["w_up"], f)[0])
    shared["wd"] = np.ascontiguousarray(np.asarray(inp["w_down"], f)[0])
    shared["bg"] = np.ascontiguousarray(np.asarray(inp["b_gate"], f)[0])
    shared["bu"] = np.ascontiguousarray(np.asarray(inp["b_up"], f)[0])
    shared["kbtab"] = np.broadcast_to(np.asarray(KBASE, f)[None, :], (128, NEXP)).copy()
    shared["bdn"] = np.ascontiguousarray(np.asarray(inp["b_down"], f)[0])
    shared["ln2g"] = np.asarray(inp["ln2_g"], f).reshape(1, D)
    shared["ln2b"] = np.asarray(inp["ln2_b"], f).reshape(1, D)
    shared["ident"] = np.eye(128, dtype=f)
    c = np.asarray(inp["c"], f)
    in_maps = []
    for k in range(NCORES):
        b, hf = k // 2, k % 2
        m = dict(shared)
        m["xo"] = np.ascontiguousarray(x[b, hf * NTOK:(hf + 1) * NTOK])
        m["xc"] = np.ascontiguousarray(x[b, 0:NTOK]) if hf == 1 else np.zeros((NTOK, D), f)
        m["flag"] = np.full((128, 1), float(hf), f)
        m["cT"] = np.ascontiguousarray(c[b].reshape(8, 128).T)
        in_maps.append(m)
    return in_maps


_NC_CACHE = {}


def kernel(**inputs):
    in_maps = host_layout(inputs)
    if "nc" not in _NC_CACHE:
        _NC_CACHE["nc"] = build()
    nc = _NC_CACHE["nc"]
    res = run_bass_kernel_spmd(nc, in_maps, core_ids=list(range(NCORES)))
    out = np.empty((4, 4096, D), np.float32)
    for k in range(NCORES):
        b, hf = k // 2, k % 2
        out[b, hf * NTOK:(hf + 1) * NTOK] = np.asarray(res.results[k]["out"], np.float32)
    return out
```

```python
import contextlib
import math
import numpy as np
import concourse.bass as bass
import concourse.mybir as mybir
from concourse.bass_utils import run_bass_kernel_spmd

F32 = mybir.dt.float32
BF16 = mybir.dt.bfloat16
I32 = mybir.dt.int32
U32 = mybir.dt.uint32
AF = mybir.ActivationFunctionType
ALU = mybir.AluOpType
AX = mybir.AxisListType

D = 1024
NTOK = 2048
NCORES = 8
NEXP = 32
DIL = (1, 4, 16)
ALPHA = 2.0 ** 0.25
EPS = 1e-5
IN_COLS = 7680
import os
PIPE_ATTN = os.environ.get('PIPE_ATTN', '1') == '1'
ZIP_STAGES = os.environ.get('ZIP_STAGES', '1') == '1'
ZIP_GMLP = os.environ.get('ZIP_GMLP', '0') == '1'
KCAP = [((min(2048, 8192 // (r + 1)) + 127) // 128) * 128 for r in range(32)]
KBASE = [sum(KCAP[:r]) for r in range(32)]
NSLOT = sum(KCAP)

ENGINES = ("pe", "act", "dve", "pool", "sp")
COMPUTE = ("pe", "act", "dve", "pool")
HANDLES = {"pe": "tensor", "act": "scalar", "dve": "vector", "pool": "gpsimd", "sp": "sync"}


class Sched:
    NDMASEM = 12
    SAME_ENG_WINDOW = 3

    def __init__(self, nc):
        self.nc = nc
        self.ops = {e: [] for e in ENGINES}
        self.lastw = {}
        self.readers = {}
        self.capture = None

    def add(self, eng, fn, reads=(), writes=(), dma=False):
        if self.capture is not None:
            self.capture.append((eng, fn, tuple(reads), tuple(writes), dma))
            return None
        ops = self.ops[eng]
        idx = len(ops)
        deps = {}
        dmadeps = set()

        def anydep(p):
            if p == (eng, idx):
                return
            if self.ops[p[0]][p[1]]["dma"]:
                dmadeps.add(p)
            elif deps.get(p[0], -1) < p[1]:
                deps[p[0]] = p[1]

        for k in reads:
            w = self.lastw.get(k)
            if w is not None:
                anydep(w)
        for k in writes:
            w = self.lastw.get(k)
            if w is not None:
                anydep(w)
            for r in self.readers.get(k, ()):
                anydep(r)
        for k in reads:
            self.readers.setdefault(k, []).append((eng, idx))
        for k in writes:
            self.lastw[k] = (eng, idx)
            self.readers[k] = []
        if eng in deps and not dma:
            if eng == "pe" or deps[eng] < idx - self.SAME_ENG_WINDOW:
                del deps[eng]
        ops.append(dict(fn=fn, deps=deps, dmadeps=sorted(dmadeps), dma=dma, marked=False))
        return (eng, idx)

    def zipped(self, *stage_fns):
        if not ZIP_STAGES:
            for f in stage_fns:
                f()
            return
        lists = []
        for f in stage_fns:
            self.capture = []
            f()
            lists.append(self.capture)
            self.capture = None
        items = []
        for li, l in enumerate(lists):
            n = len(l)
            for i, op in enumerate(l):
                items.append(((i + 0.5) / max(n, 1), li, i, op))
        items.sort(key=lambda x: (x[0], x[1], x[2]))
        for _, _, _, op in items:
            self.add(*op)

    def barrier(self):
        last = {}
        for e in COMPUTE:
            for i in range(len(self.ops[e]) - 1, -1, -1):
                op = self.ops[e][i]
                if op["fn"] is not None and not op["dma"]:
                    last[e] = i
                    break
        dmas = []
        for e in ENGINES:
            seen = 0
            for i in range(len(self.ops[e]) - 1, -1, -1):
                if self.ops[e][i]["dma"]:
                    dmas.append((e, i))
                    seen += 1
                    if seen >= self.NDMASEM:
                        break
        for e in ENGINES:
            if not self.ops[e]:
                continue
            deps = {p: i for p, i in last.items() if p != e}
            self.ops[e].append(dict(fn=None, deps=deps, dmadeps=sorted(dmas), dma=False, marked=False))
        self.lastw = {}
        self.readers = {}

    def emit(self):
        nc = self.nc
        ops = self.ops
        for e in ENGINES:
            for op in ops[e]:
                for pe_, pi in op["deps"].items():
                    ops[pe_][pi]["marked"] = True
        for e in COMPUTE:
            c = 0
            for op in ops[e]:
                if op["dma"] or op["fn"] is None:
                    op["cnt"] = c
                    continue
                if op["marked"]:
                    c += 1
                op["cnt"] = c
        stack = contextlib.ExitStack()
        csem = {e: stack.enter_context(nc.semaphore("cs_" + e)) for e in COMPUTE}
        for e in ENGINES:
            n = 0
            ring = None
            for op in ops[e]:
                if not op["dma"]:
                    continue
                if ring is None:
                    ring = [stack.enter_context(nc.semaphore("ds_%s_%d" % (e, i))) for i in range(self.NDMASEM)]
                s = n % self.NDMASEM
                k = n // self.NDMASEM
                op["dsem"] = ring[s]
                op["dsem_id"] = (e, s)
                op["dtarget"] = 16 * (k + 1)
                op["dprev"] = 16 * k
                n += 1
        block = stack.enter_context(nc.Block())

        def make(e):
            def body(h):
                waited = {}

                def wait(key, sem, val):
                    if val <= 0 or waited.get(key, 0) >= val:
                        return
                    waited[key] = val
                    h.wait_ge(sem, val)

                for op in ops[e]:
                    for pe_, pi in op["deps"].items():
                        wait(("c", pe_), csem[pe_], ops[pe_][pi]["cnt"])
                    for (qe, qi) in op["dmadeps"]:
                        q = ops[qe][qi]
                        wait(("d",) + q["dsem_id"], q["dsem"], q["dtarget"])
                    if op["fn"] is None:
                        continue
                    if op["dma"]:
                        wait(("d",) + op["dsem_id"], op["dsem"], op["dprev"])
                        ins = op["fn"](h)
                        ins.then_inc(op["dsem"], 16)
                    else:
                        ins = op["fn"](h)
                        if op["marked"]:
                            ins.then_inc(csem[e], 1)
            return body

        for e in ENGINES:
            if ops[e]:
                getattr(block, HANDLES[e])(make(e))
        stack.close()


class Arena:
    def __init__(self, ap_f32, nbytes):
        self.ap = ap_f32
        self.nbytes = nbytes

    def view(self, off, shape, dtype, parts=128):
        esz = 2 if dtype == BF16 else 4
        n = int(np.prod(shape[1:]))
        assert off % 4 == 0 and off + n * esz <= self.nbytes, (off, shape, self.nbytes)
        nw = (n * esz + 3) // 4
        v = self.ap[0:parts, off // 4: off // 4 + nw]
        if dtype != F32:
            v = v.bitcast(dtype)
        if len(shape) == 3:
            v = v.rearrange("p (a b) -> p a b", a=shape[1])
        elif len(shape) == 4:
            v = v.rearrange("p (a b c) -> p a b c", a=shape[1], b=shape[2])
        return v


def build(stop_after=None, n_experts=NEXP, dbg=()):
    nc = bass.Bass("TRN2", target_bir_lowering=False)

    def din(name, shape, dt=F32):
        return nc.dram_tensor(name, list(shape), dt, kind="ExternalInput").ap()

    xo = din("xo", [NTOK, D])
    xc = din("xc", [NTOK, D])
    flag_d = din("flag", [128, 1])
    cT_d = din("cT", [128, 8])
    w_ada = din("w_ada", [D, 6 * D])
    b_adaT = din("b_adaT", [128, 48])
    w_in = din("w_in", [D, IN_COLS])
    gm_g = din("gm_g", [1, 512])
    gm_b = din("gm_b", [1, 512])
    wsT_d = din("wsT", [128, 8 * 128])
    bsp_d = din("bsp", [128, 4 * 128])
    wa_d = din("wa", [512, D])
    wb_d = din("wb", [512, D])
    wo_d = din("wo", [D, D])
    biasT_d = din("biasT", [128, 24 * 256])
    maskT_d = din("maskT", [128, 256])
    ln1g_d = din("ln1g", [1, D])
    ln1b_d = din("ln1b", [1, D])
    wr_d = din("wr", [D, NEXP])
    br_d = din("br", [1, NEXP])
    wg_d = din("wg", [n_experts, D, D])
    wu_d = din("wu", [n_experts, D, D])
    wd_d = din("wd", [n_experts, D, D])
    bg_d = din("bg", [NEXP, D])
    bu_d = din("bu", [NEXP, D])
    kbtab_d = din("kbtab", [128, NEXP])
    bdn_d = din("bdn", [NEXP, D])
    ln2g_d = din("ln2g", [1, D])
    ln2b_d = din("ln2b", [1, D])
    ident_d = din("ident", [128, 128])
    out_d = nc.dram_tensor("out", [NTOK, D], F32, kind="ExternalOutput").ap()
    x1s_d = nc.dram_tensor("x1s", [NTOK, D], F32, kind="Internal").ap()
    xs_scr = nc.dram_tensor("xs_scr", [NSLOT, D], BF16, kind="Internal").ap()
    ys_scr = nc.dram_tensor("ys_scr", [NSLOT, D], F32, kind="Internal").ap()
    dbg_out = {}

    w_in_v = w_in.rearrange("(kc p) n -> p kc n", p=128)
    w_ada_v = w_ada.rearrange("(kc p) n -> p kc n", p=128)

    st = contextlib.ExitStack()
    ARENA_BYTES = 207 * 1024
    arena_t = st.enter_context(nc.sbuf_tensor("arena", [128, ARENA_BYTES // 4], F32))
    A = Arena(arena_t, ARENA_BYTES)
    psum_t = st.enter_context(nc.psum_tensor("psum", [128, 4096], F32))

    def bank(i, shape=None, dtype=F32, half=None):
        v = psum_t[:, i * 512:(i + 1) * 512]
        if dtype != F32:
            v = v.bitcast(dtype)
        if shape is not None and len(shape) == 3:
            v = v.rearrange("p (a b) -> p a b", a=shape[1])
        return v

    S = Sched(nc)
    K = 1024

    off = [0]

    def palloc(shape, dtype, parts=128):
        esz = 2 if dtype == BF16 else 4
        n = int(np.prod(shape[1:])) * esz
        n = (n + 3) // 4 * 4
        v = A.view(off[0], shape, dtype, parts)
        off[0] += n
        return v

    ident_f = palloc([128, 128], F32)
    ident_b = palloc([128, 128], BF16)
    ones_f = palloc([128, 128], F32)
    ones_b = palloc([128, 64], BF16)
    onesf_b = palloc([128, 64], BF16)
    flag_s = palloc([128, 1], F32)
    cT_s = palloc([128, 8], F32)
    scT = palloc([128, 8], F32)
    scT_b = palloc([128, 8], BF16)
    badaT = palloc([128, 48], F32)
    modT = palloc([128, 48], F32)
    opsc1 = palloc([128, 8], F32)
    opsc2 = palloc([128, 8], F32)
    G1B = palloc([128, 1024], F32)
    G2B = palloc([128, 1024], F32)
    gates_all = palloc([128, 16, NEXP], F32)
    bgR = palloc([128, 8, NEXP], F32)
    bu1R = palloc([128, 8, NEXP], F32)
    E4f = palloc([128, 16, 4], F32)
    W4 = palloc([128, 16, 4], F32)
    destI = palloc([128, 4, 16], I32)
    idxW = palloc([128, NEXP, 8], I32)
    Mall = palloc([128, 16, NEXP], BF16)
    mi8_ = [palloc([128, 8], U32) for _ in range(2)]
    e4_ = [palloc([128, 4], F32) for _ in range(2)]
    maskT = palloc([128, 256], F32)
    stt_ = [palloc([128, 4, 2, 6], F32) for _ in range(2)]
    mv_ = [palloc([128, 4, 2], F32) for _ in range(2)]
    sd_ = [palloc([128, 4], F32) for _ in range(2)]
    rstd_ = [palloc([128, 4], F32) for _ in range(2)]
    nmr_ = [palloc([128, 4], F32) for _ in range(2)]
    mx8_ = [palloc([128, 8], F32) for _ in range(2)]
    negm_ = [palloc([128, 1], F32) for _ in range(2)]
    den_ = [palloc([128, 1], F32) for _ in range(2)]
    rden_ = [palloc([128, 1], F32) for _ in range(2)]
    lg_ = [palloc([128, NEXP], F32) for _ in range(2)]
    msk_ = [palloc([128, NEXP], F32) for _ in range(2)]
    ex_ = [palloc([128, NEXP], F32) for _ in range(2)]
    em_ = [palloc([128, NEXP], F32) for _ in range(2)]
    brB = palloc([128, NEXP], F32)
    R0_END = 32 * K
    assert off[0] <= R0_END, off[0]
    MOE_TMP = off[0]

    def dma(q, out, in_, reads=(), writes=()):
        return S.add(q, lambda h: h.dma_start(out=out, in_=in_), reads=reads, writes=writes, dma=True)

    def mm(out, lhsT, rhs, start, stop, reads, writes):
        return S.add("pe", lambda h: h.matmul(out, lhsT=lhsT, rhs=rhs, start=start, stop=stop), reads=reads, writes=writes)

    def tr(out, in_, ident, reads, writes):
        return S.add("pe", lambda h: h.transpose(out, in_, ident), reads=reads, writes=writes)

    def act(out, in_, func, reads, writes, bias=None, scale=None):
        kw = {}
        if bias is not None:
            kw["bias"] = bias
        if scale is not None:
            kw["scale"] = scale
        return S.add("act", lambda h: h.activation(out=out, in_=in_, func=func, **kw), reads=reads, writes=writes)

    def tt(eng, out, in0, in1, op, reads, writes):
        return S.add(eng, lambda h: h.tensor_tensor(out=out, in0=in0, in1=in1, op=op), reads=reads, writes=writes)

    def ts(eng, out, in0, s1, s2, op0, op1, reads, writes):
        if op1 is None:
            return S.add(eng, lambda h: h.tensor_scalar(out=out, in0=in0, scalar1=s1, scalar2=None, op0=op0), reads=reads, writes=writes)
        return S.add(eng, lambda h: h.tensor_scalar(out=out, in0=in0, scalar1=s1, scalar2=s2, op0=op0, op1=op1), reads=reads, writes=writes)

    def stt(out, in0, scalar, in1, op0, op1, reads, writes):
        return S.add("dve", lambda h: h.scalar_tensor_tensor(out=out, in0=in0, scalar=scalar, in1=in1, op0=op0, op1=op1), reads=reads, writes=writes)

    def cp(eng, out, in_, reads, writes):
        if eng == "act":
            return S.add("act", lambda h: h.activation(out=out, in_=in_, func=AF.Copy), reads=reads, writes=writes)
        return S.add(eng, lambda h: h.tensor_copy(out=out, in_=in_), reads=reads, writes=writes)

    def memset(eng, ap, val, writes):
        return S.add(eng, lambda h: h.memset(ap, val), writes=writes)

    def bcast(row_ap):
        return row_ap.partition_broadcast(128)

    def ln_stats(x_ap, slot, t, key_in, nhalf=2):
        for hh in range(nhalf):
            S.add("dve", lambda h, hh=hh: h.bn_stats(out=stt_[slot][:, t, hh, :], in_=x_ap[:, hh * 512:(hh + 1) * 512]),
                  reads=[key_in], writes=[("st", slot, t, hh)])
        src = stt_[slot][:, t, 0:nhalf, :].rearrange("p a b -> p (a b)")
        S.add("dve", lambda h: h.bn_aggr(out=mv_[slot][:, t, :], in_=src),
              reads=[("st", slot, t, hh) for hh in range(nhalf)], writes=[("mv", slot, t)])

    def ln_finish(slot, nt):
        act(sd_[slot][:, 0:nt], mv_[slot][:, 0:nt, 1], AF.Ln, reads=[("mv", slot, t) for t in range(nt)] + ["eps"],
            writes=[("sd", slot)], bias=EPS_AP[0], scale=1.0)
        act(rstd_[slot][:, 0:nt], sd_[slot][:, 0:nt], AF.Exp, reads=[("sd", slot)], writes=[("rstd", slot)], scale=-0.5)
        stt(nmr_[slot][:, 0:nt], mv_[slot][:, 0:nt, 0], -1.0, rstd_[slot][:, 0:nt], ALU.mult, ALU.mult,
            reads=[("mv", slot, t) for t in range(nt)] + [("rstd", slot)], writes=[("nmr", slot)])

    EPS_AP = [None]
    eps_t = A.view(MOE_TMP, [128, 1], F32)
    EPS_AP[0] = eps_t
    MOE_TMP += 4

    dma("sp", ident_f, ident_d, writes=["ident_f"])
    dma("sp", flag_s, flag_d, writes=["flag"])
    dma("sp", cT_s, cT_d, writes=["cT"])
    dma("sp", badaT, b_adaT, writes=["badaT"])
    dma("sp", maskT, maskT_d, writes=["maskT"])
    memset("dve", eps_t, EPS, writes=["eps"])
    memset("dve", ones_f, 1.0, writes=["ones_f"])
    memset("dve", ones_b, 1.0, writes=["ones_b"])
    cp("dve", ident_b, ident_f, reads=["ident_f"], writes=["ident_b"])
    ts("dve", onesf_b, ones_b, flag_s[:, 0:1], None, ALU.mult, None, reads=["flag", "ones_b"], writes=["onesf_b"])
    act(scT, cT_s, AF.Silu, reads=["cT"], writes=["scT"])
    cp("dve", scT_b, scT, reads=["scT"], writes=["scT_b"])

    R5 = 144 * K
    wad = [A.view(R5 + i * 8 * K, [128, 8, 512], BF16) for i in range(4)]
    psM = bank(7)
    for blk in range(12):
        wbuf = wad[blk % 4]
        dma("pool", wbuf, w_ada_v[:, :, blk * 512:(blk + 1) * 512], writes=[("wad", blk % 4)])
        for jj in range(4):
            j = blk * 4 + jj
            for kc in range(8):
                mm(psM[:, j:j + 1], wbuf[:, kc, jj * 128:(jj + 1) * 128], scT_b[:, kc:kc + 1], kc == 0, kc == 7,
                   reads=[("wad", blk % 4), "scT_b"], writes=[("ps", 7)])
    tt("dve", modT, psM[:, 0:48], badaT, ALU.add, reads=[("ps", 7), "badaT"], writes=["modT"])
    ts("dve", opsc1, modT[:, 8:16], 1.0, None, ALU.add, None, reads=["modT"], writes=["opsc1"])
    ts("dve", opsc2, modT[:, 32:40], 1.0, None, ALU.add, None, reads=["modT"], writes=["opsc2"])
    dg = [A.view(R5 + 32 * K + i * 512, [128, 128], F32) for i in range(2)]
    for gi, (GB, col0) in enumerate(((G1B, 16), (G2B, 40))):
        for c in range(8):
            d_ = dg[c % 2]
            ts("dve", d_, ident_f, modT[:, col0 + c:col0 + c + 1], None, ALU.mult, None,
               reads=["ident_f", "modT"], writes=[("dg", c % 2)])
            pb = bank(gi * 2 + c // 4)
            mm(pb[:, (c % 4) * 128:(c % 4 + 1) * 128], ones_f, d_, True, True,
               reads=["ones_f", ("dg", c % 2)], writes=[("ps", gi * 2 + c // 4)])
        for hh in range(2):
            cp("dve", GB[:, hh * 512:(hh + 1) * 512], bank(gi * 2 + hh), reads=[("ps", gi * 2 + hh)], writes=[("GB", gi)])
    sh1 = modT[:, 0:8]
    sh2 = modT[:, 24:32]

    R1 = 32 * K
    R2 = 64 * K
    hT = A.view(R2, [128, 8, 4096], BF16)
    R3 = R2 + 64 * K
    uT = A.view(R3, [128, 4, NTOK], BF16)
    R4 = R3 + 16 * K
    ybT = A.view(R4, [128, 4, NTOK], BF16)
    R5 = R4 + 16 * K
    xs = [A.view(R1 + i * 4 * K, [128, 1024], F32) for i in range(8)]
    xn = [A.view(180 * K + i * 2 * K, [128, 1024], BF16) for i in range(4)]

    def x_tile(tt_):
        return xc[tt_ * 128:(tt_ + 1) * 128, :] if tt_ < 16 else xo[(tt_ - 16) * 128:(tt_ - 15) * 128, :]

    evac_flip = [0]

    def evac_mod(out, in_, scale_ap, bias_ap, reads, writes, eng=None):
        if eng is None:
            evac_flip[0] ^= 1
            eng = "act" if evac_flip[0] else "dve"
        if eng == "act":
            act(out, in_, AF.Identity, reads=reads, writes=writes, bias=bias_ap, scale=scale_ap)
        else:
            ts("dve", out, in_, scale_ap, bias_ap, ALU.mult, ALU.add, reads=reads, writes=writes)

    def ph1_A(b):
        slot = b % 2
        xo_ = (b % 2) * 4
        for t in range(4):
            tile = b * 4 + t
            dma("sp", xs[xo_ + t], x_tile(tile), writes=[("xs", xo_ + t)])
            ln_stats(xs[xo_ + t], slot, t, ("xs", xo_ + t))
        ln_finish(slot, 4)

    def ph1_B(b):
        slot = b % 2
        xo_ = (b % 2) * 4
        for hb in range(2):
            b0 = 2 * (hb + 2 * (b % 2))
            pT = psum_t[:, b0 * 512:(b0 + 2) * 512].bitcast(BF16).rearrange("p (a b) -> p a b", a=8)
            pkey = ("ps", b0)
            for t2 in range(2):
                t = hb * 2 + t2
                act(xn[t], xs[xo_ + t], AF.Identity, reads=[("xs", xo_ + t), ("rstd", slot), ("nmr", slot)], writes=[("xn", t)],
                    bias=nmr_[slot][:, t:t + 1], scale=rstd_[slot][:, t:t + 1])
                for kc in range(8):
                    tr(pT[:, kc, t2 * 128:(t2 + 1) * 128], xn[t][:, kc * 128:(kc + 1) * 128], ident_b,
                       reads=[("xn", t), "ident_b"], writes=[pkey, ("ps", b0 + 1)])
            tok0 = b * 512 + hb * 256
            for kc in range(8):
                evac_mod(hT[:, kc, tok0:tok0 + 256], pT[:, kc, :], opsc1[:, kc:kc + 1], sh1[:, kc:kc + 1],
                         reads=[pkey, ("ps", b0 + 1), "opsc1", "modT"], writes=[("hT", b, kc)], eng=("act" if kc < 4 else "dve"))


    ph1_A(0)
    for b in range(8):
        if b + 1 < 8:
            ph1_A(b + 1)
        ph1_B(b)

    if "hT" in dbg:
        dbg_out["hT"] = (hT, [128, 8 * 4096], BF16, [("hT", b, kc) for b in range(8) for kc in range(8)])

    NWR = 7
    wring = [A.view(R5 + i * 2 * K, [128, 8, 128], BF16) for i in range(NWR)]
    wr_n = [0]

    def load_chunk(src_v, col0, ncols=128):
        i = wr_n[0] % NWR
        wr_n[0] += 1
        dma("pool", wring[i][:, :, 0:ncols], src_v[:, :, col0:col0 + ncols], writes=[("wring", i)])
        return wring[i], ("wring", i)

    ps_n = [0]

    def next_bank(lo=0, n=2):
        i = lo + ps_n[0] % n
        ps_n[0] += 1
        return i

    def proj_T(wbuf, wkey, tb, nk=8, src=None, srckeys=None, bank_lo=0, bank_n=2):
        bi = next_bank(bank_lo, bank_n)
        pb = bank(bi)
        for kc in range(nk):
            mm(pb, wbuf[:, kc, :], hT[:, kc, tb * 512:(tb + 1) * 512], kc == 0, kc == nk - 1,
               reads=[wkey, ("hT", tb, kc)], writes=[("ps", bi)])
        return pb, ("ps", bi)

    if stop_after != "hT":
        GM = R1
        gmgB = A.view(GM, [128, 512], F32)
        gmbB = A.view(GM + 2 * K, [128, 512], F32)
        BSp = A.view(GM + 4 * K, [128, 4, 128], F32)
        wsT_f = A.view(GM + 6 * K, [128, 8, 128], F32)
        wsT_b = A.view(GM + 10 * K, [128, 8, 128], BF16)
        vg = [A.view(GM + 12 * K + i * 2 * K, [128, 512], F32) for i in range(2)]
        zz = [A.view(GM + 16 * K + i * 2 * K, [128, 512], F32) for i in range(2)]
        vn = [A.view(GM + 20 * K + i * K, [128, 512], BF16) for i in range(2)]
        tmpg = [A.view(GM + 22 * K + i * 2 * K, [128, 4, 128], F32) for i in range(2)]
        Wvg = A.view(R5 + 14 * K, [128, 8, 512], BF16)
        S.barrier()
        dma("sp", gmgB, bcast(gm_g[0:1, :]), writes=["gmgB"])
        dma("sp", gmbB, bcast(gm_b[0:1, :]), writes=["gmbB"])
        dma("sp", BSp, bsp_d.rearrange("p (a b) -> p a b", a=4), writes=["BSp"])
        dma("sp", wsT_f, wsT_d.rearrange("p (a b) -> p a b", a=8), writes=["wsT_f"])
        S.add("pool", lambda h: h.affine_select(out=wsT_f, in_=wsT_f, pattern=[[0, 8], [1, 128]], compare_op=ALU.is_ge,
                                                 fill=0.0, base=0, channel_multiplier=-1),
              reads=["wsT_f"], writes=["wsT_f"])
        cp("pool", wsT_b, wsT_f, reads=["wsT_f"], writes=["wsT_b"])
        dma("pool", Wvg, w_in_v[:, :, 512:1024], writes=["Wvg"])
        for c in range(4):
            wbuf, wkey = load_chunk(w_in_v, c * 128)
            for tb in range(4, 8):
                pb, pkey = proj_T(wbuf, wkey, tb)
                act(uT[:, c, (tb - 4) * 512:(tb - 3) * 512], pb, AF.Gelu_apprx_tanh, reads=[pkey], writes=[("uT", tb - 4)])
        def gmlpA(ti):
            tile = 16 + ti
            sl = ti % 2
            bi = next_bank(0, 2)
            pb = bank(bi)
            for kc in range(8):
                mm(pb, hT[:, kc, tile * 128:(tile + 1) * 128], Wvg[:, kc, :], kc == 0, kc == 7,
                   reads=[("hT", tile // 4, kc), "Wvg"], writes=[("ps", bi)])
            act(vg[sl], pb, AF.Gelu_apprx_tanh, reads=[("ps", bi)], writes=[("vg", sl)])
            ln_stats(vg[sl], sl, 0, ("vg", sl), nhalf=1)
            ln_finish(sl, 1)
            act(zz[sl], vg[sl], AF.Identity, reads=[("vg", sl), ("rstd", sl), ("nmr", sl)], writes=[("zz", sl)],
                bias=nmr_[sl][:, 0:1], scale=rstd_[sl][:, 0:1])
            tt("dve", zz[sl], zz[sl], gmgB, ALU.mult, reads=[("zz", sl), "gmgB"], writes=[("zz", sl)])
            tt("dve", vn[sl], zz[sl], gmbB, ALU.add, reads=[("zz", sl), "gmbB"], writes=[("vn", sl)])

        def gmlpB(ti):
            sl = ti % 2
            bs_i = 2 + ti % 2
            pS = bank(bs_i, [128, 4, 128])
            for gp in range(4):
                for hh in range(2):
                    g_ = 2 * gp + hh
                    mm(pS[hh * 64:(hh + 1) * 64, gp, :], vn[sl][:, g_ * 64:(g_ + 1) * 64], wsT_b[:, g_, :], True, True,
                       reads=[("vn", sl), "wsT_b"], writes=[("ps", bs_i)])
            tt("dve", tmpg[sl], pS, BSp, ALU.add, reads=[("ps", bs_i), "BSp"], writes=[("tmpg", sl)])
            tt("dve", uT[:, :, ti * 128:(ti + 1) * 128], tmpg[sl], uT[:, :, ti * 128:(ti + 1) * 128], ALU.mult,
               reads=[("tmpg", sl), ("uT", ti // 4)], writes=[("uT", ti // 4)])

        gmlpA(0)
        for ti in range(16):
            if ti + 1 < 16:
                if ZIP_GMLP:
                    S.zipped(lambda: gmlpA(ti + 1), lambda: gmlpB(ti))
                else:
                    gmlpA(ti + 1)
                    gmlpB(ti)
            else:
                gmlpB(ti)
        if "yaT" in dbg:
            dbg_out["yaT"] = (uT, [128, 4 * NTOK], BF16, [("uT", i) for i in range(4)])

    if stop_after not in ("hT", "gmlp"):
        AT = R1
        accND = A.view(AT, [128, 2, NTOK], F32)
        sbS = [A.view(AT + 16 * K + i * K, [128, 256], F32) for i in range(4)]
        ptS = [A.view(AT + 20 * K + i * 512, [128, 256], BF16) for i in range(4)]
        braw = [A.view(AT + 22 * K + i * 2 * K, [128, 2, 256], F32) for i in range(2)]
        biasm = [A.view(AT + 26 * K + i * 2 * K, [128, 2, 256], F32) for i in range(2)]
        QKV0 = R5 + 14 * K
        QT = [A.view(QKV0, [128, NTOK], BF16), A.view(QKV0 + 20 * K, [128, NTOK], BF16)]
        KT = [A.view(QKV0 + 4 * K, [128, 4096], BF16)] * 2
        VV = [A.view(QKV0 + 12 * K, [128, 32, 128], BF16), A.view(QKV0 + 24 * K, [128, 32, 128], BF16)]
        assert QKV0 + 32 * K <= ARENA_BYTES
        S.barrier()
        ztile = A.view(MOE_TMP, [128, 1024], BF16)
        memset("pool", ztile, 0.0, writes=["ztile"])
        ZR = 1024
        zlist = list(range(0, NSLOT, ZR))

        def zero_fill(cnt):
            for _ in range(cnt):
                if not zlist:
                    return
                z0 = zlist.pop(0)
                zn_ = min(ZR, NSLOT - z0)
                dma("sp", xs_scr[z0:z0 + zn_, :].rearrange("(t p) d -> p t d", p=128),
                    ztile.unsqueeze(1).broadcast_to([128, zn_ // 128, 1024]), reads=["ztile"], writes=[("xs_zero", z0)])
        unit_n = 0
        for j in range(4):
            for g in range(3):
                r = DIL[g]
                nb_rho = 16 // r
                sl = unit_n % 2
                unit_n += 1
                kq = ("QT", sl)
                kk = ("KT", 0)
                col = 1024 + g * 1536 + j * 128
                wq, wqk = load_chunk(w_in_v, col)
                wk, wkk = load_chunk(w_in_v, col + 512)
                wv, wvk = load_chunk(w_in_v, col + 1024)
                dma("sp", braw[sl], biasT_d.rearrange("p (a b) -> p a b", a=24)[:, g * 8 + 2 * j:g * 8 + 2 * j + 2, :],
                    writes=[("braw", sl)])
                zero_fill(3)
                for hh in range(2):
                    tt("pool", biasm[sl][:, hh, :], braw[sl][:, hh, :], maskT, ALU.add,
                       reads=[("braw", sl), "maskT"], writes=[("biasm", sl)])
                for tb in range(4, 8):
                    pb, pkey = proj_T(wq, wqk, tb)
                    act(QT[sl][:, (tb - 4) * 512:(tb - 3) * 512], pb, AF.Identity, reads=[pkey], writes=[kq], scale=0.125)
                tb_lo = 0 if g == 2 else 3
                for tb in range(tb_lo, 8):
                    pb, pkey = proj_T(wk, wkk, tb)
                    cp("dve", KT[sl][:, tb * 512:(tb + 1) * 512], pb, reads=[pkey], writes=[kk])
                for rho in range(r):
                    for ib in range(2 * nb_rho):
                        ctx = ib < nb_rho
                        if ctx and g < 2 and ib != nb_rho - 1:
                            continue
                        vt = (0 if ctx else 16) + rho * nb_rho + ib % nb_rho
                        bi = 2 + (vt % 2)
                        pv = bank(bi)[:, 0:128]
                        start = rho + r * 128 * ib
                        for kc in range(8):
                            mm(pv, hT[:, kc, start:start + 127 * r + 1:r], wv[:, kc, :], kc == 0, kc == 7,
                               reads=[("hT", b_, kc) for b_ in range(start // 512, (start + 128 * r - 1) // 512 + 1)] + [wvk],
                               writes=[("ps", bi)])
                        if ctx:
                            act(VV[sl][:, vt, :], pv, AF.Identity, reads=[("ps", bi), "flag"], writes=[("V", sl)], scale=flag_s[:, 0:1])
                        else:
                            cp("dve", VV[sl][:, vt, :], pv, reads=[("ps", bi)], writes=[("V", sl)])
                def unit_geom(qb):
                    rho = qb // nb_rho
                    ib_cur = nb_rho + qb % nb_rho
                    qcol0 = rho + r * 128 * ib_cur - NTOK
                    q_sl = slice(qcol0, qcol0 + 127 * r + 1, r)
                    ktile = []
                    for ib in (ib_cur - 1, ib_cur):
                        ctx = ib < nb_rho
                        vt = (0 if ctx else 16) + rho * nb_rho + ib % nb_rho
                        kst = rho + r * 128 * ib
                        ktile.append((slice(kst, kst + 127 * r + 1, r), vt, ctx))
                    return q_sl, ktile

                def unit_S(qb):
                    q_sl, ktile = unit_geom(qb)
                    u = qb % 2
                    for hh in range(2):
                        bS = 2 * hh + u
                        pS = bank(bS)[:, 0:256]
                        pk = ("ps", bS)
                        p0, p1 = hh * 64, hh * 64 + 64
                        for kb in range(2):
                            mm(pS[:, kb * 128:(kb + 1) * 128], KT[sl][p0:p1, ktile[kb][0]], QT[sl][p0:p1, q_sl], True, True,
                               reads=[kk, kq], writes=[pk])
                        si = u * 2 + hh
                        stt(sbS[si], pS, 60.0, biasm[sl][:, hh, :], ALU.min, ALU.add,
                            reads=[pk, ("biasm", sl)], writes=[("sbS", si)])
                        act(ptS[si], sbS[si], AF.Exp, reads=[("sbS", si)], writes=[("ptS", si)])

                def unit_PV(qb):
                    q_sl, ktile = unit_geom(qb)
                    u = qb % 2
                    pND = bank(4 + u)[:, 0:256].rearrange("p (a b) -> p a b", a=2)
                    pkN = ("ps", 4 + u)
                    for which in range(2):
                        for hh in range(2):
                            si = u * 2 + hh
                            p0, p1 = hh * 64, hh * 64 + 64
                            for kb in range(2):
                                _, vt, ctx = ktile[kb]
                                if which == 0:
                                    lhs = VV[sl][:, vt, p0:p1]
                                    rk = [("V", sl)]
                                else:
                                    lhs = onesf_b if ctx else ones_b
                                    rk = ["onesf_b", "ones_b"]
                                mm(pND[p0:p1, which, :], lhs, ptS[si][:, kb * 128:(kb + 1) * 128], kb == 0, kb == 1,
                                   reads=rk + [("ptS", si)], writes=[pkN])
                    dst = accND[:, :, q_sl]
                    if g == 0:
                        cp("dve", dst, pND, reads=[pkN], writes=["accND"])
                    else:
                        tt("dve", dst, pND, dst, ALU.add, reads=[pkN, "accND"], writes=["accND"])

                if PIPE_ATTN:
                    unit_S(0)
                    for qb in range(16):
                        if qb + 1 < 16:
                            unit_S(qb + 1)
                        unit_PV(qb)
                else:
                    for qb in range(16):
                        unit_S(qb)
                        unit_PV(qb)
            S.add("dve", lambda h: h.reciprocal(out=accND[:, 1, :], in_=accND[:, 1, :]), reads=["accND"], writes=["accND"])
            tt("dve", ybT[:, j, :], accND[:, 0, :], accND[:, 1, :], ALU.mult, reads=["accND"], writes=[("ybT", j)])
        if "ybT" in dbg:
            dbg_out["ybT"] = (ybT, [128, 4 * NTOK], BF16, [("ybT", i) for i in range(4)])

    if stop_after not in ("hT", "gmlp", "attn"):
        S.barrier()
        mT = A.view(R1, [128, 8, NTOK], BF16)
        Wa = A.view(R5 + 14 * K, [128, 4, 1024], BF16)
        Wb = A.view(R5 + 22 * K, [128, 4, 1024], BF16)
        gab = [A.view(R5 + 30 * K + i * 2 * K, [128, 512], F32) for i in range(4)]
        t12 = [A.view(R5 + 38 * K + i * 2 * K, [128, 512], F32) for i in range(4)]
        dma("pool", Wa, wa_d.rearrange("(kc p) n -> p kc n", p=128), writes=["Wa"])
        dma("pool", Wb, wb_d.rearrange("(kc p) n -> p kc n", p=128), writes=["Wb"])
        it = 0
        for m in range(8):
            wga, wgak = load_chunk(w_in_v, 5632 + m * 128)
            wgb, wgbk = load_chunk(w_in_v, 6656 + m * 128)
            for tb in range(4):
                s2 = it % 2
                it += 1
                pa = bank(0 + s2 * 4)
                pbb = bank(1 + s2 * 4)
                for kc in range(4):
                    mm(pa, Wa[:, kc, m * 128:(m + 1) * 128], uT[:, kc, tb * 512:(tb + 1) * 512], kc == 0, kc == 3,
                       reads=["Wa", ("uT", tb)], writes=[("ps", 0 + s2 * 4)])
                for kc in range(4):
                    mm(pbb, Wb[:, kc, m * 128:(m + 1) * 128], ybT[:, kc, tb * 512:(tb + 1) * 512], kc == 0, kc == 3,
                       reads=["Wb"] + [("ybT", jj) for jj in range(4)], writes=[("ps", 1 + s2 * 4)])
                pga, pgak = proj_T(wga, wgak, 4 + tb, bank_lo=2 + s2 * 4, bank_n=1)
                pgb, pgbk = proj_T(wgb, wgbk, 4 + tb, bank_lo=3 + s2 * 4, bank_n=1)
                act(gab[s2 * 2], pga, AF.Sigmoid, reads=[pgak], writes=[("gab", s2 * 2)])
                act(gab[s2 * 2 + 1], pgb, AF.Sigmoid, reads=[pgbk], writes=[("gab", s2 * 2 + 1)])
                tt("dve", t12[s2 * 2], pa, gab[s2 * 2], ALU.mult, reads=[("ps", 0 + s2 * 4), ("gab", s2 * 2)], writes=[("t12", s2 * 2)])
                tt("dve", t12[s2 * 2 + 1], pbb, gab[s2 * 2 + 1], ALU.mult, reads=[("ps", 1 + s2 * 4), ("gab", s2 * 2 + 1)], writes=[("t12", s2 * 2 + 1)])
                tt("pool", mT[:, m, tb * 512:(tb + 1) * 512], t12[s2 * 2], t12[s2 * 2 + 1], ALU.add,
                   reads=[("t12", s2 * 2), ("t12", s2 * 2 + 1)], writes=[("mT", tb)])
        if "mT" in dbg:
            dbg_out["mT"] = (mT, [128, 8 * NTOK], BF16, [("mT", i) for i in range(4)])

        S.barrier()
        H2 = R2
        xnb = A.view(H2, [128, 16, 1024], BF16)
        O5 = H2 + 32 * K
        Wo = A.view(O5, [128, 8, 1024], BF16)
        ln1gB = A.view(O5 + 16 * K, [128, 1024], F32)
        ln1bB = A.view(O5 + 20 * K, [128, 1024], F32)
        xs5 = [A.view(O5 + 24 * K + i * 4 * K, [128, 1024], F32) for i in range(2)]
        zt = [A.view(O5 + 32 * K + i * 4 * K, [128, 1024], F32) for i in range(2)]
        x1t = [A.view(O5 + 40 * K + i * 4 * K, [128, 1024], F32) for i in range(2)]
        wr_s = A.view(MOE_TMP, [128, 8, NEXP], F32)
        h2Tf = A.view(MOE_TMP + 6 * K, [128, 8, 128], F32)
        assert MOE_TMP + 10 * K + 512 <= R0_END
        dma("pool", Wo, wo_d.rearrange("(kc p) n -> p kc n", p=128), writes=["Wo"])
        dma("sp", ln1gB, bcast(ln1g_d[0:1, :]), writes=["ln1gB"])
        dma("sp", ln1bB, bcast(ln1b_d[0:1, :]), writes=["ln1bB"])
        dma("sp", wr_s, wr_d.rearrange("(kc p) n -> p kc n", p=128), writes=["wr_s"])
        dma("sp", brB, bcast(br_d[0:1, :]), writes=["brB"])
        def stage5A(ti):
            s2 = ti % 2
            dma("sp", xs5[s2], xo[ti * 128:(ti + 1) * 128, :], writes=[("xs5", s2)])
            for hh in range(2):
                bi = hh + 2 * s2
                pb = bank(bi)
                for kc in range(8):
                    mm(pb, mT[:, kc, ti * 128:(ti + 1) * 128], Wo[:, kc, hh * 512:(hh + 1) * 512], kc == 0, kc == 7,
                       reads=[("mT", ti // 4), "Wo"], writes=[("ps", bi)])
                tt("dve", zt[s2][:, hh * 512:(hh + 1) * 512], pb, G1B[:, hh * 512:(hh + 1) * 512], ALU.mult,
                   reads=[("ps", bi), ("GB", 0)], writes=[("zt", s2)])
            stt(zt[s2], xs5[s2], ALPHA, zt[s2], ALU.mult, ALU.add, reads=[("xs5", s2), ("zt", s2)], writes=[("zt", s2)])
            ln_stats(zt[s2], s2, 0, ("zt", s2))
            ln_finish(s2, 1)
            act(zt[s2], zt[s2], AF.Identity, reads=[("zt", s2), ("rstd", s2), ("nmr", s2)], writes=[("zt", s2)],
                bias=nmr_[s2][:, 0:1], scale=rstd_[s2][:, 0:1])
            tt("dve", zt[s2], zt[s2], ln1gB, ALU.mult, reads=[("zt", s2), "ln1gB"], writes=[("zt", s2)])
            tt("dve", x1t[s2], zt[s2], ln1bB, ALU.add, reads=[("zt", s2), "ln1bB"], writes=[("x1t", s2)])
            dma("sp", x1s_d[ti * 128:(ti + 1) * 128, :], x1t[s2], reads=[("x1t", s2)], writes=[("x1s", ti)])
            ln_stats(x1t[s2], s2, 1, ("x1t", s2))
            act(sd_[s2][:, 1:2], mv_[s2][:, 1:2, 1], AF.Ln, reads=[("mv", s2, 1)], writes=[("sd2", s2)], bias=eps_t, scale=1.0)
            act(rstd_[s2][:, 1:2], sd_[s2][:, 1:2], AF.Exp, reads=[("sd2", s2)], writes=[("rstd2", s2)], scale=-0.5)
            stt(nmr_[s2][:, 1:2], mv_[s2][:, 1:2, 0], -1.0, rstd_[s2][:, 1:2], ALU.mult, ALU.mult,
                reads=[("mv", s2, 1), ("rstd2", s2)], writes=[("nmr2", s2)])
            act(zt[s2], x1t[s2], AF.Identity, reads=[("x1t", s2), ("rstd2", s2), ("nmr2", s2)], writes=[("zt", s2)],
                bias=nmr_[s2][:, 1:2], scale=rstd_[s2][:, 1:2])
            cp("act", xnb[:, ti, :], zt[s2], reads=[("zt", s2)], writes=[("xnb", ti)])

        def stage5B(ti):
            s2 = ti % 2
            for hb in range(2):
                bi = 4 + hb
                pT = bank(bi, [128, 4, 128])
                for k4 in range(4):
                    kc = hb * 4 + k4
                    tr(pT[:, k4, :], zt[s2][:, kc * 128:(kc + 1) * 128], ident_f, reads=[("zt", s2), "ident_f"], writes=[("ps", bi)])
                for k4 in range(4):
                    kc = hb * 4 + k4
                    evac_mod(h2Tf[:, kc, :], pT[:, k4, :], opsc2[:, kc:kc + 1], sh2[:, kc:kc + 1],
                             reads=[("ps", bi), "opsc2", "modT"], writes=[("h2Tf", kc)], eng=("act" if hb == 0 else "dve"))
            pR = bank(6)[:, 0:NEXP]
            for kc in range(8):
                mm(pR, h2Tf[:, kc, :], wr_s[:, kc, :], kc == 0, kc == 7, reads=[("h2Tf", kc), "wr_s"], writes=[("ps", 6)])
            tt("dve", lg_[s2], pR, brB, ALU.add, reads=[("ps", 6), "brB"], writes=[("lg", s2)])
            S.add("dve", lambda h, s2=s2: h.max(out=mx8_[s2], in_=lg_[s2]), reads=[("lg", s2)], writes=[("mx8", s2)])
            ts("dve", msk_[s2], lg_[s2], mx8_[s2][:, 3:4], None, ALU.is_ge, None, reads=[("lg", s2), ("mx8", s2)], writes=[("msk", s2)])
            ts("dve", negm_[s2], mx8_[s2][:, 0:1], -1.0, None, ALU.mult, None, reads=[("mx8", s2)], writes=[("negm", s2)])
            act(ex_[s2], lg_[s2], AF.Exp, reads=[("lg", s2), ("negm", s2)], writes=[("ex", s2)], bias=negm_[s2][:, 0:1], scale=1.0)
            tt("dve", em_[s2], ex_[s2], msk_[s2], ALU.mult, reads=[("ex", s2), ("msk", s2)], writes=[("em", s2)])
            S.add("dve", lambda h, s2=s2: h.reduce_sum(out=den_[s2], in_=em_[s2], axis=AX.X), reads=[("em", s2)], writes=[("den", s2)])
            S.add("dve", lambda h, s2=s2: h.reciprocal(out=rden_[s2], in_=den_[s2]), reads=[("den", s2)], writes=[("rden", s2)])
            ts("dve", gates_all[:, ti, :], em_[s2], rden_[s2][:, 0:1], None, ALU.mult, None, reads=[("em", s2), ("rden", s2)], writes=[("gates", ti)])
            S.add("dve", lambda h, s2=s2: h.max_index(out=mi8_[s2], in_max=mx8_[s2], in_values=lg_[s2]), reads=[("lg", s2), ("mx8", s2)], writes=[("mi8", s2)])
            cp("dve", E4f[:, ti, :], mi8_[s2][:, 0:4], reads=[("mi8", s2)], writes=[("E4f", ti)])
            act(e4_[s2], mx8_[s2][:, 0:4], AF.Exp, reads=[("mx8", s2), ("negm", s2)], writes=[("e4", s2)], bias=negm_[s2][:, 0:1], scale=1.0)
            ts("dve", W4[:, ti, :], e4_[s2], rden_[s2][:, 0:1], None, ALU.mult, None, reads=[("e4", s2), ("rden", s2)], writes=[("W4", ti)])
            cp("pool", Mall[:, ti, :], msk_[s2], reads=[("msk", s2)], writes=[("Mall", ti)])

        stage5A(0)
        for ti in range(16):
            if ti + 1 < 16:
                S.zipped(lambda: stage5A(ti + 1), lambda: stage5B(ti))
            else:
                stage5B(ti)
        if "x1" in dbg:
            dbg_out["x1"] = None
        if "gates" in dbg:
            dbg_out["gates"] = (gates_all, [128, 16 * NEXP], F32, [("gates", i) for i in range(16)])

    if stop_after in (None, "moe", "route"):
        S.barrier()
        RT0 = R5
        within = A.view(RT0, [128, 16, NEXP], F32)
        tot = A.view(RT0 + 2 * K, [128, 16, NEXP], F32)
        base = A.view(RT0 + 4 * K, [128, 16, NEXP], F32)
        dall = A.view(RT0 + 6 * K, [128, 16, NEXP], F32)
        big1 = A.view(RT0 + 8 * K, [128, NEXP, NEXP], F32)
        big2 = A.view(RT0 + 12 * K, [128, NEXP, NEXP], F32)
        LTm = A.view(RT0 + 16 * K, [128, NEXP, NEXP], F32)
        oh = A.view(RT0 + 20 * K, [128, 16, NEXP], F32)
        U_f = A.view(RT0 + 22 * K, [128, 128], F32)
        U_b = A.view(RT0 + 22 * K + 512, [128, 128], BF16)
        on_b = A.view(RT0 + 22 * K + 768, [128, 128], BF16)
        iotaE = A.view(RT0 + 23 * K, [128, NEXP], F32)
        cnt = A.view(RT0 + 23 * K + 128, [128, NEXP], F32)
        rank = A.view(RT0 + 23 * K + 256, [128, NEXP], F32)
        kbv = A.view(RT0 + 23 * K + 384, [128, NEXP], F32)
        sig = A.view(RT0 + 23 * K + 512, [128, NEXP], F32)
        kbtab = A.view(RT0 + 23 * K + 640, [128, NEXP], F32)
        pk = A.view(RT0 + 23 * K + 768, [128, 8], F32)
        destF = A.view(RT0 + 24 * K, [128, 4, 16], F32)
        idxWf = A.view(RT0 + 24 * K + 256, [128, NEXP, 8], F32)
        rankT = A.view(RT0 + 26 * K, [NEXP, 1], F32, parts=NEXP)
        RTm = A.view(RT0 + 26 * K + 64, [NEXP, NEXP], F32, parts=NEXP)
        bgrow = A.view(RT0 + 27 * K, [NEXP, 1024], F32, parts=NEXP)
        burow = A.view(RT0 + 31 * K, [NEXP, 1024], F32, parts=NEXP)
        dma("sp", kbtab, kbtab_d, writes=["kbtab"])
        dma("sp", bgrow, bg_d, writes=["bgrow"])
        dma("sp", burow, bu_d, writes=["burow"])
        memset("pool", U_f, 1.0, writes=["U_f"])
        S.add("pool", lambda h: h.affine_select(out=U_f, in_=U_f, pattern=[[1, 128]], compare_op=ALU.is_gt, fill=0.0, base=0, channel_multiplier=-1),
              reads=["U_f"], writes=["U_f"])
        cp("pool", U_b, U_f, reads=["U_f"], writes=["U_b"])
        memset("pool", on_b, 1.0, writes=["on_b"])
        S.add("pool", lambda h: h.iota(iotaE, pattern=[[1, NEXP]], base=0, channel_multiplier=0, allow_small_or_imprecise_dtypes=True), writes=["iotaE"])
        S.add("pool", lambda h: h.iota(pk, pattern=[[128, 8]], base=0, channel_multiplier=1, allow_small_or_imprecise_dtypes=True), writes=["pk"])
        memset("pool", LTm, 1.0, writes=["LTm"])
        S.add("pool", lambda h: h.affine_select(out=LTm, in_=LTm, pattern=[[1, NEXP], [-1, NEXP]], compare_op=ALU.is_gt, fill=0.0, base=0, channel_multiplier=0),
              reads=["LTm"], writes=["LTm"])
        Mkeys = [("Mall", i) for i in range(16)]
        Mflat = Mall.rearrange("p a b -> p (a b)")
        mm(bank(0), U_b, Mflat, True, True, reads=["U_b"] + Mkeys, writes=[("ps", 0)])
        mm(bank(1), on_b, Mflat, True, True, reads=["on_b"] + Mkeys, writes=[("ps", 1)])
        cp("dve", within.rearrange("p a b -> p (a b)"), bank(0), reads=[("ps", 0)], writes=["within"])
        cp("act", tot.rearrange("p a b -> p (a b)"), bank(1), reads=[("ps", 1)], writes=["tot"])
        memset("dve", base[:, 0, :], 0.0, writes=["base"])
        for ti in range(1, 16):
            tt("dve", base[:, ti, :], base[:, ti - 1, :], tot[:, ti - 1, :], ALU.add, reads=["base", "tot"], writes=["base"])
        tt("dve", cnt, base[:, 15, :], tot[:, 15, :], ALU.add, reads=["base", "tot"], writes=["cnt"])
        cA = cnt.unsqueeze(1).broadcast_to([128, NEXP, NEXP])
        cB = cnt.unsqueeze(2).broadcast_to([128, NEXP, NEXP])
        tt("dve", big1, cA, cB, ALU.is_gt, reads=["cnt"], writes=["big1"])
        tt("dve", big2, cA, cB, ALU.is_equal, reads=["cnt"], writes=["big2"])
        tt("dve", big2, big2, LTm, ALU.mult, reads=["big2", "LTm"], writes=["big2"])
        tt("dve", big1, big1, big2, ALU.add, reads=["big1", "big2"], writes=["big1"])
        S.add("dve", lambda h: h.reduce_sum(out=rank, in_=big1, axis=AX.X), reads=["big1"], writes=["rank"])
        rA = rank.unsqueeze(2).broadcast_to([128, NEXP, NEXP])
        iB = iotaE.unsqueeze(1).broadcast_to([128, NEXP, NEXP])
        tt("dve", big1, rA, iB, ALU.is_equal, reads=["rank", "iotaE"], writes=["big1"])
        tt("dve", big2, big1, kbtab.unsqueeze(1).broadcast_to([128, NEXP, NEXP]), ALU.mult, reads=["big1", "kbtab"], writes=["big2"])
        S.add("dve", lambda h: h.reduce_sum(out=kbv, in_=big2, axis=AX.X), reads=["big2"], writes=["kbv"])
        tt("dve", big1, rank.unsqueeze(1).broadcast_to([128, NEXP, NEXP]), iotaE.unsqueeze(2).broadcast_to([128, NEXP, NEXP]), ALU.is_equal,
           reads=["rank", "iotaE"], writes=["big1"])
        tt("dve", big2, big1, iB, ALU.mult, reads=["big1", "iotaE"], writes=["big2"])
        S.add("dve", lambda h: h.reduce_sum(out=sig, in_=big2, axis=AX.X), reads=["big2"], writes=["sig"])
        tt("dve", dall, base, within, ALU.add, reads=["base", "within"], writes=["dall"])
        tt("dve", dall, dall, kbv.unsqueeze(1).broadcast_to([128, 16, NEXP]), ALU.add, reads=["dall", "kbv"], writes=["dall"])
        E4keys = [("E4f", i) for i in range(16)]
        for k in range(4):
            tt("dve", oh, iotaE.unsqueeze(1).broadcast_to([128, 16, NEXP]), E4f[:, :, k:k + 1].broadcast_to([128, 16, NEXP]), ALU.is_equal,
               reads=["iotaE"] + E4keys, writes=["oh"])
            tt("dve", oh, oh, dall, ALU.mult, reads=["oh", "dall"], writes=["oh"])
            S.add("dve", lambda h, k=k: h.reduce_sum(out=destF[:, k, :], in_=oh, axis=AX.X), reads=["oh"], writes=["destF"])
        cp("dve", destI, destF, reads=["destF"], writes=["destI"])
        ts("dve", sig, sig, 1024.0, None, ALU.mult, None, reads=["sig"], writes=["sig"])
        tt("dve", idxWf, sig.unsqueeze(2).broadcast_to([128, NEXP, 8]), pk.unsqueeze(1).broadcast_to([128, NEXP, 8]), ALU.add,
           reads=["sig", "pk"], writes=["idxWf"])
        cp("dve", idxW, idxWf, reads=["idxWf"], writes=["idxW"])
        pRk = bank(2)[0:NEXP, 0:128]
        tr(pRk, rank, ident_f, reads=["rank", "ident_f"], writes=[("ps", 2)])
        cp("act", rankT, pRk[:, 0:1], reads=[("ps", 2)], writes=["rankT"])
        ts("dve", RTm, iotaE[0:NEXP, :], rankT[:, 0:1], None, ALU.is_equal, None, reads=["iotaE", "rankT"], writes=["RTm"])
        for bi_, (rows, rkey, dstR, dkey) in enumerate(((bgrow, "bgrow", bgR, "bgR"), (burow, "burow", bu1R, "bu1R"))):
            pbk = bank(3 + bi_)
            for c in range(8):
                mm(pbk[:, c * NEXP:(c + 1) * NEXP], rows[:, c * 128:(c + 1) * 128], RTm, True, True, reads=[rkey, "RTm"], writes=[("ps", 3 + bi_)])
            cp("act", dstR.rearrange("p a b -> p (a b)"), pbk[:, 0:8 * NEXP], reads=[("ps", 3 + bi_)], writes=[dkey])
        ts("pool", bu1R, bu1R, 1.0, None, ALU.add, None, reads=["bu1R"], writes=["bu1R"])
        if "route" in dbg:
            dbg_out["gates"] = (gates_all, [128, 16 * NEXP], F32, [("gates", i) for i in range(16)])
            dbg_out["destI"] = (destI, [128, 64], I32, ["destI"])
            dbg_out["idxW"] = (idxW, [128, NEXP * 8], I32, ["idxW"])
            dbg_out["rank"] = (rank, [128, NEXP], F32, ["rank"])
            dbg_out["W4"] = (W4, [128, 64], F32, [("W4", i) for i in range(16)])
            dbg_out["bgR"] = (bgR, [128, 8 * NEXP], F32, ["bgR"])

    if stop_after is None or stop_after == "moe":
        NWB = 6
        wbufs = [A.view(R2 + i * 16 * K, [128, 8, 1024], BF16) for i in range(NWB)]
        tmp_g = [A.view(MOE_TMP + i * 2 * K, [128, 512], F32) for i in range(2)]
        tmp_s = [A.view(MOE_TMP + 4 * K + i * 2 * K, [128, 512], F32) for i in range(2)]
        tmp_u = [A.view(MOE_TMP + 8 * K + i * 2 * K, [128, 512], F32) for i in range(2)]
        assert MOE_TMP + 12 * K <= R0_END, (MOE_TMP, R0_END)
        wn = [2]

        def load_w(src, r):
            i = wn[0] % NWB
            wn[0] += 1
            rows = src.rearrange("e k n -> (e k) n")
            for kc in range(8):
                S.add("pool", lambda h, i=i, kc=kc: h.indirect_dma_start(
                    out=wbufs[i][:, kc, :], out_offset=None, in_=rows[:, :],
                    in_offset=bass.IndirectOffsetOnAxis(ap=idxW[:, r, kc:kc + 1], axis=0)),
                    reads=["idxW"], writes=[("wb", i, kc)], dma=True)
            return wbufs[i], i

        def load_rank(r):
            return (load_w(wg_d, r), load_w(wu_d, r), load_w(wd_d, r))

        W_rank0 = load_rank(0)
        for ti in range(16):
            for k in range(4):
                S.add("pool", lambda h, ti=ti, k=k: h.indirect_dma_start(
                    out=xs_scr[:, :], out_offset=bass.IndirectOffsetOnAxis(ap=destI[:, k, ti:ti + 1], axis=0),
                    in_=xnb[:, ti, :], in_offset=None), reads=["destI", ("xnb", ti)], writes=[("xs_scr", ti, k)], dma=True)
        S.barrier()
        xrow = [A.view(R5 + i * 8 * K, [128, 4, 1024], BF16) for i in range(2)]
        yrow = [A.view(R5 + 16 * K + i * 4 * K, [128, 1024], F32) for i in range(2)]
        xeT = [A.view(R1 + i * 8 * K, [128, 8, 512], BF16) for i in range(2)] + [A.view(R5 + 24 * K, [128, 8, 512], BF16)]
        actT = [A.view(R1 + 16 * K + i * 8 * K, [128, 8, 512], BF16) for i in range(2)]
        itc = [0]
        ytile_n = [0]

        def blk_load(blk):
            r, sb_, bidx, W = blk
            n = min(512, KCAP[r] - sb_ * 512)
            nt = n // 128
            row0 = KBASE[r] + sb_ * 512
            xr = bidx % 2
            dma("sp", xrow[xr][:, 0:nt, :], xs_scr[row0:row0 + n, :].rearrange("(t p) d -> p t d", p=128),
                reads=["xs_scr"], writes=[("xrow", xr)])

        def blk_T(blk):
            r, sb_, bidx, W = blk
            n = min(512, KCAP[r] - sb_ * 512)
            nt = n // 128
            xr, x3 = bidx % 2, bidx % 3
            for kg in range(2):
                pT = psum_t[:, 4 * 512:6 * 512].bitcast(BF16).rearrange("p (a b) -> p a b", a=4)
                for t in range(nt):
                    for k4 in range(4):
                        kc = kg * 4 + k4
                        tr(pT[:, k4, t * 128:(t + 1) * 128], xrow[xr][:, t, kc * 128:(kc + 1) * 128], ident_b,
                           reads=[("xrow", xr), "ident_b"], writes=[("ps", 4), ("ps", 5)])
                for k4 in range(4):
                    kc = kg * 4 + k4
                    evac_mod(xeT[x3][:, kc, 0:n], pT[:, k4, 0:n], opsc2[:, kc:kc + 1], sh2[:, kc:kc + 1],
                             reads=[("ps", 4), ("ps", 5), "opsc2", "modT"], writes=[("xeT", x3, kc)], eng=("act" if k4 < 2 else "dve"))

        def blk_GU(blk):
            r, sb_, bidx, W = blk
            (Wg_, Wgk), (Wu_, Wuk), (Wd_, Wdk) = W
            n = min(512, KCAP[r] - sb_ * 512)
            x3, xs2 = bidx % 3, bidx % 2
            for c in range(8):
                s2 = itc[0] % 2
                itc[0] += 1
                bg_i, bu_i = 0 + s2, 2 + s2
                pg, pu = bank(bg_i)[:, 0:n], bank(bu_i)[:, 0:n]
                for kc in range(8):
                    mm(pg, Wg_[:, kc, c * 128:(c + 1) * 128], xeT[x3][:, kc, 0:n], kc == 0, kc == 7,
                       reads=[("wb", Wgk, kc), ("xeT", x3, kc)], writes=[("ps", bg_i)])
                for kc in range(8):
                    mm(pu, Wu_[:, kc, c * 128:(c + 1) * 128], xeT[x3][:, kc, 0:n], kc == 0, kc == 7,
                       reads=[("wb", Wuk, kc), ("xeT", x3, kc)], writes=[("ps", bu_i)])
                tg, tsg, tu = tmp_g[s2][:, 0:n], tmp_s[s2][:, 0:n], tmp_u[s2][:, 0:n]
                ts("dve", tg, pg, bgR[:, c, r:r + 1], 7.0, ALU.add, ALU.min, reads=[("ps", bg_i), "bgR"], writes=[("tg", s2)])
                act(tsg, tg, AF.Sigmoid, reads=[("tg", s2)], writes=[("tsg", s2)], scale=1.702)
                act(tu, pu, AF.Identity, reads=[("ps", bu_i), "bu1R"], writes=[("tu", s2)], bias=bu1R[:, c, r:r + 1], scale=1.0)
                ts("dve", tu, tu, 8.0, -6.0, ALU.min, ALU.max, reads=[("tu", s2)], writes=[("tu", s2)])
                tt("dve", tsg, tg, tsg, ALU.mult, reads=[("tg", s2), ("tsg", s2)], writes=[("tsg", s2)])
                tt("dve", actT[xs2][:, c, 0:n], tsg, tu, ALU.mult, reads=[("tsg", s2), ("tu", s2)], writes=[("actT", xs2)])

        def blk_back(blk):
            r, sb_, bidx, W = blk
            xs2 = bidx % 2
            (Wg_, Wgk), (Wu_, Wuk), (Wd_, Wdk) = W
            n = min(512, KCAP[r] - sb_ * 512)
            nt = n // 128
            row0 = KBASE[r] + sb_ * 512
            for t in range(nt):
                ys2 = ytile_n[0] % 2
                ytile_n[0] += 1
                for hh in range(2):
                    bi = 6 + hh
                    py = bank(bi)
                    for c in range(8):
                        mm(py, actT[xs2][:, c, t * 128:(t + 1) * 128], Wd_[:, c, hh * 512:(hh + 1) * 512], c == 0, c == 7,
                           reads=[("actT", xs2), ("wb", Wdk, c)], writes=[("ps", bi)])
                    cp("act" if hh == 0 else "dve", yrow[ys2][:, hh * 512:(hh + 1) * 512], py, reads=[("ps", bi)], writes=[("yrow", ys2, hh)])
                dma("sp", ys_scr[row0 + t * 128:row0 + (t + 1) * 128, :], yrow[ys2], reads=[("yrow", ys2, 0), ("yrow", ys2, 1)], writes=[("ys_scr", ytile_n[0])])

        blist = []
        for r in range(n_experts):
            for sb_ in range((KCAP[r] + 511) // 512):
                blist.append([r, sb_, len(blist), None])
        NBLK = len(blist)
        Wsets = {0: W_rank0}

        def attach_w(bi_):
            r = blist[bi_][0]
            blist[bi_][3] = Wsets[r]

        blk_load(blist[0])
        if NBLK > 1:
            blk_load(blist[1])
        blk_T(blist[0])
        if NBLK > 2:
            blk_load(blist[2])
        if NBLK > 1:
            blk_T(blist[1])
        attach_w(0)
        blk_GU(blist[0])
        if 1 < n_experts:
            Wsets[1] = load_rank(1)
        for bi_ in range(NBLK):
            if bi_ + 3 < NBLK:
                blk_load(blist[bi_ + 3])
            if bi_ + 2 < NBLK:
                blk_T(blist[bi_ + 2])
            if bi_ + 1 < NBLK:
                attach_w(bi_ + 1)
                blk_GU(blist[bi_ + 1])
            blk_back(blist[bi_])
            if bi_ + 1 < NBLK and blist[bi_ + 1][1] == 0:
                r1 = blist[bi_ + 1][0]
                if r1 + 1 < n_experts and (r1 + 1) not in Wsets:
                    Wsets[r1 + 1] = load_rank(r1 + 1)
        S.barrier()
        F7 = R2
        ln2gB = A.view(F7, [128, 1024], F32)
        ln2bB = A.view(F7 + 4 * K, [128, 1024], F32)
        x1r = [A.view(F7 + 8 * K + i * 4 * K, [128, 1024], F32) for i in range(2)]
        z2 = [A.view(F7 + 16 * K + i * 4 * K, [128, 1024], F32) for i in range(2)]
        ot = [A.view(F7 + 24 * K + i * 4 * K, [128, 1024], F32) for i in range(2)]
        acct = [A.view(F7 + 32 * K + i * 4 * K, [128, 1024], F32) for i in range(2)]
        ygt = [A.view(F7 + 40 * K + i * 4 * K, [128, 1024], F32) for i in range(4)]
        bdn_s = A.view(F7 + 56 * K, [NEXP, 1024], F32, parts=NEXP)
        gT_s = [A.view(F7 + 60 * K + i * 512, [NEXP, 128], F32, parts=NEXP) for i in range(2)]
        dma("sp", ln2gB, bcast(ln2g_d[0:1, :]), writes=["ln2gB"])
        dma("sp", ln2bB, bcast(ln2b_d[0:1, :]), writes=["ln2bB"])
        dma("sp", bdn_s, bdn_d, writes=["bdn_s"])
        outs = []
        gi = 0
        gi_ = [0]
        def fin_A(ti):
            s2 = ti % 2
            dma("sp", x1r[s2], x1s_d[ti * 128:(ti + 1) * 128, :], reads=[("x1s", ti)], writes=[("x1r", s2)])
            pG = bank(4 + s2)[0:NEXP, 0:128]
            tr(pG, gates_all[:, ti, :], ident_f, reads=[("gates", ti), "ident_f"], writes=[("ps", 4 + s2)])
            cp("act", gT_s[s2], pG, reads=[("ps", 4 + s2)], writes=[("gT_s", s2)])
            for hh in range(2):
                bi = hh + 2 * s2
                pb = bank(bi)
                mm(pb, gT_s[s2], bdn_s[:, hh * 512:(hh + 1) * 512], True, True, reads=[("gT_s", s2), "bdn_s"], writes=[("ps", bi)])
                cp("act", acct[s2][:, hh * 512:(hh + 1) * 512], pb, reads=[("ps", bi)], writes=[("acct", s2)])
            for k in range(4):
                g4 = gi_[0] % 4
                gi_[0] += 1
                S.add("pool", lambda h, ti=ti, k=k, g4=g4: h.indirect_dma_start(
                    out=ygt[g4][:, :], out_offset=None, in_=ys_scr[:, :],
                    in_offset=bass.IndirectOffsetOnAxis(ap=destI[:, k, ti:ti + 1], axis=0)),
                    reads=["destI"], writes=[("ygt", g4)], dma=True)
                stt(acct[s2], ygt[g4], W4[:, ti, k:k + 1], acct[s2], ALU.mult, ALU.add,
                    reads=[("ygt", g4), ("W4", ti), ("acct", s2)], writes=[("acct", s2)])

        def fin_B(ti):
            s2 = ti % 2
            tt("dve", z2[s2], acct[s2], G2B, ALU.mult, reads=[("acct", s2), ("GB", 1)], writes=[("z2", s2)])
            stt(z2[s2], x1r[s2], ALPHA, z2[s2], ALU.mult, ALU.add, reads=[("x1r", s2), ("z2", s2)], writes=[("z2", s2)])
            ln_stats(z2[s2], s2, 0, ("z2", s2))
            ln_finish(s2, 1)
            act(z2[s2], z2[s2], AF.Identity, reads=[("z2", s2), ("rstd", s2), ("nmr", s2)], writes=[("z2", s2)],
                bias=nmr_[s2][:, 0:1], scale=rstd_[s2][:, 0:1])
            tt("dve", z2[s2], z2[s2], ln2gB, ALU.mult, reads=[("z2", s2), "ln2gB"], writes=[("z2", s2)])
            tt("dve", ot[s2], z2[s2], ln2bB, ALU.add, reads=[("z2", s2), "ln2bB"], writes=[("ot", s2)])
            dma("sp", out_d[ti * 128:(ti + 1) * 128, :], ot[s2], reads=[("ot", s2)], writes=[("out", ti)])
            outs.append(("out", ti))

        fin_A(0)
        for ti in range(16):
            if ti + 1 < 16:
                S.zipped(lambda: fin_A(ti + 1), lambda: fin_B(ti))
            else:
                fin_B(ti)
        S.add("sp", None, reads=outs)

    dkeys = []
    for name, spec in dbg_out.items():
        if spec is None:
            continue
        ap, shape, dt, keys = spec
        dd = nc.dram_tensor("dbg_" + name, shape, dt, kind="ExternalOutput").ap()
        flat = ap
        if len(ap.shape) == 3:
            flat = ap.rearrange("p a b -> p (a b)")
        dma("sp", dd, flat, reads=keys, writes=[("dbg", name)])
        dkeys.append(("dbg", name))
    if dkeys:
        S.add("sp", None, reads=dkeys)
    S.emit()
    st.close()
    return nc


def t5_bucket_np(dist):
    d = dist.astype(np.float32)
    large = np.float32(16) + np.log(np.maximum(d, np.float32(16.0)) / np.float32(16)) / np.float32(math.log(2048 / 16)) * np.float32(16)
    large = np.minimum(large.astype(np.int32), 31)
    return np.where(dist < 16, dist, large)


def host_layout(inp):
    f = np.float32
    x = np.asarray(inp["x"], f)
    shared = {}
    shared["w_ada"] = np.ascontiguousarray(np.asarray(inp["w_ada"], f)[0])
    shared["b_adaT"] = np.ascontiguousarray(np.asarray(inp["b_ada"], f)[0].reshape(48, 128).T)
    shared["w_in"] = np.ascontiguousarray(np.asarray(inp["w_in"], f)[0])
    shared["gm_g"] = np.asarray(inp["gm_ln_g"], f).reshape(1, 512)
    shared["gm_b"] = np.asarray(inp["gm_ln_b"], f).reshape(1, 512)
    ws = np.asarray(inp["gm_w_s"], f)[0]
    shared["wsT"] = np.ascontiguousarray(ws.transpose(2, 0, 1).reshape(128, 8 * 128))
    bs = np.asarray(inp["gm_b_s"], f)[0]
    bsp = np.empty((128, 4, 128), f)
    for gp in range(4):
        bsp[0:64, gp, :] = bs[2 * gp][None, :]
        bsp[64:128, gp, :] = bs[2 * gp + 1][None, :]
    shared["bsp"] = bsp.reshape(128, 512)
    shared["wa"] = np.ascontiguousarray(np.asarray(inp["w_branch_a"], f)[0])
    shared["wb"] = np.ascontiguousarray(np.asarray(inp["w_branch_b"], f)[0])
    shared["wo"] = np.ascontiguousarray(np.asarray(inp["w_out"], f)[0])
    rel = np.asarray(inp["rel_bias"], f)
    kk = np.arange(128)[:, None]
    qq = np.arange(128)[None, :]
    d_prev = qq + 128 - kk
    d_cur = qq - kk
    biasT = np.empty((128, 24, 2, 128), f)
    for g in range(3):
        bp = t5_bucket_np(np.clip(d_prev, 0, None).astype(np.int32) * DIL[g])
        bc = t5_bucket_np(np.clip(d_cur, 0, None).astype(np.int32) * DIL[g])
        for h in range(8):
            biasT[:, g * 8 + h, 0, :] = rel[bp, g * 8 + h]
            biasT[:, g * 8 + h, 1, :] = rel[bc, g * 8 + h]
    shared["biasT"] = biasT.reshape(128, 24 * 256)
    maskT = np.zeros((128, 2, 128), f)
    maskT[:, 0, :][d_prev > 128] = -1e30
    maskT[:, 1, :][d_cur < 0] = -1e30
    shared["maskT"] = maskT.reshape(128, 256)
    shared["ln1g"] = np.asarray(inp["ln1_g"], f).reshape(1, D)
    shared["ln1b"] = np.asarray(inp["ln1_b"], f).reshape(1, D)
    shared["wr"] = np.ascontiguousarray(np.asarray(inp["w_router"], f)[0])
    shared["br"] = np.asarray(inp["b_router"], f).reshape(1, NEXP)
    shared["wg"] = np.ascontiguousarray(np.asarray(inp["w_gate"], f)[0])
    shared["wu"] = np.ascontiguousarray(np.asarray(inp["w_up"], f)[0])
    shared["wd"] = np.ascontiguousarray(np.asarray(inp["w_down"], f)[0])
    shared["bg"] = np.ascontiguousarray(np.asarray(inp["b_gate"], f)[0])
    shared["bu"] = np.ascontiguousarray(np.asarray(inp["b_up"], f)[0])
    shared["kbtab"] = np.broadcast_to(np.asarray(KBASE, f)[None, :], (128, NEXP)).copy()
    shared["bdn"] = np.ascontiguousarray(np.asarray(inp["b_down"], f)[0])
    shared["ln2g"] = np.asarray(inp["ln2_g"], f).reshape(1, D)
    shared["ln2b"] = np.asarray(inp["ln2_b"], f).reshape(1, D)
    shared["ident"] = np.eye(128, dtype=f)
    c = np.asarray(inp["c"], f)
    in_maps = []
    for k in range(NCORES):
        b, hf = k // 2, k % 2
        m = dict(shared)
        m["xo"] = np.ascontiguousarray(x[b, hf * NTOK:(hf + 1) * NTOK])
        m["xc"] = np.ascontiguousarray(x[b, 0:NTOK]) if hf == 1 else np.zeros((NTOK, D), f)
        m["flag"] = np.full((128, 1), float(hf), f)
        m["cT"] = np.ascontiguousarray(c[b].reshape(8, 128).T)
        in_maps.append(m)
    return in_maps


_NC_CACHE = {}


def kernel(**inputs):
    in_maps = host_layout(inputs)
    if "nc" not in _NC_CACHE:
        _NC_CACHE["nc"] = build()
    nc = _NC_CACHE["nc"]
    res = run_bass_kernel_spmd(nc, in_maps, core_ids=list(range(NCORES)))
    out = np.empty((4, 4096, D), np.float32)
    for k in range(NCORES):
        b, hf = k // 2, k % 2
        out[b, hf * NTOK:(hf + 1) * NTOK] = np.asarray(res.results[k]["out"], np.float32)
    return out
```

```python
import contextlib
import math
import numpy as np
import concourse.bass as bass
import concourse.mybir as mybir
from concourse.bass_utils import run_bass_kernel_spmd

F32 = mybir.dt.float32
BF16 = mybir.dt.bfloat16
I32 = mybir.dt.int32
U32 = mybir.dt.uint32
AF = mybir.ActivationFunctionType
ALU = mybir.AluOpType
AX = mybir.AxisListType

D = 1024
NTOK = 2048
NCORES = 8
NEXP = 32
DIL = (1, 4, 16)
ALPHA = 2.0 ** 0.25
EPS = 1e-5
IN_COLS = 7680
import os
PIPE_ATTN = os.environ.get('PIPE_ATTN', '1') == '1'
ZIP_STAGES = os.environ.get('ZIP_STAGES', '1') == '1'
KCAP = [((min(2048, 8192 // (r + 1)) + 127) // 128) * 128 for r in range(32)]
KBASE = [sum(KCAP[:r]) for r in range(32)]
NSLOT = sum(KCAP)

ENGINES = ("pe", "act", "dve", "pool", "sp")
COMPUTE = ("pe", "act", "dve", "pool")
HANDLES = {"pe": "tensor", "act": "scalar", "dve": "vector", "pool": "gpsimd", "sp": "sync"}


class Sched:
    NDMASEM = 12
    SAME_ENG_WINDOW = 3

    def __init__(self, nc):
        self.nc = nc
        self.ops = {e: [] for e in ENGINES}
        self.lastw = {}
        self.readers = {}
        self.capture = None

    def add(self, eng, fn, reads=(), writes=(), dma=False):
        if self.capture is not None:
            self.capture.append((eng, fn, tuple(reads), tuple(writes), dma))
            return None
        ops = self.ops[eng]
        idx = len(ops)
        deps = {}
        dmadeps = set()

        def anydep(p):
            if p == (eng, idx):
                return
            if self.ops[p[0]][p[1]]["dma"]:
                dmadeps.add(p)
            elif deps.get(p[0], -1) < p[1]:
                deps[p[0]] = p[1]

        for k in reads:
            w = self.lastw.get(k)
            if w is not None:
                anydep(w)
        for k in writes:
            w = self.lastw.get(k)
            if w is not None:
                anydep(w)
            for r in self.readers.get(k, ()):
                anydep(r)
        for k in reads:
            self.readers.setdefault(k, []).append((eng, idx))
        for k in writes:
            self.lastw[k] = (eng, idx)
            self.readers[k] = []
        if eng in deps and not dma:
            if eng == "pe" or deps[eng] < idx - self.SAME_ENG_WINDOW:
                del deps[eng]
        ops.append(dict(fn=fn, deps=deps, dmadeps=sorted(dmadeps), dma=dma, marked=False))
        return (eng, idx)

    def zipped(self, *stage_fns):
        if not ZIP_STAGES:
            for f in stage_fns:
                f()
            return
        lists = []
        for f in stage_fns:
            self.capture = []
            f()
            lists.append(self.capture)
            self.capture = None
        items = []
        for li, l in enumerate(lists):
            n = len(l)
            for i, op in enumerate(l):
                items.append(((i + 0.5) / max(n, 1), li, i, op))
        items.sort(key=lambda x: (x[0], x[1], x[2]))
        for _, _, _, op in items:
            self.add(*op)

    def barrier(self):
        last = {}
        for e in COMPUTE:
            for i in range(len(self.ops[e]) - 1, -1, -1):
                op = self.ops[e][i]
                if op["fn"] is not None and not op["dma"]:
                    last[e] = i
                    break
        dmas = []
        for e in ENGINES:
            seen = 0
            for i in range(len(self.ops[e]) - 1, -1, -1):
                if self.ops[e][i]["dma"]:
                    dmas.append((e, i))
                    seen += 1
                    if seen >= self.NDMASEM:
                        break
        for e in ENGINES:
            if not self.ops[e]:
                continue
            deps = {p: i for p, i in last.items() if p != e}
            self.ops[e].append(dict(fn=None, deps=deps, dmadeps=sorted(dmas), dma=False, marked=False))
        self.lastw = {}
        self.readers = {}

    def emit(self):
        nc = self.nc
        ops = self.ops
        for e in ENGINES:
            for op in ops[e]:
                for pe_, pi in op["deps"].items():
                    ops[pe_][pi]["marked"] = True
        for e in COMPUTE:
            c = 0
            for op in ops[e]:
                if op["dma"] or op["fn"] is None:
                    op["cnt"] = c
                    continue
                if op["marked"]:
                    c += 1
                op["cnt"] = c
        stack = contextlib.ExitStack()
        csem = {e: stack.enter_context(nc.semaphore("cs_" + e)) for e in COMPUTE}
        for e in ENGINES:
            n = 0
            ring = None
            for op in ops[e]:
                if not op["dma"]:
                    continue
                if ring is None:
                    ring = [stack.enter_context(nc.semaphore("ds_%s_%d" % (e, i))) for i in range(self.NDMASEM)]
                s = n % self.NDMASEM
                k = n // self.NDMASEM
                op["dsem"] = ring[s]
                op["dsem_id"] = (e, s)
                op["dtarget"] = 16 * (k + 1)
                op["dprev"] = 16 * k
                n += 1
        block = stack.enter_context(nc.Block())

        def make(e):
            def body(h):
                waited = {}

                def wait(key, sem, val):
                    if val <= 0 or waited.get(key, 0) >= val:
                        return
                    waited[key] = val
                    h.wait_ge(sem, val)

                for op in ops[e]:
                    for pe_, pi in op["deps"].items():
                        wait(("c", pe_), csem[pe_], ops[pe_][pi]["cnt"])
                    for (qe, qi) in op["dmadeps"]:
                        q = ops[qe][qi]
                        wait(("d",) + q["dsem_id"], q["dsem"], q["dtarget"])
                    if op["fn"] is None:
                        continue
                    if op["dma"]:
                        wait(("d",) + op["dsem_id"], op["dsem"], op["dprev"])
                        ins = op["fn"](h)
                        ins.then_inc(op["dsem"], 16)
                    else:
                        ins = op["fn"](h)
                        if op["marked"]:
                            ins.then_inc(csem[e], 1)
            return body

        for e in ENGINES:
            if ops[e]:
                getattr(block, HANDLES[e])(make(e))
        stack.close()


class Arena:
    def __init__(self, ap_f32, nbytes):
        self.ap = ap_f32
        self.nbytes = nbytes

    def view(self, off, shape, dtype, parts=128):
        esz = 2 if dtype == BF16 else 4
        n = int(np.prod(shape[1:]))
        assert off % 4 == 0 and off + n * esz <= self.nbytes, (off, shape, self.nbytes)
        nw = (n * esz + 3) // 4
        v = self.ap[0:parts, off // 4: off // 4 + nw]
        if dtype != F32:
            v = v.bitcast(dtype)
        if len(shape) == 3:
            v = v.rearrange("p (a b) -> p a b", a=shape[1])
        elif len(shape) == 4:
            v = v.rearrange("p (a b c) -> p a b c", a=shape[1], b=shape[2])
        return v


def build(stop_after=None, n_experts=NEXP, dbg=()):
    nc = bass.Bass("TRN2", target_bir_lowering=False)

    def din(name, shape, dt=F32):
        return nc.dram_tensor(name, list(shape), dt, kind="ExternalInput").ap()

    xo = din("xo", [NTOK, D])
    xc = din("xc", [NTOK, D])
    flag_d = din("flag", [128, 1])
    cT_d = din("cT", [128, 8])
    w_ada = din("w_ada", [D, 6 * D])
    b_adaT = din("b_adaT", [128, 48])
    w_in = din("w_in", [D, IN_COLS])
    gm_g = din("gm_g", [1, 512])
    gm_b = din("gm_b", [1, 512])
    wsT_d = din("wsT", [128, 8 * 128])
    bsp_d = din("bsp", [128, 4 * 128])
    wa_d = din("wa", [512, D])
    wb_d = din("wb", [512, D])
    wo_d = din("wo", [D, D])
    biasT_d = din("biasT", [128, 24 * 256])
    maskT_d = din("maskT", [128, 256])
    ln1g_d = din("ln1g", [1, D])
    ln1b_d = din("ln1b", [1, D])
    wr_d = din("wr", [D, NEXP])
    br_d = din("br", [1, NEXP])
    wg_d = din("wg", [n_experts, D, D])
    wu_d = din("wu", [n_experts, D, D])
    wd_d = din("wd", [n_experts, D, D])
    bg_d = din("bg", [NEXP, D])
    bu_d = din("bu", [NEXP, D])
    kbtab_d = din("kbtab", [128, NEXP])
    bdn_d = din("bdn", [NEXP, D])
    ln2g_d = din("ln2g", [1, D])
    ln2b_d = din("ln2b", [1, D])
    ident_d = din("ident", [128, 128])
    out_d = nc.dram_tensor("out", [NTOK, D], F32, kind="ExternalOutput").ap()
    x1s_d = nc.dram_tensor("x1s", [NTOK, D], F32, kind="Internal").ap()
    xs_scr = nc.dram_tensor("xs_scr", [NSLOT, D], BF16, kind="Internal").ap()
    ys_scr = nc.dram_tensor("ys_scr", [NSLOT, D], F32, kind="Internal").ap()
    dbg_out = {}

    w_in_v = w_in.rearrange("(kc p) n -> p kc n", p=128)
    w_ada_v = w_ada.rearrange("(kc p) n -> p kc n", p=128)

    st = contextlib.ExitStack()
    ARENA_BYTES = 207 * 1024
    arena_t = st.enter_context(nc.sbuf_tensor("arena", [128, ARENA_BYTES // 4], F32))
    A = Arena(arena_t, ARENA_BYTES)
    psum_t = st.enter_context(nc.psum_tensor("psum", [128, 4096], F32))

    def bank(i, shape=None, dtype=F32, half=None):
        v = psum_t[:, i * 512:(i + 1) * 512]
        if dtype != F32:
            v = v.bitcast(dtype)
        if shape is not None and len(shape) == 3:
            v = v.rearrange("p (a b) -> p a b", a=shape[1])
        return v

    S = Sched(nc)
    K = 1024

    off = [0]

    def palloc(shape, dtype, parts=128):
        esz = 2 if dtype == BF16 else 4
        n = int(np.prod(shape[1:])) * esz
        n = (n + 3) // 4 * 4
        v = A.view(off[0], shape, dtype, parts)
        off[0] += n
        return v

    ident_f = palloc([128, 128], F32)
    ident_b = palloc([128, 128], BF16)
    ones_f = palloc([128, 128], F32)
    ones_b = palloc([128, 64], BF16)
    onesf_b = palloc([128, 64], BF16)
    flag_s = palloc([128, 1], F32)
    cT_s = palloc([128, 8], F32)
    scT = palloc([128, 8], F32)
    scT_b = palloc([128, 8], BF16)
    badaT = palloc([128, 48], F32)
    modT = palloc([128, 48], F32)
    opsc1 = palloc([128, 8], F32)
    opsc2 = palloc([128, 8], F32)
    G1B = palloc([128, 1024], F32)
    G2B = palloc([128, 1024], F32)
    gates_all = palloc([128, 16, NEXP], F32)
    bgR = palloc([128, 8, NEXP], F32)
    bu1R = palloc([128, 8, NEXP], F32)
    E4f = palloc([128, 16, 4], F32)
    W4 = palloc([128, 16, 4], F32)
    destI = palloc([128, 4, 16], I32)
    idxW = palloc([128, NEXP, 8], I32)
    Mall = palloc([128, 16, NEXP], BF16)
    mi8_ = [palloc([128, 8], U32) for _ in range(2)]
    e4_ = [palloc([128, 4], F32) for _ in range(2)]
    maskT = palloc([128, 256], F32)
    stt_ = [palloc([128, 4, 2, 6], F32) for _ in range(2)]
    mv_ = [palloc([128, 4, 2], F32) for _ in range(2)]
    sd_ = [palloc([128, 4], F32) for _ in range(2)]
    rstd_ = [palloc([128, 4], F32) for _ in range(2)]
    nmr_ = [palloc([128, 4], F32) for _ in range(2)]
    mx8_ = [palloc([128, 8], F32) for _ in range(2)]
    negm_ = [palloc([128, 1], F32) for _ in range(2)]
    den_ = [palloc([128, 1], F32) for _ in range(2)]
    rden_ = [palloc([128, 1], F32) for _ in range(2)]
    lg_ = [palloc([128, NEXP], F32) for _ in range(2)]
    msk_ = [palloc([128, NEXP], F32) for _ in range(2)]
    ex_ = [palloc([128, NEXP], F32) for _ in range(2)]
    em_ = [palloc([128, NEXP], F32) for _ in range(2)]
    brB = palloc([128, NEXP], F32)
    R0_END = 32 * K
    assert off[0] <= R0_END, off[0]
    MOE_TMP = off[0]

    def dma(q, out, in_, reads=(), writes=()):
        return S.add(q, lambda h: h.dma_start(out=out, in_=in_), reads=reads, writes=writes, dma=True)

    def mm(out, lhsT, rhs, start, stop, reads, writes):
        return S.add("pe", lambda h: h.matmul(out, lhsT=lhsT, rhs=rhs, start=start, stop=stop), reads=reads, writes=writes)

    def tr(out, in_, ident, reads, writes):
        return S.add("pe", lambda h: h.transpose(out, in_, ident), reads=reads, writes=writes)

    def act(out, in_, func, reads, writes, bias=None, scale=None):
        kw = {}
        if bias is not None:
            kw["bias"] = bias
        if scale is not None:
            kw["scale"] = scale
        return S.add("act", lambda h: h.activation(out=out, in_=in_, func=func, **kw), reads=reads, writes=writes)

    def tt(eng, out, in0, in1, op, reads, writes):
        return S.add(eng, lambda h: h.tensor_tensor(out=out, in0=in0, in1=in1, op=op), reads=reads, writes=writes)

    def ts(eng, out, in0, s1, s2, op0, op1, reads, writes):
        if op1 is None:
            return S.add(eng, lambda h: h.tensor_scalar(out=out, in0=in0, scalar1=s1, scalar2=None, op0=op0), reads=reads, writes=writes)
        return S.add(eng, lambda h: h.tensor_scalar(out=out, in0=in0, scalar1=s1, scalar2=s2, op0=op0, op1=op1), reads=reads, writes=writes)

    def stt(out, in0, scalar, in1, op0, op1, reads, writes):
        return S.add("dve", lambda h: h.scalar_tensor_tensor(out=out, in0=in0, scalar=scalar, in1=in1, op0=op0, op1=op1), reads=reads, writes=writes)

    def cp(eng, out, in_, reads, writes):
        if eng == "act":
            return S.add("act", lambda h: h.activation(out=out, in_=in_, func=AF.Copy), reads=reads, writes=writes)
        return S.add(eng, lambda h: h.tensor_copy(out=out, in_=in_), reads=reads, writes=writes)

    def memset(eng, ap, val, writes):
        return S.add(eng, lambda h: h.memset(ap, val), writes=writes)

    def bcast(row_ap):
        return row_ap.partition_broadcast(128)

    def ln_stats(x_ap, slot, t, key_in, nhalf=2):
        for hh in range(nhalf):
            S.add("dve", lambda h, hh=hh: h.bn_stats(out=stt_[slot][:, t, hh, :], in_=x_ap[:, hh * 512:(hh + 1) * 512]),
                  reads=[key_in], writes=[("st", slot, t, hh)])
        src = stt_[slot][:, t, 0:nhalf, :].rearrange("p a b -> p (a b)")
        S.add("dve", lambda h: h.bn_aggr(out=mv_[slot][:, t, :], in_=src),
              reads=[("st", slot, t, hh) for hh in range(nhalf)], writes=[("mv", slot, t)])

    def ln_finish(slot, nt):
        act(sd_[slot][:, 0:nt], mv_[slot][:, 0:nt, 1], AF.Ln, reads=[("mv", slot, t) for t in range(nt)] + ["eps"],
            writes=[("sd", slot)], bias=EPS_AP[0], scale=1.0)
        act(rstd_[slot][:, 0:nt], sd_[slot][:, 0:nt], AF.Exp, reads=[("sd", slot)], writes=[("rstd", slot)], scale=-0.5)
        stt(nmr_[slot][:, 0:nt], mv_[slot][:, 0:nt, 0], -1.0, rstd_[slot][:, 0:nt], ALU.mult, ALU.mult,
            reads=[("mv", slot, t) for t in range(nt)] + [("rstd", slot)], writes=[("nmr", slot)])

    EPS_AP = [None]
    eps_t = A.view(MOE_TMP, [128, 1], F32)
    EPS_AP[0] = eps_t
    MOE_TMP += 4

    dma("sp", ident_f, ident_d, writes=["ident_f"])
    dma("sp", flag_s, flag_d, writes=["flag"])
    dma("sp", cT_s, cT_d, writes=["cT"])
    dma("sp", badaT, b_adaT, writes=["badaT"])
    dma("sp", maskT, maskT_d, writes=["maskT"])
    memset("dve", eps_t, EPS, writes=["eps"])
    memset("dve", ones_f, 1.0, writes=["ones_f"])
    memset("dve", ones_b, 1.0, writes=["ones_b"])
    cp("dve", ident_b, ident_f, reads=["ident_f"], writes=["ident_b"])
    ts("dve", onesf_b, ones_b, flag_s[:, 0:1], None, ALU.mult, None, reads=["flag", "ones_b"], writes=["onesf_b"])
    act(scT, cT_s, AF.Silu, reads=["cT"], writes=["scT"])
    cp("dve", scT_b, scT, reads=["scT"], writes=["scT_b"])

    R5 = 144 * K
    wad = [A.view(R5 + i * 8 * K, [128, 8, 512], BF16) for i in range(4)]
    psM = bank(7)
    for blk in range(12):
        wbuf = wad[blk % 4]
        dma("pool", wbuf, w_ada_v[:, :, blk * 512:(blk + 1) * 512], writes=[("wad", blk % 4)])
        for jj in range(4):
            j = blk * 4 + jj
            for kc in range(8):
                mm(psM[:, j:j + 1], wbuf[:, kc, jj * 128:(jj + 1) * 128], scT_b[:, kc:kc + 1], kc == 0, kc == 7,
                   reads=[("wad", blk % 4), "scT_b"], writes=[("ps", 7)])
    tt("dve", modT, psM[:, 0:48], badaT, ALU.add, reads=[("ps", 7), "badaT"], writes=["modT"])
    ts("dve", opsc1, modT[:, 8:16], 1.0, None, ALU.add, None, reads=["modT"], writes=["opsc1"])
    ts("dve", opsc2, modT[:, 32:40], 1.0, None, ALU.add, None, reads=["modT"], writes=["opsc2"])
    dg = [A.view(R5 + 32 * K + i * 512, [128, 128], F32) for i in range(2)]
    for gi, (GB, col0) in enumerate(((G1B, 16), (G2B, 40))):
        for c in range(8):
            d_ = dg[c % 2]
            ts("dve", d_, ident_f, modT[:, col0 + c:col0 + c + 1], None, ALU.mult, None,
               reads=["ident_f", "modT"], writes=[("dg", c % 2)])
            pb = bank(gi * 2 + c // 4)
            mm(pb[:, (c % 4) * 128:(c % 4 + 1) * 128], ones_f, d_, True, True,
               reads=["ones_f", ("dg", c % 2)], writes=[("ps", gi * 2 + c // 4)])
        for hh in range(2):
            cp("dve", GB[:, hh * 512:(hh + 1) * 512], bank(gi * 2 + hh), reads=[("ps", gi * 2 + hh)], writes=[("GB", gi)])
    sh1 = modT[:, 0:8]
    sh2 = modT[:, 24:32]

    R1 = 32 * K
    R2 = 64 * K
    hT = A.view(R2, [128, 8, 4096], BF16)
    R3 = R2 + 64 * K
    uT = A.view(R3, [128, 4, NTOK], BF16)
    R4 = R3 + 16 * K
    ybT = A.view(R4, [128, 4, NTOK], BF16)
    R5 = R4 + 16 * K
    xs = [A.view(R1 + i * 4 * K, [128, 1024], F32) for i in range(8)]
    xn = [A.view(180 * K + i * 2 * K, [128, 1024], BF16) for i in range(4)]

    def x_tile(tt_):
        return xc[tt_ * 128:(tt_ + 1) * 128, :] if tt_ < 16 else xo[(tt_ - 16) * 128:(tt_ - 15) * 128, :]

    evac_flip = [0]

    def evac_mod(out, in_, scale_ap, bias_ap, reads, writes, eng=None):
        if eng is None:
            evac_flip[0] ^= 1
            eng = "act" if evac_flip[0] else "dve"
        if eng == "act":
            act(out, in_, AF.Identity, reads=reads, writes=writes, bias=bias_ap, scale=scale_ap)
        else:
            ts("dve", out, in_, scale_ap, bias_ap, ALU.mult, ALU.add, reads=reads, writes=writes)

    for b in range(8):
        slot = b % 2
        xo_ = (b % 2) * 4
        for t in range(4):
            tile = b * 4 + t
            dma("sp", xs[xo_ + t], x_tile(tile), writes=[("xs", xo_ + t)])
            ln_stats(xs[xo_ + t], slot, t, ("xs", xo_ + t))
        ln_finish(slot, 4)
        for hb in range(2):
            b0 = 2 * (hb + 2 * (b % 2))
            pT = psum_t[:, b0 * 512:(b0 + 2) * 512].bitcast(BF16).rearrange("p (a b) -> p a b", a=8)
            pkey = ("ps", b0)
            for t2 in range(2):
                t = hb * 2 + t2
                act(xn[t], xs[xo_ + t], AF.Identity, reads=[("xs", xo_ + t), ("rstd", slot), ("nmr", slot)], writes=[("xn", t)],
                    bias=nmr_[slot][:, t:t + 1], scale=rstd_[slot][:, t:t + 1])
                for kc in range(8):
                    tr(pT[:, kc, t2 * 128:(t2 + 1) * 128], xn[t][:, kc * 128:(kc + 1) * 128], ident_b,
                       reads=[("xn", t), "ident_b"], writes=[pkey, ("ps", b0 + 1)])
            tok0 = b * 512 + hb * 256
            for kc in range(8):
                evac_mod(hT[:, kc, tok0:tok0 + 256], pT[:, kc, :], opsc1[:, kc:kc + 1], sh1[:, kc:kc + 1],
                         reads=[pkey, ("ps", b0 + 1), "opsc1", "modT"], writes=[("hT", b, kc)], eng=("act" if kc < 4 else "dve"))

    if "hT" in dbg:
        dbg_out["hT"] = (hT, [128, 8 * 4096], BF16, [("hT", b, kc) for b in range(8) for kc in range(8)])

    NWR = 7
    wring = [A.view(R5 + i * 2 * K, [128, 8, 128], BF16) for i in range(NWR)]
    wr_n = [0]

    def load_chunk(src_v, col0, ncols=128):
        i = wr_n[0] % NWR
        wr_n[0] += 1
        dma("pool", wring[i][:, :, 0:ncols], src_v[:, :, col0:col0 + ncols], writes=[("wring", i)])
        return wring[i], ("wring", i)

    ps_n = [0]

    def next_bank(lo=0, n=2):
        i = lo + ps_n[0] % n
        ps_n[0] += 1
        return i

    def proj_T(wbuf, wkey, tb, nk=8, src=None, srckeys=None, bank_lo=0, bank_n=2):
        bi = next_bank(bank_lo, bank_n)
        pb = bank(bi)
        for kc in range(nk):
            mm(pb, wbuf[:, kc, :], hT[:, kc, tb * 512:(tb + 1) * 512], kc == 0, kc == nk - 1,
               reads=[wkey, ("hT", tb, kc)], writes=[("ps", bi)])
        return pb, ("ps", bi)

    if stop_after != "hT":
        GM = R1
        gmgB = A.view(GM, [128, 512], F32)
        gmbB = A.view(GM + 2 * K, [128, 512], F32)
        BSp = A.view(GM + 4 * K, [128, 4, 128], F32)
        wsT_f = A.view(GM + 6 * K, [128, 8, 128], F32)
        wsT_b = A.view(GM + 10 * K, [128, 8, 128], BF16)
        vg = [A.view(GM + 12 * K + i * 2 * K, [128, 512], F32) for i in range(2)]
        zz = [A.view(GM + 16 * K + i * 2 * K, [128, 512], F32) for i in range(2)]
        vn = [A.view(GM + 20 * K + i * K, [128, 512], BF16) for i in range(2)]
        tmpg = [A.view(GM + 22 * K + i * 2 * K, [128, 4, 128], F32) for i in range(2)]
        Wvg = A.view(R5 + 14 * K, [128, 8, 512], BF16)
        S.barrier()
        dma("sp", gmgB, bcast(gm_g[0:1, :]), writes=["gmgB"])
        dma("sp", gmbB, bcast(gm_b[0:1, :]), writes=["gmbB"])
        dma("sp", BSp, bsp_d.rearrange("p (a b) -> p a b", a=4), writes=["BSp"])
        dma("sp", wsT_f, wsT_d.rearrange("p (a b) -> p a b", a=8), writes=["wsT_f"])
        S.add("pool", lambda h: h.affine_select(out=wsT_f, in_=wsT_f, pattern=[[0, 8], [1, 128]], compare_op=ALU.is_ge,
                                                 fill=0.0, base=0, channel_multiplier=-1),
              reads=["wsT_f"], writes=["wsT_f"])
        cp("pool", wsT_b, wsT_f, reads=["wsT_f"], writes=["wsT_b"])
        dma("pool", Wvg, w_in_v[:, :, 512:1024], writes=["Wvg"])
        for c in range(4):
            wbuf, wkey = load_chunk(w_in_v, c * 128)
            for tb in range(4, 8):
                pb, pkey = proj_T(wbuf, wkey, tb)
                act(uT[:, c, (tb - 4) * 512:(tb - 3) * 512], pb, AF.Gelu_apprx_tanh, reads=[pkey], writes=[("uT", tb - 4)])
        def gmlpA(ti):
            tile = 16 + ti
            sl = ti % 2
            bi = next_bank(0, 2)
            pb = bank(bi)
            for kc in range(8):
                mm(pb, hT[:, kc, tile * 128:(tile + 1) * 128], Wvg[:, kc, :], kc == 0, kc == 7,
                   reads=[("hT", tile // 4, kc), "Wvg"], writes=[("ps", bi)])
            act(vg[sl], pb, AF.Gelu_apprx_tanh, reads=[("ps", bi)], writes=[("vg", sl)])
            ln_stats(vg[sl], sl, 0, ("vg", sl), nhalf=1)
            ln_finish(sl, 1)
            act(zz[sl], vg[sl], AF.Identity, reads=[("vg", sl), ("rstd", sl), ("nmr", sl)], writes=[("zz", sl)],
                bias=nmr_[sl][:, 0:1], scale=rstd_[sl][:, 0:1])
            tt("dve", zz[sl], zz[sl], gmgB, ALU.mult, reads=[("zz", sl), "gmgB"], writes=[("zz", sl)])
            tt("dve", vn[sl], zz[sl], gmbB, ALU.add, reads=[("zz", sl), "gmbB"], writes=[("vn", sl)])

        def gmlpB(ti):
            sl = ti % 2
            bs_i = 2 + ti % 2
            pS = bank(bs_i, [128, 4, 128])
            for gp in range(4):
                for hh in range(2):
                    g_ = 2 * gp + hh
                    mm(pS[hh * 64:(hh + 1) * 64, gp, :], vn[sl][:, g_ * 64:(g_ + 1) * 64], wsT_b[:, g_, :], True, True,
                       reads=[("vn", sl), "wsT_b"], writes=[("ps", bs_i)])
            tt("dve", tmpg[sl], pS, BSp, ALU.add, reads=[("ps", bs_i), "BSp"], writes=[("tmpg", sl)])
            tt("dve", uT[:, :, ti * 128:(ti + 1) * 128], tmpg[sl], uT[:, :, ti * 128:(ti + 1) * 128], ALU.mult,
               reads=[("tmpg", sl), ("uT", ti // 4)], writes=[("uT", ti // 4)])

        gmlpA(0)
        for ti in range(16):
            if ti + 1 < 16:
                gmlpA(ti + 1)
                gmlpB(ti)
            else:
                gmlpB(ti)
        if "yaT" in dbg:
            dbg_out["yaT"] = (uT, [128, 4 * NTOK], BF16, [("uT", i) for i in range(4)])

    if stop_after not in ("hT", "gmlp"):
        AT = R1
        accND = A.view(AT, [128, 2, NTOK], F32)
        sbS = [A.view(AT + 16 * K + i * K, [128, 256], F32) for i in range(4)]
        ptS = [A.view(AT + 20 * K + i * 512, [128, 256], BF16) for i in range(4)]
        braw = [A.view(AT + 22 * K + i * 2 * K, [128, 2, 256], F32) for i in range(2)]
        biasm = [A.view(AT + 26 * K + i * 2 * K, [128, 2, 256], F32) for i in range(2)]
        QKV0 = R5 + 14 * K
        QT = [A.view(QKV0, [128, NTOK], BF16), A.view(QKV0 + 20 * K, [128, NTOK], BF16)]
        KT = [A.view(QKV0 + 4 * K, [128, 4096], BF16)] * 2
        VV = [A.view(QKV0 + 12 * K, [128, 32, 128], BF16), A.view(QKV0 + 24 * K, [128, 32, 128], BF16)]
        assert QKV0 + 32 * K <= ARENA_BYTES
        S.barrier()
        ztile = A.view(MOE_TMP, [128, 1024], BF16)
        memset("pool", ztile, 0.0, writes=["ztile"])
        ZR = 1024
        zlist = list(range(0, NSLOT, ZR))

        def zero_fill(cnt):
            for _ in range(cnt):
                if not zlist:
                    return
                z0 = zlist.pop(0)
                zn_ = min(ZR, NSLOT - z0)
                dma("sp", xs_scr[z0:z0 + zn_, :].rearrange("(t p) d -> p t d", p=128),
                    ztile.unsqueeze(1).broadcast_to([128, zn_ // 128, 1024]), reads=["ztile"], writes=[("xs_zero", z0)])
        unit_n = 0
        for j in range(4):
            for g in range(3):
                r = DIL[g]
                nb_rho = 16 // r
                sl = unit_n % 2
                unit_n += 1
                kq = ("QT", sl)
                kk = ("KT", 0)
                col = 1024 + g * 1536 + j * 128
                wq, wqk = load_chunk(w_in_v, col)
                wk, wkk = load_chunk(w_in_v, col + 512)
                wv, wvk = load_chunk(w_in_v, col + 1024)
                dma("sp", braw[sl], biasT_d.rearrange("p (a b) -> p a b", a=24)[:, g * 8 + 2 * j:g * 8 + 2 * j + 2, :],
                    writes=[("braw", sl)])
                zero_fill(3)
                for hh in range(2):
                    tt("pool", biasm[sl][:, hh, :], braw[sl][:, hh, :], maskT, ALU.add,
                       reads=[("braw", sl), "maskT"], writes=[("biasm", sl)])
                for tb in range(4, 8):
                    pb, pkey = proj_T(wq, wqk, tb)
                    act(QT[sl][:, (tb - 4) * 512:(tb - 3) * 512], pb, AF.Identity, reads=[pkey], writes=[kq], scale=0.125)
                tb_lo = 0 if g == 2 else 3
                for tb in range(tb_lo, 8):
                    pb, pkey = proj_T(wk, wkk, tb)
                    cp("dve", KT[sl][:, tb * 512:(tb + 1) * 512], pb, reads=[pkey], writes=[kk])
                for rho in range(r):
                    for ib in range(2 * nb_rho):
                        ctx = ib < nb_rho
                        if ctx and g < 2 and ib != nb_rho - 1:
                            continue
                        vt = (0 if ctx else 16) + rho * nb_rho + ib % nb_rho
                        bi = 2 + (vt % 2)
                        pv = bank(bi)[:, 0:128]
                        start = rho + r * 128 * ib
                        for kc in range(8):
                            mm(pv, hT[:, kc, start:start + 127 * r + 1:r], wv[:, kc, :], kc == 0, kc == 7,
                               reads=[("hT", b_, kc) for b_ in range(start // 512, (start + 128 * r - 1) // 512 + 1)] + [wvk],
                               writes=[("ps", bi)])
                        if ctx:
                            act(VV[sl][:, vt, :], pv, AF.Identity, reads=[("ps", bi), "flag"], writes=[("V", sl)], scale=flag_s[:, 0:1])
                        else:
                            cp("dve", VV[sl][:, vt, :], pv, reads=[("ps", bi)], writes=[("V", sl)])
                def unit_geom(qb):
                    rho = qb // nb_rho
                    ib_cur = nb_rho + qb % nb_rho
                    qcol0 = rho + r * 128 * ib_cur - NTOK
                    q_sl = slice(qcol0, qcol0 + 127 * r + 1, r)
                    ktile = []
                    for ib in (ib_cur - 1, ib_cur):
                        ctx = ib < nb_rho
                        vt = (0 if ctx else 16) + rho * nb_rho + ib % nb_rho
                        kst = rho + r * 128 * ib
                        ktile.append((slice(kst, kst + 127 * r + 1, r), vt, ctx))
                    return q_sl, ktile

                def unit_S(qb):
                    q_sl, ktile = unit_geom(qb)
                    u = qb % 2
                    for hh in range(2):
                        bS = 2 * hh + u
                        pS = bank(bS)[:, 0:256]
                        pk = ("ps", bS)
                        p0, p1 = hh * 64, hh * 64 + 64
                        for kb in range(2):
                            mm(pS[:, kb * 128:(kb + 1) * 128], KT[sl][p0:p1, ktile[kb][0]], QT[sl][p0:p1, q_sl], True, True,
                               reads=[kk, kq], writes=[pk])
                        si = u * 2 + hh
                        stt(sbS[si], pS, 60.0, biasm[sl][:, hh, :], ALU.min, ALU.add,
                            reads=[pk, ("biasm", sl)], writes=[("sbS", si)])
                        act(ptS[si], sbS[si], AF.Exp, reads=[("sbS", si)], writes=[("ptS", si)])

                def unit_PV(qb):
                    q_sl, ktile = unit_geom(qb)
                    u = qb % 2
                    pND = bank(4 + u)[:, 0:256].rearrange("p (a b) -> p a b", a=2)
                    pkN = ("ps", 4 + u)
                    for which in range(2):
                        for hh in range(2):
                            si = u * 2 + hh
                            p0, p1 = hh * 64, hh * 64 + 64
                            for kb in range(2):
                                _, vt, ctx = ktile[kb]
                                if which == 0:
                                    lhs = VV[sl][:, vt, p0:p1]
                                    rk = [("V", sl)]
                                else:
                                    lhs = onesf_b if ctx else ones_b
                                    rk = ["onesf_b", "ones_b"]
                                mm(pND[p0:p1, which, :], lhs, ptS[si][:, kb * 128:(kb + 1) * 128], kb == 0, kb == 1,
                                   reads=rk + [("ptS", si)], writes=[pkN])
                    dst = accND[:, :, q_sl]
                    if g == 0:
                        cp("dve", dst, pND, reads=[pkN], writes=["accND"])
                    else:
                        tt("dve", dst, pND, dst, ALU.add, reads=[pkN, "accND"], writes=["accND"])

                if PIPE_ATTN:
                    unit_S(0)
                    for qb in range(16):
                        if qb + 1 < 16:
                            unit_S(qb + 1)
                        unit_PV(qb)
                else:
                    for qb in range(16):
                        unit_S(qb)
                        unit_PV(qb)
            S.add("dve", lambda h: h.reciprocal(out=accND[:, 1, :], in_=accND[:, 1, :]), reads=["accND"], writes=["accND"])
            tt("dve", ybT[:, j, :], accND[:, 0, :], accND[:, 1, :], ALU.mult, reads=["accND"], writes=[("ybT", j)])
        if "ybT" in dbg:
            dbg_out["ybT"] = (ybT, [128, 4 * NTOK], BF16, [("ybT", i) for i in range(4)])

    if stop_after not in ("hT", "gmlp", "attn"):
        S.barrier()
        mT = A.view(R1, [128, 8, NTOK], BF16)
        Wa = A.view(R5 + 14 * K, [128, 4, 1024], BF16)
        Wb = A.view(R5 + 22 * K, [128, 4, 1024], BF16)
        gab = [A.view(R5 + 30 * K + i * 2 * K, [128, 512], F32) for i in range(4)]
        t12 = [A.view(R5 + 38 * K + i * 2 * K, [128, 512], F32) for i in range(4)]
        dma("pool", Wa, wa_d.rearrange("(kc p) n -> p kc n", p=128), writes=["Wa"])
        dma("pool", Wb, wb_d.rearrange("(kc p) n -> p kc n", p=128), writes=["Wb"])
        it = 0
        for m in range(8):
            wga, wgak = load_chunk(w_in_v, 5632 + m * 128)
            wgb, wgbk = load_chunk(w_in_v, 6656 + m * 128)
            for tb in range(4):
                s2 = it % 2
                it += 1
                pa = bank(0 + s2 * 4)
                pbb = bank(1 + s2 * 4)
                for kc in range(4):
                    mm(pa, Wa[:, kc, m * 128:(m + 1) * 128], uT[:, kc, tb * 512:(tb + 1) * 512], kc == 0, kc == 3,
                       reads=["Wa", ("uT", tb)], writes=[("ps", 0 + s2 * 4)])
                for kc in range(4):
                    mm(pbb, Wb[:, kc, m * 128:(m + 1) * 128], ybT[:, kc, tb * 512:(tb + 1) * 512], kc == 0, kc == 3,
                       reads=["Wb"] + [("ybT", jj) for jj in range(4)], writes=[("ps", 1 + s2 * 4)])
                pga, pgak = proj_T(wga, wgak, 4 + tb, bank_lo=2 + s2 * 4, bank_n=1)
                pgb, pgbk = proj_T(wgb, wgbk, 4 + tb, bank_lo=3 + s2 * 4, bank_n=1)
                act(gab[s2 * 2], pga, AF.Sigmoid, reads=[pgak], writes=[("gab", s2 * 2)])
                act(gab[s2 * 2 + 1], pgb, AF.Sigmoid, reads=[pgbk], writes=[("gab", s2 * 2 + 1)])
                tt("dve", t12[s2 * 2], pa, gab[s2 * 2], ALU.mult, reads=[("ps", 0 + s2 * 4), ("gab", s2 * 2)], writes=[("t12", s2 * 2)])
                tt("dve", t12[s2 * 2 + 1], pbb, gab[s2 * 2 + 1], ALU.mult, reads=[("ps", 1 + s2 * 4), ("gab", s2 * 2 + 1)], writes=[("t12", s2 * 2 + 1)])
                tt("pool", mT[:, m, tb * 512:(tb + 1) * 512], t12[s2 * 2], t12[s2 * 2 + 1], ALU.add,
                   reads=[("t12", s2 * 2), ("t12", s2 * 2 + 1)], writes=[("mT", tb)])
        if "mT" in dbg:
            dbg_out["mT"] = (mT, [128, 8 * NTOK], BF16, [("mT", i) for i in range(4)])

        S.barrier()
        H2 = R2
        xnb = A.view(H2, [128, 16, 1024], BF16)
        O5 = H2 + 32 * K
        Wo = A.view(O5, [128, 8, 1024], BF16)
        ln1gB = A.view(O5 + 16 * K, [128, 1024], F32)
        ln1bB = A.view(O5 + 20 * K, [128, 1024], F32)
        xs5 = [A.view(O5 + 24 * K + i * 4 * K, [128, 1024], F32) for i in range(2)]
        zt = [A.view(O5 + 32 * K + i * 4 * K, [128, 1024], F32) for i in range(2)]
        x1t = [A.view(O5 + 40 * K + i * 4 * K, [128, 1024], F32) for i in range(2)]
        wr_s = A.view(MOE_TMP, [128, 8, NEXP], F32)
        h2Tf = A.view(MOE_TMP + 6 * K, [128, 8, 128], F32)
        assert MOE_TMP + 10 * K + 512 <= R0_END
        dma("pool", Wo, wo_d.rearrange("(kc p) n -> p kc n", p=128), writes=["Wo"])
        dma("sp", ln1gB, bcast(ln1g_d[0:1, :]), writes=["ln1gB"])
        dma("sp", ln1bB, bcast(ln1b_d[0:1, :]), writes=["ln1bB"])
        dma("sp", wr_s, wr_d.rearrange("(kc p) n -> p kc n", p=128), writes=["wr_s"])
        dma("sp", brB, bcast(br_d[0:1, :]), writes=["brB"])
        def stage5A(ti):
            s2 = ti % 2
            dma("sp", xs5[s2], xo[ti * 128:(ti + 1) * 128, :], writes=[("xs5", s2)])
            for hh in range(2):
                bi = hh + 2 * s2
                pb = bank(bi)
                for kc in range(8):
                    mm(pb, mT[:, kc, ti * 128:(ti + 1) * 128], Wo[:, kc, hh * 512:(hh + 1) * 512], kc == 0, kc == 7,
                       reads=[("mT", ti // 4), "Wo"], writes=[("ps", bi)])
                tt("dve", zt[s2][:, hh * 512:(hh + 1) * 512], pb, G1B[:, hh * 512:(hh + 1) * 512], ALU.mult,
                   reads=[("ps", bi), ("GB", 0)], writes=[("zt", s2)])
            stt(zt[s2], xs5[s2], ALPHA, zt[s2], ALU.mult, ALU.add, reads=[("xs5", s2), ("zt", s2)], writes=[("zt", s2)])
            ln_stats(zt[s2], s2, 0, ("zt", s2))
            ln_finish(s2, 1)
            act(zt[s2], zt[s2], AF.Identity, reads=[("zt", s2), ("rstd", s2), ("nmr", s2)], writes=[("zt", s2)],
                bias=nmr_[s2][:, 0:1], scale=rstd_[s2][:, 0:1])
            tt("dve", zt[s2], zt[s2], ln1gB, ALU.mult, reads=[("zt", s2), "ln1gB"], writes=[("zt", s2)])
            tt("dve", x1t[s2], zt[s2], ln1bB, ALU.add, reads=[("zt", s2), "ln1bB"], writes=[("x1t", s2)])
            dma("sp", x1s_d[ti * 128:(ti + 1) * 128, :], x1t[s2], reads=[("x1t", s2)], writes=[("x1s", ti)])
            ln_stats(x1t[s2], s2, 1, ("x1t", s2))
            act(sd_[s2][:, 1:2], mv_[s2][:, 1:2, 1], AF.Ln, reads=[("mv", s2, 1)], writes=[("sd2", s2)], bias=eps_t, scale=1.0)
            act(rstd_[s2][:, 1:2], sd_[s2][:, 1:2], AF.Exp, reads=[("sd2", s2)], writes=[("rstd2", s2)], scale=-0.5)
            stt(nmr_[s2][:, 1:2], mv_[s2][:, 1:2, 0], -1.0, rstd_[s2][:, 1:2], ALU.mult, ALU.mult,
                reads=[("mv", s2, 1), ("rstd2", s2)], writes=[("nmr2", s2)])
            act(zt[s2], x1t[s2], AF.Identity, reads=[("x1t", s2), ("rstd2", s2), ("nmr2", s2)], writes=[("zt", s2)],
                bias=nmr_[s2][:, 1:2], scale=rstd_[s2][:, 1:2])
            cp("act", xnb[:, ti, :], zt[s2], reads=[("zt", s2)], writes=[("xnb", ti)])

        def stage5B(ti):
            s2 = ti % 2
            for hb in range(2):
                bi = 4 + hb
                pT = bank(bi, [128, 4, 128])
                for k4 in range(4):
                    kc = hb * 4 + k4
                    tr(pT[:, k4, :], zt[s2][:, kc * 128:(kc + 1) * 128], ident_f, reads=[("zt", s2), "ident_f"], writes=[("ps", bi)])
                for k4 in range(4):
                    kc = hb * 4 + k4
                    evac_mod(h2Tf[:, kc, :], pT[:, k4, :], opsc2[:, kc:kc + 1], sh2[:, kc:kc + 1],
                             reads=[("ps", bi), "opsc2", "modT"], writes=[("h2Tf", kc)], eng=("act" if hb == 0 else "dve"))
            pR = bank(6)[:, 0:NEXP]
            for kc in range(8):
                mm(pR, h2Tf[:, kc, :], wr_s[:, kc, :], kc == 0, kc == 7, reads=[("h2Tf", kc), "wr_s"], writes=[("ps", 6)])
            tt("dve", lg_[s2], pR, brB, ALU.add, reads=[("ps", 6), "brB"], writes=[("lg", s2)])
            S.add("dve", lambda h, s2=s2: h.max(out=mx8_[s2], in_=lg_[s2]), reads=[("lg", s2)], writes=[("mx8", s2)])
            ts("dve", msk_[s2], lg_[s2], mx8_[s2][:, 3:4], None, ALU.is_ge, None, reads=[("lg", s2), ("mx8", s2)], writes=[("msk", s2)])
            ts("dve", negm_[s2], mx8_[s2][:, 0:1], -1.0, None, ALU.mult, None, reads=[("mx8", s2)], writes=[("negm", s2)])
            act(ex_[s2], lg_[s2], AF.Exp, reads=[("lg", s2), ("negm", s2)], writes=[("ex", s2)], bias=negm_[s2][:, 0:1], scale=1.0)
            tt("dve", em_[s2], ex_[s2], msk_[s2], ALU.mult, reads=[("ex", s2), ("msk", s2)], writes=[("em", s2)])
            S.add("dve", lambda h, s2=s2: h.reduce_sum(out=den_[s2], in_=em_[s2], axis=AX.X), reads=[("em", s2)], writes=[("den", s2)])
            S.add("dve", lambda h, s2=s2: h.reciprocal(out=rden_[s2], in_=den_[s2]), reads=[("den", s2)], writes=[("rden", s2)])
            ts("dve", gates_all[:, ti, :], em_[s2], rden_[s2][:, 0:1], None, ALU.mult, None, reads=[("em", s2), ("rden", s2)], writes=[("gates", ti)])
            S.add("dve", lambda h, s2=s2: h.max_index(out=mi8_[s2], in_max=mx8_[s2], in_values=lg_[s2]), reads=[("lg", s2), ("mx8", s2)], writes=[("mi8", s2)])
            cp("dve", E4f[:, ti, :], mi8_[s2][:, 0:4], reads=[("mi8", s2)], writes=[("E4f", ti)])
            act(e4_[s2], mx8_[s2][:, 0:4], AF.Exp, reads=[("mx8", s2), ("negm", s2)], writes=[("e4", s2)], bias=negm_[s2][:, 0:1], scale=1.0)
            ts("dve", W4[:, ti, :], e4_[s2], rden_[s2][:, 0:1], None, ALU.mult, None, reads=[("e4", s2), ("rden", s2)], writes=[("W4", ti)])
            cp("pool", Mall[:, ti, :], msk_[s2], reads=[("msk", s2)], writes=[("Mall", ti)])

        stage5A(0)
        for ti in range(16):
            if ti + 1 < 16:
                S.zipped(lambda: stage5A(ti + 1), lambda: stage5B(ti))
            else:
                stage5B(ti)
        if "x1" in dbg:
            dbg_out["x1"] = None
        if "gates" in dbg:
            dbg_out["gates"] = (gates_all, [128, 16 * NEXP], F32, [("gates", i) for i in range(16)])

    if stop_after in (None, "moe", "route"):
        S.barrier()
        RT0 = R5
        within = A.view(RT0, [128, 16, NEXP], F32)
        tot = A.view(RT0 + 2 * K, [128, 16, NEXP], F32)
        base = A.view(RT0 + 4 * K, [128, 16, NEXP], F32)
        dall = A.view(RT0 + 6 * K, [128, 16, NEXP], F32)
        big1 = A.view(RT0 + 8 * K, [128, NEXP, NEXP], F32)
        big2 = A.view(RT0 + 12 * K, [128, NEXP, NEXP], F32)
        LTm = A.view(RT0 + 16 * K, [128, NEXP, NEXP], F32)
        oh = A.view(RT0 + 20 * K, [128, 16, NEXP], F32)
        U_f = A.view(RT0 + 22 * K, [128, 128], F32)
        U_b = A.view(RT0 + 22 * K + 512, [128, 128], BF16)
        on_b = A.view(RT0 + 22 * K + 768, [128, 128], BF16)
        iotaE = A.view(RT0 + 23 * K, [128, NEXP], F32)
        cnt = A.view(RT0 + 23 * K + 128, [128, NEXP], F32)
        rank = A.view(RT0 + 23 * K + 256, [128, NEXP], F32)
        kbv = A.view(RT0 + 23 * K + 384, [128, NEXP], F32)
        sig = A.view(RT0 + 23 * K + 512, [128, NEXP], F32)
        kbtab = A.view(RT0 + 23 * K + 640, [128, NEXP], F32)
        pk = A.view(RT0 + 23 * K + 768, [128, 8], F32)
        destF = A.view(RT0 + 24 * K, [128, 4, 16], F32)
        idxWf = A.view(RT0 + 24 * K + 256, [128, NEXP, 8], F32)
        rankT = A.view(RT0 + 26 * K, [NEXP, 1], F32, parts=NEXP)
        RTm = A.view(RT0 + 26 * K + 64, [NEXP, NEXP], F32, parts=NEXP)
        bgrow = A.view(RT0 + 27 * K, [NEXP, 1024], F32, parts=NEXP)
        burow = A.view(RT0 + 31 * K, [NEXP, 1024], F32, parts=NEXP)
        dma("sp", kbtab, kbtab_d, writes=["kbtab"])
        dma("sp", bgrow, bg_d, writes=["bgrow"])
        dma("sp", burow, bu_d, writes=["burow"])
        memset("pool", U_f, 1.0, writes=["U_f"])
        S.add("pool", lambda h: h.affine_select(out=U_f, in_=U_f, pattern=[[1, 128]], compare_op=ALU.is_gt, fill=0.0, base=0, channel_multiplier=-1),
              reads=["U_f"], writes=["U_f"])
        cp("pool", U_b, U_f, reads=["U_f"], writes=["U_b"])
        memset("pool", on_b, 1.0, writes=["on_b"])
        S.add("pool", lambda h: h.iota(iotaE, pattern=[[1, NEXP]], base=0, channel_multiplier=0, allow_small_or_imprecise_dtypes=True), writes=["iotaE"])
        S.add("pool", lambda h: h.iota(pk, pattern=[[128, 8]], base=0, channel_multiplier=1, allow_small_or_imprecise_dtypes=True), writes=["pk"])
        memset("pool", LTm, 1.0, writes=["LTm"])
        S.add("pool", lambda h: h.affine_select(out=LTm, in_=LTm, pattern=[[1, NEXP], [-1, NEXP]], compare_op=ALU.is_gt, fill=0.0, base=0, channel_multiplier=0),
              reads=["LTm"], writes=["LTm"])
        Mkeys = [("Mall", i) for i in range(16)]
        Mflat = Mall.rearrange("p a b -> p (a b)")
        mm(bank(0), U_b, Mflat, True, True, reads=["U_b"] + Mkeys, writes=[("ps", 0)])
        mm(bank(1), on_b, Mflat, True, True, reads=["on_b"] + Mkeys, writes=[("ps", 1)])
        cp("dve", within.rearrange("p a b -> p (a b)"), bank(0), reads=[("ps", 0)], writes=["within"])
        cp("act", tot.rearrange("p a b -> p (a b)"), bank(1), reads=[("ps", 1)], writes=["tot"])
        memset("dve", base[:, 0, :], 0.0, writes=["base"])
        for ti in range(1, 16):
            tt("dve", base[:, ti, :], base[:, ti - 1, :], tot[:, ti - 1, :], ALU.add, reads=["base", "tot"], writes=["base"])
        tt("dve", cnt, base[:, 15, :], tot[:, 15, :], ALU.add, reads=["base", "tot"], writes=["cnt"])
        cA = cnt.unsqueeze(1).broadcast_to([128, NEXP, NEXP])
        cB = cnt.unsqueeze(2).broadcast_to([128, NEXP, NEXP])
        tt("dve", big1, cA, cB, ALU.is_gt, reads=["cnt"], writes=["big1"])
        tt("dve", big2, cA, cB, ALU.is_equal, reads=["cnt"], writes=["big2"])
        tt("dve", big2, big2, LTm, ALU.mult, reads=["big2", "LTm"], writes=["big2"])
        tt("dve", big1, big1, big2, ALU.add, reads=["big1", "big2"], writes=["big1"])
        S.add("dve", lambda h: h.reduce_sum(out=rank, in_=big1, axis=AX.X), reads=["big1"], writes=["rank"])
        rA = rank.unsqueeze(2).broadcast_to([128, NEXP, NEXP])
        iB = iotaE.unsqueeze(1).broadcast_to([128, NEXP, NEXP])
        tt("dve", big1, rA, iB, ALU.is_equal, reads=["rank", "iotaE"], writes=["big1"])
        tt("dve", big2, big1, kbtab.unsqueeze(1).broadcast_to([128, NEXP, NEXP]), ALU.mult, reads=["big1", "kbtab"], writes=["big2"])
        S.add("dve", lambda h: h.reduce_sum(out=kbv, in_=big2, axis=AX.X), reads=["big2"], writes=["kbv"])
        tt("dve", big1, rank.unsqueeze(1).broadcast_to([128, NEXP, NEXP]), iotaE.unsqueeze(2).broadcast_to([128, NEXP, NEXP]), ALU.is_equal,
           reads=["rank", "iotaE"], writes=["big1"])
        tt("dve", big2, big1, iB, ALU.mult, reads=["big1", "iotaE"], writes=["big2"])
        S.add("dve", lambda h: h.reduce_sum(out=sig, in_=big2, axis=AX.X), reads=["big2"], writes=["sig"])
        tt("dve", dall, base, within, ALU.add, reads=["base", "within"], writes=["dall"])
        tt("dve", dall, dall, kbv.unsqueeze(1).broadcast_to([128, 16, NEXP]), ALU.add, reads=["dall", "kbv"], writes=["dall"])
        E4keys = [("E4f", i) for i in range(16)]
        for k in range(4):
            tt("dve", oh, iotaE.unsqueeze(1).broadcast_to([128, 16, NEXP]), E4f[:, :, k:k + 1].broadcast_to([128, 16, NEXP]), ALU.is_equal,
               reads=["iotaE"] + E4keys, writes=["oh"])
            tt("dve", oh, oh, dall, ALU.mult, reads=["oh", "dall"], writes=["oh"])
            S.add("dve", lambda h, k=k: h.reduce_sum(out=destF[:, k, :], in_=oh, axis=AX.X), reads=["oh"], writes=["destF"])
        cp("dve", destI, destF, reads=["destF"], writes=["destI"])
        ts("dve", sig, sig, 1024.0, None, ALU.mult, None, reads=["sig"], writes=["sig"])
        tt("dve", idxWf, sig.unsqueeze(2).broadcast_to([128, NEXP, 8]), pk.unsqueeze(1).broadcast_to([128, NEXP, 8]), ALU.add,
           reads=["sig", "pk"], writes=["idxWf"])
        cp("dve", idxW, idxWf, reads=["idxWf"], writes=["idxW"])
        pRk = bank(2)[0:NEXP, 0:128]
        tr(pRk, rank, ident_f, reads=["rank", "ident_f"], writes=[("ps", 2)])
        cp("act", rankT, pRk[:, 0:1], reads=[("ps", 2)], writes=["rankT"])
        ts("dve", RTm, iotaE[0:NEXP, :], rankT[:, 0:1], None, ALU.is_equal, None, reads=["iotaE", "rankT"], writes=["RTm"])
        for bi_, (rows, rkey, dstR, dkey) in enumerate(((bgrow, "bgrow", bgR, "bgR"), (burow, "burow", bu1R, "bu1R"))):
            pbk = bank(3 + bi_)
            for c in range(8):
                mm(pbk[:, c * NEXP:(c + 1) * NEXP], rows[:, c * 128:(c + 1) * 128], RTm, True, True, reads=[rkey, "RTm"], writes=[("ps", 3 + bi_)])
            cp("act", dstR.rearrange("p a b -> p (a b)"), pbk[:, 0:8 * NEXP], reads=[("ps", 3 + bi_)], writes=[dkey])
        ts("pool", bu1R, bu1R, 1.0, None, ALU.add, None, reads=["bu1R"], writes=["bu1R"])
        if "route" in dbg:
            dbg_out["gates"] = (gates_all, [128, 16 * NEXP], F32, [("gates", i) for i in range(16)])
            dbg_out["destI"] = (destI, [128, 64], I32, ["destI"])
            dbg_out["idxW"] = (idxW, [128, NEXP * 8], I32, ["idxW"])
            dbg_out["rank"] = (rank, [128, NEXP], F32, ["rank"])
            dbg_out["W4"] = (W4, [128, 64], F32, [("W4", i) for i in range(16)])
            dbg_out["bgR"] = (bgR, [128, 8 * NEXP], F32, ["bgR"])

    if stop_after is None or stop_after == "moe":
        for ti in range(16):
            for k in range(4):
                S.add("pool", lambda h, ti=ti, k=k: h.indirect_dma_start(
                    out=xs_scr[:, :], out_offset=bass.IndirectOffsetOnAxis(ap=destI[:, k, ti:ti + 1], axis=0),
                    in_=xnb[:, ti, :], in_offset=None), reads=["destI", ("xnb", ti)], writes=[("xs_scr", ti, k)], dma=True)
        S.barrier()
        xrow = [A.view(R5 + i * 8 * K, [128, 4, 1024], BF16) for i in range(2)]
        yrow = [A.view(R5 + 16 * K + i * 4 * K, [128, 1024], F32) for i in range(2)]
        xeT = [A.view(R1 + i * 8 * K, [128, 8, 512], BF16) for i in range(2)] + [A.view(R5 + 24 * K, [128, 8, 512], BF16)]
        actT = [A.view(R1 + 16 * K + i * 8 * K, [128, 8, 512], BF16) for i in range(2)]
        NWB = 6
        wbufs = [A.view(R2 + i * 16 * K, [128, 8, 1024], BF16) for i in range(NWB)]
        tmp_g = [A.view(MOE_TMP + i * 2 * K, [128, 512], F32) for i in range(2)]
        tmp_s = [A.view(MOE_TMP + 4 * K + i * 2 * K, [128, 512], F32) for i in range(2)]
        tmp_u = [A.view(MOE_TMP + 8 * K + i * 2 * K, [128, 512], F32) for i in range(2)]
        assert MOE_TMP + 12 * K <= R0_END, (MOE_TMP, R0_END)
        wn = [0]

        def load_w(src, r):
            i = wn[0] % NWB
            wn[0] += 1
            rows = src.rearrange("e k n -> (e k) n")
            for kc in range(8):
                S.add("pool", lambda h, i=i, kc=kc: h.indirect_dma_start(
                    out=wbufs[i][:, kc, :], out_offset=None, in_=rows[:, :],
                    in_offset=bass.IndirectOffsetOnAxis(ap=idxW[:, r, kc:kc + 1], axis=0)),
                    reads=["idxW"], writes=[("wb", i, kc)], dma=True)
            return wbufs[i], i

        def load_rank(r):
            return (load_w(wg_d, r), load_w(wu_d, r), load_w(wd_d, r))

        itc = [0]
        ytile_n = [0]

        def blk_load(blk):
            r, sb_, bidx, W = blk
            n = min(512, KCAP[r] - sb_ * 512)
            nt = n // 128
            row0 = KBASE[r] + sb_ * 512
            xr = bidx % 2
            dma("sp", xrow[xr][:, 0:nt, :], xs_scr[row0:row0 + n, :].rearrange("(t p) d -> p t d", p=128),
                reads=["xs_scr"], writes=[("xrow", xr)])

        def blk_T(blk):
            r, sb_, bidx, W = blk
            n = min(512, KCAP[r] - sb_ * 512)
            nt = n // 128
            xr, x3 = bidx % 2, bidx % 3
            for kg in range(2):
                pT = psum_t[:, 4 * 512:6 * 512].bitcast(BF16).rearrange("p (a b) -> p a b", a=4)
                for t in range(nt):
                    for k4 in range(4):
                        kc = kg * 4 + k4
                        tr(pT[:, k4, t * 128:(t + 1) * 128], xrow[xr][:, t, kc * 128:(kc + 1) * 128], ident_b,
                           reads=[("xrow", xr), "ident_b"], writes=[("ps", 4), ("ps", 5)])
                for k4 in range(4):
                    kc = kg * 4 + k4
                    evac_mod(xeT[x3][:, kc, 0:n], pT[:, k4, 0:n], opsc2[:, kc:kc + 1], sh2[:, kc:kc + 1],
                             reads=[("ps", 4), ("ps", 5), "opsc2", "modT"], writes=[("xeT", x3, kc)], eng=("act" if k4 < 2 else "dve"))

        def blk_GU(blk):
            r, sb_, bidx, W = blk
            (Wg_, Wgk), (Wu_, Wuk), (Wd_, Wdk) = W
            n = min(512, KCAP[r] - sb_ * 512)
            x3, xs2 = bidx % 3, bidx % 2
            for c in range(8):
                s2 = itc[0] % 2
                itc[0] += 1
                bg_i, bu_i = 0 + s2, 2 + s2
                pg, pu = bank(bg_i)[:, 0:n], bank(bu_i)[:, 0:n]
                for kc in range(8):
                    mm(pg, Wg_[:, kc, c * 128:(c + 1) * 128], xeT[x3][:, kc, 0:n], kc == 0, kc == 7,
                       reads=[("wb", Wgk, kc), ("xeT", x3, kc)], writes=[("ps", bg_i)])
                for kc in range(8):
                    mm(pu, Wu_[:, kc, c * 128:(c + 1) * 128], xeT[x3][:, kc, 0:n], kc == 0, kc == 7,
                       reads=[("wb", Wuk, kc), ("xeT", x3, kc)], writes=[("ps", bu_i)])
                tg, tsg, tu = tmp_g[s2][:, 0:n], tmp_s[s2][:, 0:n], tmp_u[s2][:, 0:n]
                ts("dve", tg, pg, bgR[:, c, r:r + 1], 7.0, ALU.add, ALU.min, reads=[("ps", bg_i), "bgR"], writes=[("tg", s2)])
                act(tsg, tg, AF.Sigmoid, reads=[("tg", s2)], writes=[("tsg", s2)], scale=1.702)
                act(tu, pu, AF.Identity, reads=[("ps", bu_i), "bu1R"], writes=[("tu", s2)], bias=bu1R[:, c, r:r + 1], scale=1.0)
                ts("dve", tu, tu, 8.0, -6.0, ALU.min, ALU.max, reads=[("tu", s2)], writes=[("tu", s2)])
                tt("dve", tsg, tg, tsg, ALU.mult, reads=[("tg", s2), ("tsg", s2)], writes=[("tsg", s2)])
                tt("dve", actT[xs2][:, c, 0:n], tsg, tu, ALU.mult, reads=[("tsg", s2), ("tu", s2)], writes=[("actT", xs2)])

        def blk_back(blk):
            r, sb_, bidx, W = blk
            xs2 = bidx % 2
            (Wg_, Wgk), (Wu_, Wuk), (Wd_, Wdk) = W
            n = min(512, KCAP[r] - sb_ * 512)
            nt = n // 128
            row0 = KBASE[r] + sb_ * 512
            for t in range(nt):
                ys2 = ytile_n[0] % 2
                ytile_n[0] += 1
                for hh in range(2):
                    bi = 6 + hh
                    py = bank(bi)
                    for c in range(8):
                        mm(py, actT[xs2][:, c, t * 128:(t + 1) * 128], Wd_[:, c, hh * 512:(hh + 1) * 512], c == 0, c == 7,
                           reads=[("actT", xs2), ("wb", Wdk, c)], writes=[("ps", bi)])
                    cp("act" if hh == 0 else "dve", yrow[ys2][:, hh * 512:(hh + 1) * 512], py, reads=[("ps", bi)], writes=[("yrow", ys2, hh)])
                dma("sp", ys_scr[row0 + t * 128:row0 + (t + 1) * 128, :], yrow[ys2], reads=[("yrow", ys2, 0), ("yrow", ys2, 1)], writes=[("ys_scr", ytile_n[0])])

        blist = []
        for r in range(n_experts):
            for sb_ in range((KCAP[r] + 511) // 512):
                blist.append([r, sb_, len(blist), None])
        NBLK = len(blist)
        Wsets = {0: load_rank(0)}

        def attach_w(bi_):
            r = blist[bi_][0]
            blist[bi_][3] = Wsets[r]

        blk_load(blist[0])
        if NBLK > 1:
            blk_load(blist[1])
        blk_T(blist[0])
        if NBLK > 2:
            blk_load(blist[2])
        if NBLK > 1:
            blk_T(blist[1])
        attach_w(0)
        blk_GU(blist[0])
        if 1 < n_experts:
            Wsets[1] = load_rank(1)
        for bi_ in range(NBLK):
            if bi_ + 3 < NBLK:
                blk_load(blist[bi_ + 3])
            if bi_ + 2 < NBLK:
                blk_T(blist[bi_ + 2])
            if bi_ + 1 < NBLK:
                attach_w(bi_ + 1)
                blk_GU(blist[bi_ + 1])
            blk_back(blist[bi_])
            if bi_ + 1 < NBLK and blist[bi_ + 1][1] == 0:
                r1 = blist[bi_ + 1][0]
                if r1 + 1 < n_experts and (r1 + 1) not in Wsets:
                    Wsets[r1 + 1] = load_rank(r1 + 1)
        S.barrier()
        F7 = R2
        ln2gB = A.view(F7, [128, 1024], F32)
        ln2bB = A.view(F7 + 4 * K, [128, 1024], F32)
        x1r = [A.view(F7 + 8 * K + i * 4 * K, [128, 1024], F32) for i in range(2)]
        z2 = [A.view(F7 + 16 * K + i * 4 * K, [128, 1024], F32) for i in range(2)]
        ot = [A.view(F7 + 24 * K + i * 4 * K, [128, 1024], F32) for i in range(2)]
        acct = [A.view(F7 + 32 * K + i * 4 * K, [128, 1024], F32) for i in range(2)]
        ygt = [A.view(F7 + 40 * K + i * 4 * K, [128, 1024], F32) for i in range(4)]
        bdn_s = A.view(F7 + 56 * K, [NEXP, 1024], F32, parts=NEXP)
        gT_s = [A.view(F7 + 60 * K + i * 512, [NEXP, 128], F32, parts=NEXP) for i in range(2)]
        dma("sp", ln2gB, bcast(ln2g_d[0:1, :]), writes=["ln2gB"])
        dma("sp", ln2bB, bcast(ln2b_d[0:1, :]), writes=["ln2bB"])
        dma("sp", bdn_s, bdn_d, writes=["bdn_s"])
        outs = []
        gi = 0
        gi_ = [0]
        def fin_A(ti):
            s2 = ti % 2
            dma("sp", x1r[s2], x1s_d[ti * 128:(ti + 1) * 128, :], reads=[("x1s", ti)], writes=[("x1r", s2)])
            pG = bank(4 + s2)[0:NEXP, 0:128]
            tr(pG, gates_all[:, ti, :], ident_f, reads=[("gates", ti), "ident_f"], writes=[("ps", 4 + s2)])
            cp("act", gT_s[s2], pG, reads=[("ps", 4 + s2)], writes=[("gT_s", s2)])
            for hh in range(2):
                bi = hh + 2 * s2
                pb = bank(bi)
                mm(pb, gT_s[s2], bdn_s[:, hh * 512:(hh + 1) * 512], True, True, reads=[("gT_s", s2), "bdn_s"], writes=[("ps", bi)])
                cp("act", acct[s2][:, hh * 512:(hh + 1) * 512], pb, reads=[("ps", bi)], writes=[("acct", s2)])
            for k in range(4):
                g4 = gi_[0] % 4
                gi_[0] += 1
                S.add("pool", lambda h, ti=ti, k=k, g4=g4: h.indirect_dma_start(
                    out=ygt[g4][:, :], out_offset=None, in_=ys_scr[:, :],
                    in_offset=bass.IndirectOffsetOnAxis(ap=destI[:, k, ti:ti + 1], axis=0)),
                    reads=["destI"], writes=[("ygt", g4)], dma=True)
                stt(acct[s2], ygt[g4], W4[:, ti, k:k + 1], acct[s2], ALU.mult, ALU.add,
                    reads=[("ygt", g4), ("W4", ti), ("acct", s2)], writes=[("acct", s2)])

        def fin_B(ti):
            s2 = ti % 2
            tt("dve", z2[s2], acct[s2], G2B, ALU.mult, reads=[("acct", s2), ("GB", 1)], writes=[("z2", s2)])
            stt(z2[s2], x1r[s2], ALPHA, z2[s2], ALU.mult, ALU.add, reads=[("x1r", s2), ("z2", s2)], writes=[("z2", s2)])
            ln_stats(z2[s2], s2, 0, ("z2", s2))
            ln_finish(s2, 1)
            act(z2[s2], z2[s2], AF.Identity, reads=[("z2", s2), ("rstd", s2), ("nmr", s2)], writes=[("z2", s2)],
                bias=nmr_[s2][:, 0:1], scale=rstd_[s2][:, 0:1])
            tt("dve", z2[s2], z2[s2], ln2gB, ALU.mult, reads=[("z2", s2), "ln2gB"], writes=[("z2", s2)])
            tt("dve", ot[s2], z2[s2], ln2bB, ALU.add, reads=[("z2", s2), "ln2bB"], writes=[("ot", s2)])
            dma("sp", out_d[ti * 128:(ti + 1) * 128, :], ot[s2], reads=[("ot", s2)], writes=[("out", ti)])
            outs.append(("out", ti))

        fin_A(0)
        for ti in range(16):
            if ti + 1 < 16:
                S.zipped(lambda: fin_A(ti + 1), lambda: fin_B(ti))
            else:
                fin_B(ti)
        S.add("sp", None, reads=outs)

    dkeys = []
    for name, spec in dbg_out.items():
        if spec is None:
            continue
        ap, shape, dt, keys = spec
        dd = nc.dram_tensor("dbg_" + name, shape, dt, kind="ExternalOutput").ap()
        flat = ap
        if len(ap.shape) == 3:
            flat = ap.rearrange("p a b -> p (a b)")
        dma("sp", dd, flat, reads=keys, writes=[("dbg", name)])
        dkeys.append(("dbg", name))
    if dkeys:
        S.add("sp", None, reads=dkeys)
    S.emit()
    st.close()
    return nc


def t5_bucket_np(dist):
    d = dist.astype(np.float32)
    large = np.float32(16) + np.log(np.maximum(d, np.float32(16.0)) / np.float32(16)) / np.float32(math.log(2048 / 16)) * np.float32(16)
    large = np.minimum(large.astype(np.int32), 31)
    return np.where(dist < 16, dist, large)


def host_layout(inp):
    f = np.float32
    x = np.asarray(inp["x"], f)
    shared = {}
    shared["w_ada"] = np.ascontiguousarray(np.asarray(inp["w_ada"], f)[0])
    shared["b_adaT"] = np.ascontiguousarray(np.asarray(inp["b_ada"], f)[0].reshape(48, 128).T)
    shared["w_in"] = np.ascontiguousarray(np.asarray(inp["w_in"], f)[0])
    shared["gm_g"] = np.asarray(inp["gm_ln_g"], f).reshape(1, 512)
    shared["gm_b"] = np.asarray(inp["gm_ln_b"], f).reshape(1, 512)
    ws = np.asarray(inp["gm_w_s"], f)[0]
    shared["wsT"] = np.ascontiguousarray(ws.transpose(2, 0, 1).reshape(128, 8 * 128))
    bs = np.asarray(inp["gm_b_s"], f)[0]
    bsp = np.empty((128, 4, 128), f)
    for gp in range(4):
        bsp[0:64, gp, :] = bs[2 * gp][None, :]
        bsp[64:128, gp, :] = bs[2 * gp + 1][None, :]
    shared["bsp"] = bsp.reshape(128, 512)
    shared["wa"] = np.ascontiguousarray(np.asarray(inp["w_branch_a"], f)[0])
    shared["wb"] = np.ascontiguousarray(np.asarray(inp["w_branch_b"], f)[0])
    shared["wo"] = np.ascontiguousarray(np.asarray(inp["w_out"], f)[0])
    rel = np.asarray(inp["rel_bias"], f)
    kk = np.arange(128)[:, None]
    qq = np.arange(128)[None, :]
    d_prev = qq + 128 - kk
    d_cur = qq - kk
    biasT = np.empty((128, 24, 2, 128), f)
    for g in range(3):
        bp = t5_bucket_np(np.clip(d_prev, 0, None).astype(np.int32) * DIL[g])
        bc = t5_bucket_np(np.clip(d_cur, 0, None).astype(np.int32) * DIL[g])
        for h in range(8):
            biasT[:, g * 8 + h, 0, :] = rel[bp, g * 8 + h]
            biasT[:, g * 8 + h, 1, :] = rel[bc, g * 8 + h]
    shared["biasT"] = biasT.reshape(128, 24 * 256)
    maskT = np.zeros((128, 2, 128), f)
    maskT[:, 0, :][d_prev > 128] = -1e30
    maskT[:, 1, :][d_cur < 0] = -1e30
    shared["maskT"] = maskT.reshape(128, 256)
    shared["ln1g"] = np.asarray(inp["ln1_g"], f).reshape(1, D)
    shared["ln1b"] = np.asarray(inp["ln1_b"], f).reshape(1, D)
    shared["wr"] = np.ascontiguousarray(np.asarray(inp["w_router"], f)[0])
    shared["br"] = np.asarray(inp["b_router"], f).reshape(1, NEXP)
    shared["wg"] = np.ascontiguousarray(np.asarray(inp["w_gate"], f)[0])
    shared["wu"] = np.ascontiguousarray(np.asarray(inp["w_up"], f)[0])
    shared["wd"] = np.ascontiguousarray(np.asarray(inp["w_down"], f)[0])
    shared["bg"] = np.ascontiguousarray(np.asarray(inp["b_gate"], f)[0])
    shared["bu"] = np.ascontiguousarray(np.asarray(inp["b_up"], f)[0])
    shared["kbtab"] = np.broadcast_to(np.asarray(KBASE, f)[None, :], (128, NEXP)).copy()
    shared["bdn"] = np.ascontiguousarray(np.asarray(inp["b_down"], f)[0])
    shared["ln2g"] = np.asarray(inp["ln2_g"], f).reshape(1, D)
    shared["ln2b"] = np.asarray(inp["ln2_b"], f).reshape(1, D)
    shared["ident"] = np.eye(128, dtype=f)
    c = np.asarray(inp["c"], f)
    in_maps = []
    for k in range(NCORES):
        b, hf = k // 2, k % 2
        m = dict(shared)
        m["xo"] = np.ascontiguousarray(x[b, hf * NTOK:(hf + 1) * NTOK])
        m["xc"] = np.ascontiguousarray(x[b, 0:NTOK]) if hf == 1 else np.zeros((NTOK, D), f)
        m["flag"] = np.full((128, 1), float(hf), f)
        m["cT"] = np.ascontiguousarray(c[b].reshape(8, 128).T)
        in_maps.append(m)
    return in_maps


_NC_CACHE = {}


def kernel(**inputs):
    in_maps = host_layout(inputs)
    if "nc" not in _NC_CACHE:
        _NC_CACHE["nc"] = build()
    nc = _NC_CACHE["nc"]
    res = run_bass_kernel_spmd(nc, in_maps, core_ids=list(range(NCORES)))
    out = np.empty((4, 4096, D), np.float32)
    for k in range(NCORES):
        b, hf = k // 2, k % 2
        out[b, hf * NTOK:(hf + 1) * NTOK] = np.asarray(res.results[k]["out"], np.float32)
    return out
```

```python
import contextlib
import math
import numpy as np
import concourse.bass as bass
import concourse.mybir as mybir
from concourse.bass_utils import run_bass_kernel_spmd

F32 = mybir.dt.float32
BF16 = mybir.dt.bfloat16
I32 = mybir.dt.int32
U32 = mybir.dt.uint32
AF = mybir.ActivationFunctionType
ALU = mybir.AluOpType
AX = mybir.AxisListType

D = 1024
NTOK = 2048
NCORES = 8
NEXP = 32
DIL = (1, 4, 16)
ALPHA = 2.0 ** 0.25
EPS = 1e-5
IN_COLS = 7680
import os
PIPE_ATTN = os.environ.get('PIPE_ATTN', '1') == '1'
ZIP_STAGES = os.environ.get('ZIP_STAGES', '1') == '1'
KCAP = [((min(2048, 8192 // (r + 1)) + 127) // 128) * 128 for r in range(32)]
KBASE = [sum(KCAP[:r]) for r in range(32)]
NSLOT = sum(KCAP)

ENGINES = ("pe", "act", "dve", "pool", "sp")
COMPUTE = ("pe", "act", "dve", "pool")
HANDLES = {"pe": "tensor", "act": "scalar", "dve": "vector", "pool": "gpsimd", "sp": "sync"}


class Sched:
    NDMASEM = 12
    SAME_ENG_WINDOW = 2

    def __init__(self, nc):
        self.nc = nc
        self.ops = {e: [] for e in ENGINES}
        self.lastw = {}
        self.readers = {}
        self.capture = None

    def add(self, eng, fn, reads=(), writes=(), dma=False):
        if self.capture is not None:
            self.capture.append((eng, fn, tuple(reads), tuple(writes), dma))
            return None
        ops = self.ops[eng]
        idx = len(ops)
        deps = {}
        dmadeps = set()

        def anydep(p):
            if p == (eng, idx):
                return
            if self.ops[p[0]][p[1]]["dma"]:
                dmadeps.add(p)
            elif deps.get(p[0], -1) < p[1]:
                deps[p[0]] = p[1]

        for k in reads:
            w = self.lastw.get(k)
            if w is not None:
                anydep(w)
        for k in writes:
            w = self.lastw.get(k)
            if w is not None:
                anydep(w)
            for r in self.readers.get(k, ()):
                anydep(r)
        for k in reads:
            self.readers.setdefault(k, []).append((eng, idx))
        for k in writes:
            self.lastw[k] = (eng, idx)
            self.readers[k] = []
        if eng in deps and not dma:
            if eng == "pe" or deps[eng] < idx - self.SAME_ENG_WINDOW:
                del deps[eng]
        ops.append(dict(fn=fn, deps=deps, dmadeps=sorted(dmadeps), dma=dma, marked=False))
        return (eng, idx)

    def zipped(self, *stage_fns):
        if not ZIP_STAGES:
            for f in stage_fns:
                f()
            return
        lists = []
        for f in stage_fns:
            self.capture = []
            f()
            lists.append(self.capture)
            self.capture = None
        items = []
        for li, l in enumerate(lists):
            n = len(l)
            for i, op in enumerate(l):
                items.append(((i + 0.5) / max(n, 1), li, i, op))
        items.sort(key=lambda x: (x[0], x[1], x[2]))
        for _, _, _, op in items:
            self.add(*op)

    def barrier(self):
        last = {}
        for e in COMPUTE:
            for i in range(len(self.ops[e]) - 1, -1, -1):
                op = self.ops[e][i]
                if op["fn"] is not None and not op["dma"]:
                    last[e] = i
                    break
        dmas = []
        for e in ENGINES:
            seen = 0
            for i in range(len(self.ops[e]) - 1, -1, -1):
                if self.ops[e][i]["dma"]:
                    dmas.append((e, i))
                    seen += 1
                    if seen >= self.NDMASEM:
                        break
        for e in ENGINES:
            if not self.ops[e]:
                continue
            deps = {p: i for p, i in last.items() if p != e}
            self.ops[e].append(dict(fn=None, deps=deps, dmadeps=sorted(dmas), dma=False, marked=False))
        self.lastw = {}
        self.readers = {}

    def emit(self):
        nc = self.nc
        ops = self.ops
        for e in ENGINES:
            for op in ops[e]:
                for pe_, pi in op["deps"].items():
                    ops[pe_][pi]["marked"] = True
        for e in COMPUTE:
            c = 0
            for op in ops[e]:
                if op["dma"] or op["fn"] is None:
                    op["cnt"] = c
                    continue
                if op["marked"]:
                    c += 1
                op["cnt"] = c
        stack = contextlib.ExitStack()
        csem = {e: stack.enter_context(nc.semaphore("cs_" + e)) for e in COMPUTE}
        for e in ENGINES:
            n = 0
            ring = None
            for op in ops[e]:
                if not op["dma"]:
                    continue
                if ring is None:
                    ring = [stack.enter_context(nc.semaphore("ds_%s_%d" % (e, i))) for i in range(self.NDMASEM)]
                s = n % self.NDMASEM
                k = n // self.NDMASEM
                op["dsem"] = ring[s]
                op["dsem_id"] = (e, s)
                op["dtarget"] = 16 * (k + 1)
                op["dprev"] = 16 * k
                n += 1
        block = stack.enter_context(nc.Block())

        def make(e):
            def body(h):
                waited = {}

                def wait(key, sem, val):
                    if val <= 0 or waited.get(key, 0) >= val:
                        return
                    waited[key] = val
                    h.wait_ge(sem, val)

                for op in ops[e]:
                    for pe_, pi in op["deps"].items():
                        wait(("c", pe_), csem[pe_], ops[pe_][pi]["cnt"])
                    for (qe, qi) in op["dmadeps"]:
                        q = ops[qe][qi]
                        wait(("d",) + q["dsem_id"], q["dsem"], q["dtarget"])
                    if op["fn"] is None:
                        continue
                    if op["dma"]:
                        wait(("d",) + op["dsem_id"], op["dsem"], op["dprev"])
                        ins = op["fn"](h)
                        ins.then_inc(op["dsem"], 16)
                    else:
                        ins = op["fn"](h)
                        if op["marked"]:
                            ins.then_inc(csem[e], 1)
            return body

        for e in ENGINES:
            if ops[e]:
                getattr(block, HANDLES[e])(make(e))
        stack.close()


class Arena:
    def __init__(self, ap_f32, nbytes):
        self.ap = ap_f32
        self.nbytes = nbytes

    def view(self, off, shape, dtype, parts=128):
        esz = 2 if dtype == BF16 else 4
        n = int(np.prod(shape[1:]))
        assert off % 4 == 0 and off + n * esz <= self.nbytes, (off, shape, self.nbytes)
        nw = (n * esz + 3) // 4
        v = self.ap[0:parts, off // 4: off // 4 + nw]
        if dtype != F32:
            v = v.bitcast(dtype)
        if len(shape) == 3:
            v = v.rearrange("p (a b) -> p a b", a=shape[1])
        elif len(shape) == 4:
            v = v.rearrange("p (a b c) -> p a b c", a=shape[1], b=shape[2])
        return v


def build(stop_after=None, n_experts=NEXP, dbg=()):
    nc = bass.Bass("TRN2", target_bir_lowering=False)

    def din(name, shape, dt=F32):
        return nc.dram_tensor(name, list(shape), dt, kind="ExternalInput").ap()

    xo = din("xo", [NTOK, D])
    xc = din("xc", [NTOK, D])
    flag_d = din("flag", [128, 1])
    cT_d = din("cT", [128, 8])
    w_ada = din("w_ada", [D, 6 * D])
    b_adaT = din("b_adaT", [128, 48])
    w_in = din("w_in", [D, IN_COLS])
    gm_g = din("gm_g", [1, 512])
    gm_b = din("gm_b", [1, 512])
    wsT_d = din("wsT", [128, 8 * 128])
    bsp_d = din("bsp", [128, 4 * 128])
    wa_d = din("wa", [512, D])
    wb_d = din("wb", [512, D])
    wo_d = din("wo", [D, D])
    biasT_d = din("biasT", [128, 24 * 256])
    maskT_d = din("maskT", [128, 256])
    ln1g_d = din("ln1g", [1, D])
    ln1b_d = din("ln1b", [1, D])
    wr_d = din("wr", [D, NEXP])
    br_d = din("br", [1, NEXP])
    wg_d = din("wg", [n_experts, D, D])
    wu_d = din("wu", [n_experts, D, D])
    wd_d = din("wd", [n_experts, D, D])
    bg_d = din("bg", [NEXP, D])
    bu_d = din("bu", [NEXP, D])
    kbtab_d = din("kbtab", [128, NEXP])
    bdn_d = din("bdn", [NEXP, D])
    ln2g_d = din("ln2g", [1, D])
    ln2b_d = din("ln2b", [1, D])
    ident_d = din("ident", [128, 128])
    out_d = nc.dram_tensor("out", [NTOK, D], F32, kind="ExternalOutput").ap()
    x1s_d = nc.dram_tensor("x1s", [NTOK, D], F32, kind="Internal").ap()
    xs_scr = nc.dram_tensor("xs_scr", [NSLOT, D], BF16, kind="Internal").ap()
    ys_scr = nc.dram_tensor("ys_scr", [NSLOT, D], F32, kind="Internal").ap()
    dbg_out = {}

    w_in_v = w_in.rearrange("(kc p) n -> p kc n", p=128)
    w_ada_v = w_ada.rearrange("(kc p) n -> p kc n", p=128)

    st = contextlib.ExitStack()
    ARENA_BYTES = 207 * 1024
    arena_t = st.enter_context(nc.sbuf_tensor("arena", [128, ARENA_BYTES // 4], F32))
    A = Arena(arena_t, ARENA_BYTES)
    psum_t = st.enter_context(nc.psum_tensor("psum", [128, 4096], F32))

    def bank(i, shape=None, dtype=F32, half=None):
        v = psum_t[:, i * 512:(i + 1) * 512]
        if dtype != F32:
            v = v.bitcast(dtype)
        if shape is not None and len(shape) == 3:
            v = v.rearrange("p (a b) -> p a b", a=shape[1])
        return v

    S = Sched(nc)
    K = 1024

    off = [0]

    def palloc(shape, dtype, parts=128):
        esz = 2 if dtype == BF16 else 4
        n = int(np.prod(shape[1:])) * esz
        n = (n + 3) // 4 * 4
        v = A.view(off[0], shape, dtype, parts)
        off[0] += n
        return v

    ident_f = palloc([128, 128], F32)
    ident_b = palloc([128, 128], BF16)
    ones_f = palloc([128, 128], F32)
    ones_b = palloc([128, 64], BF16)
    onesf_b = palloc([128, 64], BF16)
    flag_s = palloc([128, 1], F32)
    cT_s = palloc([128, 8], F32)
    scT = palloc([128, 8], F32)
    scT_b = palloc([128, 8], BF16)
    badaT = palloc([128, 48], F32)
    modT = palloc([128, 48], F32)
    opsc1 = palloc([128, 8], F32)
    opsc2 = palloc([128, 8], F32)
    G1B = palloc([128, 1024], F32)
    G2B = palloc([128, 1024], F32)
    gates_all = palloc([128, 16, NEXP], F32)
    bgR = palloc([128, 8, NEXP], F32)
    bu1R = palloc([128, 8, NEXP], F32)
    E4f = palloc([128, 16, 4], F32)
    W4 = palloc([128, 16, 4], F32)
    destI = palloc([128, 4, 16], I32)
    idxW = palloc([128, NEXP, 8], I32)
    Mall = palloc([128, 16, NEXP], BF16)
    mi8_ = [palloc([128, 8], U32) for _ in range(2)]
    e4_ = [palloc([128, 4], F32) for _ in range(2)]
    maskT = palloc([128, 256], F32)
    stt_ = [palloc([128, 4, 2, 6], F32) for _ in range(2)]
    mv_ = [palloc([128, 4, 2], F32) for _ in range(2)]
    sd_ = [palloc([128, 4], F32) for _ in range(2)]
    rstd_ = [palloc([128, 4], F32) for _ in range(2)]
    nmr_ = [palloc([128, 4], F32) for _ in range(2)]
    mx8_ = [palloc([128, 8], F32) for _ in range(2)]
    negm_ = [palloc([128, 1], F32) for _ in range(2)]
    den_ = [palloc([128, 1], F32) for _ in range(2)]
    rden_ = [palloc([128, 1], F32) for _ in range(2)]
    lg_ = [palloc([128, NEXP], F32) for _ in range(2)]
    msk_ = [palloc([128, NEXP], F32) for _ in range(2)]
    ex_ = [palloc([128, NEXP], F32) for _ in range(2)]
    em_ = [palloc([128, NEXP], F32) for _ in range(2)]
    brB = palloc([128, NEXP], F32)
    R0_END = 32 * K
    assert off[0] <= R0_END, off[0]
    MOE_TMP = off[0]

    def dma(q, out, in_, reads=(), writes=()):
        return S.add(q, lambda h: h.dma_start(out=out, in_=in_), reads=reads, writes=writes, dma=True)

    def mm(out, lhsT, rhs, start, stop, reads, writes):
        return S.add("pe", lambda h: h.matmul(out, lhsT=lhsT, rhs=rhs, start=start, stop=stop), reads=reads, writes=writes)

    def tr(out, in_, ident, reads, writes):
        return S.add("pe", lambda h: h.transpose(out, in_, ident), reads=reads, writes=writes)

    def act(out, in_, func, reads, writes, bias=None, scale=None):
        kw = {}
        if bias is not None:
            kw["bias"] = bias
        if scale is not None:
            kw["scale"] = scale
        return S.add("act", lambda h: h.activation(out=out, in_=in_, func=func, **kw), reads=reads, writes=writes)

    def tt(eng, out, in0, in1, op, reads, writes):
        return S.add(eng, lambda h: h.tensor_tensor(out=out, in0=in0, in1=in1, op=op), reads=reads, writes=writes)

    def ts(eng, out, in0, s1, s2, op0, op1, reads, writes):
        if op1 is None:
            return S.add(eng, lambda h: h.tensor_scalar(out=out, in0=in0, scalar1=s1, scalar2=None, op0=op0), reads=reads, writes=writes)
        return S.add(eng, lambda h: h.tensor_scalar(out=out, in0=in0, scalar1=s1, scalar2=s2, op0=op0, op1=op1), reads=reads, writes=writes)

    def stt(out, in0, scalar, in1, op0, op1, reads, writes):
        return S.add("dve", lambda h: h.scalar_tensor_tensor(out=out, in0=in0, scalar=scalar, in1=in1, op0=op0, op1=op1), reads=reads, writes=writes)

    def cp(eng, out, in_, reads, writes):
        if eng == "act":
            return S.add("act", lambda h: h.activation(out=out, in_=in_, func=AF.Copy), reads=reads, writes=writes)
        return S.add(eng, lambda h: h.tensor_copy(out=out, in_=in_), reads=reads, writes=writes)

    def memset(eng, ap, val, writes):
        return S.add(eng, lambda h: h.memset(ap, val), writes=writes)

    def bcast(row_ap):
        return row_ap.partition_broadcast(128)

    def ln_stats(x_ap, slot, t, key_in, nhalf=2):
        for hh in range(nhalf):
            S.add("dve", lambda h, hh=hh: h.bn_stats(out=stt_[slot][:, t, hh, :], in_=x_ap[:, hh * 512:(hh + 1) * 512]),
                  reads=[key_in], writes=[("st", slot, t, hh)])
        src = stt_[slot][:, t, 0:nhalf, :].rearrange("p a b -> p (a b)")
        S.add("dve", lambda h: h.bn_aggr(out=mv_[slot][:, t, :], in_=src),
              reads=[("st", slot, t, hh) for hh in range(nhalf)], writes=[("mv", slot, t)])

    def ln_finish(slot, nt):
        act(sd_[slot][:, 0:nt], mv_[slot][:, 0:nt, 1], AF.Ln, reads=[("mv", slot, t) for t in range(nt)] + ["eps"],
            writes=[("sd", slot)], bias=EPS_AP[0], scale=1.0)
        act(rstd_[slot][:, 0:nt], sd_[slot][:, 0:nt], AF.Exp, reads=[("sd", slot)], writes=[("rstd", slot)], scale=-0.5)
        stt(nmr_[slot][:, 0:nt], mv_[slot][:, 0:nt, 0], -1.0, rstd_[slot][:, 0:nt], ALU.mult, ALU.mult,
            reads=[("mv", slot, t) for t in range(nt)] + [("rstd", slot)], writes=[("nmr", slot)])

    EPS_AP = [None]
    eps_t = A.view(MOE_TMP, [128, 1], F32)
    EPS_AP[0] = eps_t
    MOE_TMP += 4

    dma("sp", ident_f, ident_d, writes=["ident_f"])
    dma("sp", flag_s, flag_d, writes=["flag"])
    dma("sp", cT_s, cT_d, writes=["cT"])
    dma("sp", badaT, b_adaT, writes=["badaT"])
    dma("sp", maskT, maskT_d, writes=["maskT"])
    memset("dve", eps_t, EPS, writes=["eps"])
    memset("dve", ones_f, 1.0, writes=["ones_f"])
    memset("dve", ones_b, 1.0, writes=["ones_b"])
    cp("dve", ident_b, ident_f, reads=["ident_f"], writes=["ident_b"])
    ts("dve", onesf_b, ones_b, flag_s[:, 0:1], None, ALU.mult, None, reads=["flag", "ones_b"], writes=["onesf_b"])
    act(scT, cT_s, AF.Silu, reads=["cT"], writes=["scT"])
    cp("dve", scT_b, scT, reads=["scT"], writes=["scT_b"])

    R5 = 144 * K
    wad = [A.view(R5 + i * 8 * K, [128, 8, 512], BF16) for i in range(4)]
    psM = bank(7)
    for blk in range(12):
        wbuf = wad[blk % 4]
        dma("pool", wbuf, w_ada_v[:, :, blk * 512:(blk + 1) * 512], writes=[("wad", blk % 4)])
        for jj in range(4):
            j = blk * 4 + jj
            for kc in range(8):
                mm(psM[:, j:j + 1], wbuf[:, kc, jj * 128:(jj + 1) * 128], scT_b[:, kc:kc + 1], kc == 0, kc == 7,
                   reads=[("wad", blk % 4), "scT_b"], writes=[("ps", 7)])
    tt("dve", modT, psM[:, 0:48], badaT, ALU.add, reads=[("ps", 7), "badaT"], writes=["modT"])
    ts("dve", opsc1, modT[:, 8:16], 1.0, None, ALU.add, None, reads=["modT"], writes=["opsc1"])
    ts("dve", opsc2, modT[:, 32:40], 1.0, None, ALU.add, None, reads=["modT"], writes=["opsc2"])
    dg = [A.view(R5 + 32 * K + i * 512, [128, 128], F32) for i in range(2)]
    for gi, (GB, col0) in enumerate(((G1B, 16), (G2B, 40))):
        for c in range(8):
            d_ = dg[c % 2]
            ts("dve", d_, ident_f, modT[:, col0 + c:col0 + c + 1], None, ALU.mult, None,
               reads=["ident_f", "modT"], writes=[("dg", c % 2)])
            pb = bank(gi * 2 + c // 4)
            mm(pb[:, (c % 4) * 128:(c % 4 + 1) * 128], ones_f, d_, True, True,
               reads=["ones_f", ("dg", c % 2)], writes=[("ps", gi * 2 + c // 4)])
        for hh in range(2):
            cp("dve", GB[:, hh * 512:(hh + 1) * 512], bank(gi * 2 + hh), reads=[("ps", gi * 2 + hh)], writes=[("GB", gi)])
    sh1 = modT[:, 0:8]
    sh2 = modT[:, 24:32]

    R1 = 32 * K
    R2 = 64 * K
    hT = A.view(R2, [128, 8, 4096], BF16)
    R3 = R2 + 64 * K
    uT = A.view(R3, [128, 4, NTOK], BF16)
    R4 = R3 + 16 * K
    ybT = A.view(R4, [128, 4, NTOK], BF16)
    R5 = R4 + 16 * K
    xs = [A.view(R1 + i * 4 * K, [128, 1024], F32) for i in range(8)]
    xn = [A.view(180 * K + i * 2 * K, [128, 1024], BF16) for i in range(4)]

    def x_tile(tt_):
        return xc[tt_ * 128:(tt_ + 1) * 128, :] if tt_ < 16 else xo[(tt_ - 16) * 128:(tt_ - 15) * 128, :]

    evac_flip = [0]

    def evac_mod(out, in_, scale_ap, bias_ap, reads, writes, eng=None):
        if eng is None:
            evac_flip[0] ^= 1
            eng = "act" if evac_flip[0] else "dve"
        if eng == "act":
            act(out, in_, AF.Identity, reads=reads, writes=writes, bias=bias_ap, scale=scale_ap)
        else:
            ts("dve", out, in_, scale_ap, bias_ap, ALU.mult, ALU.add, reads=reads, writes=writes)

    for b in range(8):
        slot = b % 2
        xo_ = (b % 2) * 4
        for t in range(4):
            tile = b * 4 + t
            dma("sp", xs[xo_ + t], x_tile(tile), writes=[("xs", xo_ + t)])
            ln_stats(xs[xo_ + t], slot, t, ("xs", xo_ + t))
        ln_finish(slot, 4)
        for hb in range(2):
            b0 = 2 * (hb + 2 * (b % 2))
            pT = psum_t[:, b0 * 512:(b0 + 2) * 512].bitcast(BF16).rearrange("p (a b) -> p a b", a=8)
            pkey = ("ps", b0)
            for t2 in range(2):
                t = hb * 2 + t2
                act(xn[t], xs[xo_ + t], AF.Identity, reads=[("xs", xo_ + t), ("rstd", slot), ("nmr", slot)], writes=[("xn", t)],
                    bias=nmr_[slot][:, t:t + 1], scale=rstd_[slot][:, t:t + 1])
                for kc in range(8):
                    tr(pT[:, kc, t2 * 128:(t2 + 1) * 128], xn[t][:, kc * 128:(kc + 1) * 128], ident_b,
                       reads=[("xn", t), "ident_b"], writes=[pkey, ("ps", b0 + 1)])
            tok0 = b * 512 + hb * 256
            for kc in range(8):
                evac_mod(hT[:, kc, tok0:tok0 + 256], pT[:, kc, :], opsc1[:, kc:kc + 1], sh1[:, kc:kc + 1],
                         reads=[pkey, ("ps", b0 + 1), "opsc1", "modT"], writes=[("hT", b, kc)], eng=("act" if kc < 4 else "dve"))

    if "hT" in dbg:
        dbg_out["hT"] = (hT, [128, 8 * 4096], BF16, [("hT", b, kc) for b in range(8) for kc in range(8)])

    NWR = 7
    wring = [A.view(R5 + i * 2 * K, [128, 8, 128], BF16) for i in range(NWR)]
    wr_n = [0]

    def load_chunk(src_v, col0, ncols=128):
        i = wr_n[0] % NWR
        wr_n[0] += 1
        dma("pool", wring[i][:, :, 0:ncols], src_v[:, :, col0:col0 + ncols], writes=[("wring", i)])
        return wring[i], ("wring", i)

    ps_n = [0]

    def next_bank(lo=0, n=2):
        i = lo + ps_n[0] % n
        ps_n[0] += 1
        return i

    def proj_T(wbuf, wkey, tb, nk=8, src=None, srckeys=None, bank_lo=0, bank_n=2):
        bi = next_bank(bank_lo, bank_n)
        pb = bank(bi)
        for kc in range(nk):
            mm(pb, wbuf[:, kc, :], hT[:, kc, tb * 512:(tb + 1) * 512], kc == 0, kc == nk - 1,
               reads=[wkey, ("hT", tb, kc)], writes=[("ps", bi)])
        return pb, ("ps", bi)

    if stop_after != "hT":
        GM = R1
        gmgB = A.view(GM, [128, 512], F32)
        gmbB = A.view(GM + 2 * K, [128, 512], F32)
        BSp = A.view(GM + 4 * K, [128, 4, 128], F32)
        wsT_f = A.view(GM + 6 * K, [128, 8, 128], F32)
        wsT_b = A.view(GM + 10 * K, [128, 8, 128], BF16)
        vg = [A.view(GM + 12 * K + i * 2 * K, [128, 512], F32) for i in range(2)]
        zz = [A.view(GM + 16 * K + i * 2 * K, [128, 512], F32) for i in range(2)]
        vn = [A.view(GM + 20 * K + i * K, [128, 512], BF16) for i in range(2)]
        tmpg = [A.view(GM + 22 * K + i * 2 * K, [128, 4, 128], F32) for i in range(2)]
        Wvg = A.view(R5 + 14 * K, [128, 8, 512], BF16)
        S.barrier()
        dma("sp", gmgB, bcast(gm_g[0:1, :]), writes=["gmgB"])
        dma("sp", gmbB, bcast(gm_b[0:1, :]), writes=["gmbB"])
        dma("sp", BSp, bsp_d.rearrange("p (a b) -> p a b", a=4), writes=["BSp"])
        dma("sp", wsT_f, wsT_d.rearrange("p (a b) -> p a b", a=8), writes=["wsT_f"])
        S.add("pool", lambda h: h.affine_select(out=wsT_f, in_=wsT_f, pattern=[[0, 8], [1, 128]], compare_op=ALU.is_ge,
                                                 fill=0.0, base=0, channel_multiplier=-1),
              reads=["wsT_f"], writes=["wsT_f"])
        cp("pool", wsT_b, wsT_f, reads=["wsT_f"], writes=["wsT_b"])
        dma("pool", Wvg, w_in_v[:, :, 512:1024], writes=["Wvg"])
        for c in range(4):
            wbuf, wkey = load_chunk(w_in_v, c * 128)
            for tb in range(4, 8):
                pb, pkey = proj_T(wbuf, wkey, tb)
                act(uT[:, c, (tb - 4) * 512:(tb - 3) * 512], pb, AF.Gelu_apprx_tanh, reads=[pkey], writes=[("uT", tb - 4)])
        def gmlpA(ti):
            tile = 16 + ti
            sl = ti % 2
            bi = next_bank(0, 2)
            pb = bank(bi)
            for kc in range(8):
                mm(pb, hT[:, kc, tile * 128:(tile + 1) * 128], Wvg[:, kc, :], kc == 0, kc == 7,
                   reads=[("hT", tile // 4, kc), "Wvg"], writes=[("ps", bi)])
            act(vg[sl], pb, AF.Gelu_apprx_tanh, reads=[("ps", bi)], writes=[("vg", sl)])
            ln_stats(vg[sl], sl, 0, ("vg", sl), nhalf=1)
            ln_finish(sl, 1)
            act(zz[sl], vg[sl], AF.Identity, reads=[("vg", sl), ("rstd", sl), ("nmr", sl)], writes=[("zz", sl)],
                bias=nmr_[sl][:, 0:1], scale=rstd_[sl][:, 0:1])
            tt("dve", zz[sl], zz[sl], gmgB, ALU.mult, reads=[("zz", sl), "gmgB"], writes=[("zz", sl)])
            tt("dve", vn[sl], zz[sl], gmbB, ALU.add, reads=[("zz", sl), "gmbB"], writes=[("vn", sl)])

        def gmlpB(ti):
            sl = ti % 2
            bs_i = 2 + ti % 2
            pS = bank(bs_i, [128, 4, 128])
            for gp in range(4):
                for hh in range(2):
                    g_ = 2 * gp + hh
                    mm(pS[hh * 64:(hh + 1) * 64, gp, :], vn[sl][:, g_ * 64:(g_ + 1) * 64], wsT_b[:, g_, :], True, True,
                       reads=[("vn", sl), "wsT_b"], writes=[("ps", bs_i)])
            tt("dve", tmpg[sl], pS, BSp, ALU.add, reads=[("ps", bs_i), "BSp"], writes=[("tmpg", sl)])
            tt("dve", uT[:, :, ti * 128:(ti + 1) * 128], tmpg[sl], uT[:, :, ti * 128:(ti + 1) * 128], ALU.mult,
               reads=[("tmpg", sl), ("uT", ti // 4)], writes=[("uT", ti // 4)])

        gmlpA(0)
        for ti in range(16):
            if ti + 1 < 16:
                S.zipped(lambda: gmlpA(ti + 1), lambda: gmlpB(ti))
            else:
                gmlpB(ti)
        if "yaT" in dbg:
            dbg_out["yaT"] = (uT, [128, 4 * NTOK], BF16, [("uT", i) for i in range(4)])

    if stop_after not in ("hT", "gmlp"):
        AT = R1
        accND = A.view(AT, [128, 2, NTOK], F32)
        sbS = [A.view(AT + 16 * K + i * K, [128, 256], F32) for i in range(4)]
        ptS = [A.view(AT + 20 * K + i * 512, [128, 256], BF16) for i in range(4)]
        braw = [A.view(AT + 22 * K + i * 2 * K, [128, 2, 256], F32) for i in range(2)]
        biasm = [A.view(AT + 26 * K + i * 2 * K, [128, 2, 256], F32) for i in range(2)]
        QKV0 = R5 + 14 * K
        QT = [A.view(QKV0, [128, NTOK], BF16), A.view(QKV0 + 20 * K, [128, NTOK], BF16)]
        KT = [A.view(QKV0 + 4 * K, [128, 4096], BF16)] * 2
        VV = [A.view(QKV0 + 12 * K, [128, 32, 128], BF16), A.view(QKV0 + 24 * K, [128, 32, 128], BF16)]
        assert QKV0 + 32 * K <= ARENA_BYTES
        S.barrier()
        ztile = A.view(MOE_TMP, [128, 1024], BF16)
        memset("pool", ztile, 0.0, writes=["ztile"])
        ZR = 1024
        zlist = list(range(0, NSLOT, ZR))

        def zero_fill(cnt):
            for _ in range(cnt):
                if not zlist:
                    return
                z0 = zlist.pop(0)
                zn_ = min(ZR, NSLOT - z0)
                dma("sp", xs_scr[z0:z0 + zn_, :].rearrange("(t p) d -> p t d", p=128),
                    ztile.unsqueeze(1).broadcast_to([128, zn_ // 128, 1024]), reads=["ztile"], writes=[("xs_zero", z0)])
        unit_n = 0
        for j in range(4):
            for g in range(3):
                r = DIL[g]
                nb_rho = 16 // r
                sl = unit_n % 2
                unit_n += 1
                kq = ("QT", sl)
                kk = ("KT", 0)
                col = 1024 + g * 1536 + j * 128
                wq, wqk = load_chunk(w_in_v, col)
                wk, wkk = load_chunk(w_in_v, col + 512)
                wv, wvk = load_chunk(w_in_v, col + 1024)
                dma("sp", braw[sl], biasT_d.rearrange("p (a b) -> p a b", a=24)[:, g * 8 + 2 * j:g * 8 + 2 * j + 2, :],
                    writes=[("braw", sl)])
                zero_fill(3)
                for hh in range(2):
                    tt("pool", biasm[sl][:, hh, :], braw[sl][:, hh, :], maskT, ALU.add,
                       reads=[("braw", sl), "maskT"], writes=[("biasm", sl)])
                for tb in range(4, 8):
                    pb, pkey = proj_T(wq, wqk, tb)
                    act(QT[sl][:, (tb - 4) * 512:(tb - 3) * 512], pb, AF.Identity, reads=[pkey], writes=[kq], scale=0.125)
                tb_lo = 0 if g == 2 else 3
                for tb in range(tb_lo, 8):
                    pb, pkey = proj_T(wk, wkk, tb)
                    cp("dve", KT[sl][:, tb * 512:(tb + 1) * 512], pb, reads=[pkey], writes=[kk])
                for rho in range(r):
                    for ib in range(2 * nb_rho):
                        ctx = ib < nb_rho
                        if ctx and g < 2 and ib != nb_rho - 1:
                            continue
                        vt = (0 if ctx else 16) + rho * nb_rho + ib % nb_rho
                        bi = 2 + (vt % 2)
                        pv = bank(bi)[:, 0:128]
                        start = rho + r * 128 * ib
                        for kc in range(8):
                            mm(pv, hT[:, kc, start:start + 127 * r + 1:r], wv[:, kc, :], kc == 0, kc == 7,
                               reads=[("hT", b_, kc) for b_ in range(start // 512, (start + 128 * r - 1) // 512 + 1)] + [wvk],
                               writes=[("ps", bi)])
                        if ctx:
                            act(VV[sl][:, vt, :], pv, AF.Identity, reads=[("ps", bi), "flag"], writes=[("V", sl)], scale=flag_s[:, 0:1])
                        else:
                            cp("dve", VV[sl][:, vt, :], pv, reads=[("ps", bi)], writes=[("V", sl)])
                def unit_geom(qb):
                    rho = qb // nb_rho
                    ib_cur = nb_rho + qb % nb_rho
                    qcol0 = rho + r * 128 * ib_cur - NTOK
                    q_sl = slice(qcol0, qcol0 + 127 * r + 1, r)
                    ktile = []
                    for ib in (ib_cur - 1, ib_cur):
                        ctx = ib < nb_rho
                        vt = (0 if ctx else 16) + rho * nb_rho + ib % nb_rho
                        kst = rho + r * 128 * ib
                        ktile.append((slice(kst, kst + 127 * r + 1, r), vt, ctx))
                    return q_sl, ktile

                def unit_S(qb):
                    q_sl, ktile = unit_geom(qb)
                    u = qb % 2
                    for hh in range(2):
                        bS = 2 * hh + u
                        pS = bank(bS)[:, 0:256]
                        pk = ("ps", bS)
                        p0, p1 = hh * 64, hh * 64 + 64
                        for kb in range(2):
                            mm(pS[:, kb * 128:(kb + 1) * 128], KT[sl][p0:p1, ktile[kb][0]], QT[sl][p0:p1, q_sl], True, True,
                               reads=[kk, kq], writes=[pk])
                        si = u * 2 + hh
                        stt(sbS[si], pS, 60.0, biasm[sl][:, hh, :], ALU.min, ALU.add,
                            reads=[pk, ("biasm", sl)], writes=[("sbS", si)])
                        act(ptS[si], sbS[si], AF.Exp, reads=[("sbS", si)], writes=[("ptS", si)])

                def unit_PV(qb):
                    q_sl, ktile = unit_geom(qb)
                    u = qb % 2
                    pND = bank(4 + u)[:, 0:256].rearrange("p (a b) -> p a b", a=2)
                    pkN = ("ps", 4 + u)
                    for which in range(2):
                        for hh in range(2):
                            si = u * 2 + hh
                            p0, p1 = hh * 64, hh * 64 + 64
                            for kb in range(2):
                                _, vt, ctx = ktile[kb]
                                if which == 0:
                                    lhs = VV[sl][:, vt, p0:p1]
                                    rk = [("V", sl)]
                                else:
                                    lhs = onesf_b if ctx else ones_b
                                    rk = ["onesf_b", "ones_b"]
                                mm(pND[p0:p1, which, :], lhs, ptS[si][:, kb * 128:(kb + 1) * 128], kb == 0, kb == 1,
                                   reads=rk + [("ptS", si)], writes=[pkN])
                    dst = accND[:, :, q_sl]
                    if g == 0:
                        cp("dve", dst, pND, reads=[pkN], writes=["accND"])
                    else:
                        tt("dve", dst, pND, dst, ALU.add, reads=[pkN, "accND"], writes=["accND"])

                if PIPE_ATTN:
                    unit_S(0)
                    for qb in range(16):
                        if qb + 1 < 16:
                            unit_S(qb + 1)
                        unit_PV(qb)
                else:
                    for qb in range(16):
                        unit_S(qb)
                        unit_PV(qb)
            S.add("dve", lambda h: h.reciprocal(out=accND[:, 1, :], in_=accND[:, 1, :]), reads=["accND"], writes=["accND"])
            tt("dve", ybT[:, j, :], accND[:, 0, :], accND[:, 1, :], ALU.mult, reads=["accND"], writes=[("ybT", j)])
        if "ybT" in dbg:
            dbg_out["ybT"] = (ybT, [128, 4 * NTOK], BF16, [("ybT", i) for i in range(4)])

    if stop_after not in ("hT", "gmlp", "attn"):
        S.barrier()
        mT = A.view(R1, [128, 8, NTOK], BF16)
        Wa = A.view(R5 + 14 * K, [128, 4, 1024], BF16)
        Wb = A.view(R5 + 22 * K, [128, 4, 1024], BF16)
        gab = [A.view(R5 + 30 * K + i * 2 * K, [128, 512], F32) for i in range(4)]
        t12 = [A.view(R5 + 38 * K + i * 2 * K, [128, 512], F32) for i in range(4)]
        dma("pool", Wa, wa_d.rearrange("(kc p) n -> p kc n", p=128), writes=["Wa"])
        dma("pool", Wb, wb_d.rearrange("(kc p) n -> p kc n", p=128), writes=["Wb"])
        it = 0
        for m in range(8):
            wga, wgak = load_chunk(w_in_v, 5632 + m * 128)
            wgb, wgbk = load_chunk(w_in_v, 6656 + m * 128)
            for tb in range(4):
                s2 = it % 2
                it += 1
                pa = bank(0 + s2 * 4)
                pbb = bank(1 + s2 * 4)
                for kc in range(4):
                    mm(pa, Wa[:, kc, m * 128:(m + 1) * 128], uT[:, kc, tb * 512:(tb + 1) * 512], kc == 0, kc == 3,
                       reads=["Wa", ("uT", tb)], writes=[("ps", 0 + s2 * 4)])
                for kc in range(4):
                    mm(pbb, Wb[:, kc, m * 128:(m + 1) * 128], ybT[:, kc, tb * 512:(tb + 1) * 512], kc == 0, kc == 3,
                       reads=["Wb"] + [("ybT", jj) for jj in range(4)], writes=[("ps", 1 + s2 * 4)])
                pga, pgak = proj_T(wga, wgak, 4 + tb, bank_lo=2 + s2 * 4, bank_n=1)
                pgb, pgbk = proj_T(wgb, wgbk, 4 + tb, bank_lo=3 + s2 * 4, bank_n=1)
                act(gab[s2 * 2], pga, AF.Sigmoid, reads=[pgak], writes=[("gab", s2 * 2)])
                act(gab[s2 * 2 + 1], pgb, AF.Sigmoid, reads=[pgbk], writes=[("gab", s2 * 2 + 1)])
                tt("dve", t12[s2 * 2], pa, gab[s2 * 2], ALU.mult, reads=[("ps", 0 + s2 * 4), ("gab", s2 * 2)], writes=[("t12", s2 * 2)])
                tt("dve", t12[s2 * 2 + 1], pbb, gab[s2 * 2 + 1], ALU.mult, reads=[("ps", 1 + s2 * 4), ("gab", s2 * 2 + 1)], writes=[("t12", s2 * 2 + 1)])
                tt("pool", mT[:, m, tb * 512:(tb + 1) * 512], t12[s2 * 2], t12[s2 * 2 + 1], ALU.add,
                   reads=[("t12", s2 * 2), ("t12", s2 * 2 + 1)], writes=[("mT", tb)])
        if "mT" in dbg:
            dbg_out["mT"] = (mT, [128, 8 * NTOK], BF16, [("mT", i) for i in range(4)])

        S.barrier()
        H2 = R2
        xnb = A.view(H2, [128, 16, 1024], BF16)
        O5 = H2 + 32 * K
        Wo = A.view(O5, [128, 8, 1024], BF16)
        ln1gB = A.view(O5 + 16 * K, [128, 1024], F32)
        ln1bB = A.view(O5 + 20 * K, [128, 1024], F32)
        xs5 = [A.view(O5 + 24 * K + i * 4 * K, [128, 1024], F32) for i in range(2)]
        zt = [A.view(O5 + 32 * K + i * 4 * K, [128, 1024], F32) for i in range(2)]
        x1t = [A.view(O5 + 40 * K + i * 4 * K, [128, 1024], F32) for i in range(2)]
        wr_s = A.view(MOE_TMP, [128, 8, NEXP], F32)
        h2Tf = A.view(MOE_TMP + 6 * K, [128, 8, 128], F32)
        assert MOE_TMP + 10 * K + 512 <= R0_END
        dma("pool", Wo, wo_d.rearrange("(kc p) n -> p kc n", p=128), writes=["Wo"])
        dma("sp", ln1gB, bcast(ln1g_d[0:1, :]), writes=["ln1gB"])
        dma("sp", ln1bB, bcast(ln1b_d[0:1, :]), writes=["ln1bB"])
        dma("sp", wr_s, wr_d.rearrange("(kc p) n -> p kc n", p=128), writes=["wr_s"])
        dma("sp", brB, bcast(br_d[0:1, :]), writes=["brB"])
        def stage5A(ti):
            s2 = ti % 2
            dma("sp", xs5[s2], xo[ti * 128:(ti + 1) * 128, :], writes=[("xs5", s2)])
            for hh in range(2):
                bi = hh + 2 * s2
                pb = bank(bi)
                for kc in range(8):
                    mm(pb, mT[:, kc, ti * 128:(ti + 1) * 128], Wo[:, kc, hh * 512:(hh + 1) * 512], kc == 0, kc == 7,
                       reads=[("mT", ti // 4), "Wo"], writes=[("ps", bi)])
                tt("dve", zt[s2][:, hh * 512:(hh + 1) * 512], pb, G1B[:, hh * 512:(hh + 1) * 512], ALU.mult,
                   reads=[("ps", bi), ("GB", 0)], writes=[("zt", s2)])
            stt(zt[s2], xs5[s2], ALPHA, zt[s2], ALU.mult, ALU.add, reads=[("xs5", s2), ("zt", s2)], writes=[("zt", s2)])
            ln_stats(zt[s2], s2, 0, ("zt", s2))
            ln_finish(s2, 1)
            act(zt[s2], zt[s2], AF.Identity, reads=[("zt", s2), ("rstd", s2), ("nmr", s2)], writes=[("zt", s2)],
                bias=nmr_[s2][:, 0:1], scale=rstd_[s2][:, 0:1])
            tt("dve", zt[s2], zt[s2], ln1gB, ALU.mult, reads=[("zt", s2), "ln1gB"], writes=[("zt", s2)])
            tt("dve", x1t[s2], zt[s2], ln1bB, ALU.add, reads=[("zt", s2), "ln1bB"], writes=[("x1t", s2)])
            dma("sp", x1s_d[ti * 128:(ti + 1) * 128, :], x1t[s2], reads=[("x1t", s2)], writes=[("x1s", ti)])
            ln_stats(x1t[s2], s2, 1, ("x1t", s2))
            act(sd_[s2][:, 1:2], mv_[s2][:, 1:2, 1], AF.Ln, reads=[("mv", s2, 1)], writes=[("sd2", s2)], bias=eps_t, scale=1.0)
            act(rstd_[s2][:, 1:2], sd_[s2][:, 1:2], AF.Exp, reads=[("sd2", s2)], writes=[("rstd2", s2)], scale=-0.5)
            stt(nmr_[s2][:, 1:2], mv_[s2][:, 1:2, 0], -1.0, rstd_[s2][:, 1:2], ALU.mult, ALU.mult,
                reads=[("mv", s2, 1), ("rstd2", s2)], writes=[("nmr2", s2)])
            act(zt[s2], x1t[s2], AF.Identity, reads=[("x1t", s2), ("rstd2", s2), ("nmr2", s2)], writes=[("zt", s2)],
                bias=nmr_[s2][:, 1:2], scale=rstd_[s2][:, 1:2])
            cp("act", xnb[:, ti, :], zt[s2], reads=[("zt", s2)], writes=[("xnb", ti)])

        def stage5B(ti):
            s2 = ti % 2
            for hb in range(2):
                bi = 4 + hb
                pT = bank(bi, [128, 4, 128])
                for k4 in range(4):
                    kc = hb * 4 + k4
                    tr(pT[:, k4, :], zt[s2][:, kc * 128:(kc + 1) * 128], ident_f, reads=[("zt", s2), "ident_f"], writes=[("ps", bi)])
                for k4 in range(4):
                    kc = hb * 4 + k4
                    evac_mod(h2Tf[:, kc, :], pT[:, k4, :], opsc2[:, kc:kc + 1], sh2[:, kc:kc + 1],
                             reads=[("ps", bi), "opsc2", "modT"], writes=[("h2Tf", kc)], eng=("act" if hb == 0 else "dve"))
            pR = bank(6)[:, 0:NEXP]
            for kc in range(8):
                mm(pR, h2Tf[:, kc, :], wr_s[:, kc, :], kc == 0, kc == 7, reads=[("h2Tf", kc), "wr_s"], writes=[("ps", 6)])
            tt("dve", lg_[s2], pR, brB, ALU.add, reads=[("ps", 6), "brB"], writes=[("lg", s2)])
            S.add("dve", lambda h, s2=s2: h.max(out=mx8_[s2], in_=lg_[s2]), reads=[("lg", s2)], writes=[("mx8", s2)])
            ts("dve", msk_[s2], lg_[s2], mx8_[s2][:, 3:4], None, ALU.is_ge, None, reads=[("lg", s2), ("mx8", s2)], writes=[("msk", s2)])
            ts("dve", negm_[s2], mx8_[s2][:, 0:1], -1.0, None, ALU.mult, None, reads=[("mx8", s2)], writes=[("negm", s2)])
            act(ex_[s2], lg_[s2], AF.Exp, reads=[("lg", s2), ("negm", s2)], writes=[("ex", s2)], bias=negm_[s2][:, 0:1], scale=1.0)
            tt("dve", em_[s2], ex_[s2], msk_[s2], ALU.mult, reads=[("ex", s2), ("msk", s2)], writes=[("em", s2)])
            S.add("dve", lambda h, s2=s2: h.reduce_sum(out=den_[s2], in_=em_[s2], axis=AX.X), reads=[("em", s2)], writes=[("den", s2)])
            S.add("dve", lambda h, s2=s2: h.reciprocal(out=rden_[s2], in_=den_[s2]), reads=[("den", s2)], writes=[("rden", s2)])
            ts("dve", gates_all[:, ti, :], em_[s2], rden_[s2][:, 0:1], None, ALU.mult, None, reads=[("em", s2), ("rden", s2)], writes=[("gates", ti)])
            S.add("dve", lambda h, s2=s2: h.max_index(out=mi8_[s2], in_max=mx8_[s2], in_values=lg_[s2]), reads=[("lg", s2), ("mx8", s2)], writes=[("mi8", s2)])
            cp("dve", E4f[:, ti, :], mi8_[s2][:, 0:4], reads=[("mi8", s2)], writes=[("E4f", ti)])
            act(e4_[s2], mx8_[s2][:, 0:4], AF.Exp, reads=[("mx8", s2), ("negm", s2)], writes=[("e4", s2)], bias=negm_[s2][:, 0:1], scale=1.0)
            ts("dve", W4[:, ti, :], e4_[s2], rden_[s2][:, 0:1], None, ALU.mult, None, reads=[("e4", s2), ("rden", s2)], writes=[("W4", ti)])
            cp("pool", Mall[:, ti, :], msk_[s2], reads=[("msk", s2)], writes=[("Mall", ti)])

        stage5A(0)
        for ti in range(16):
            if ti + 1 < 16:
                S.zipped(lambda: stage5A(ti + 1), lambda: stage5B(ti))
            else:
                stage5B(ti)
        if "x1" in dbg:
            dbg_out["x1"] = None
        if "gates" in dbg:
            dbg_out["gates"] = (gates_all, [128, 16 * NEXP], F32, [("gates", i) for i in range(16)])

    if stop_after in (None, "moe", "route"):
        S.barrier()
        RT0 = R5
        within = A.view(RT0, [128, 16, NEXP], F32)
        tot = A.view(RT0 + 2 * K, [128, 16, NEXP], F32)
        base = A.view(RT0 + 4 * K, [128, 16, NEXP], F32)
        dall = A.view(RT0 + 6 * K, [128, 16, NEXP], F32)
        big1 = A.view(RT0 + 8 * K, [128, NEXP, NEXP], F32)
        big2 = A.view(RT0 + 12 * K, [128, NEXP, NEXP], F32)
        LTm = A.view(RT0 + 16 * K, [128, NEXP, NEXP], F32)
        oh = A.view(RT0 + 20 * K, [128, 16, NEXP], F32)
        U_f = A.view(RT0 + 22 * K, [128, 128], F32)
        U_b = A.view(RT0 + 22 * K + 512, [128, 128], BF16)
        on_b = A.view(RT0 + 22 * K + 768, [128, 128], BF16)
        iotaE = A.view(RT0 + 23 * K, [128, NEXP], F32)
        cnt = A.view(RT0 + 23 * K + 128, [128, NEXP], F32)
        rank = A.view(RT0 + 23 * K + 256, [128, NEXP], F32)
        kbv = A.view(RT0 + 23 * K + 384, [128, NEXP], F32)
        sig = A.view(RT0 + 23 * K + 512, [128, NEXP], F32)
        kbtab = A.view(RT0 + 23 * K + 640, [128, NEXP], F32)
        pk = A.view(RT0 + 23 * K + 768, [128, 8], F32)
        destF = A.view(RT0 + 24 * K, [128, 4, 16], F32)
        idxWf = A.view(RT0 + 24 * K + 256, [128, NEXP, 8], F32)
        rankT = A.view(RT0 + 26 * K, [NEXP, 1], F32, parts=NEXP)
        RTm = A.view(RT0 + 26 * K + 64, [NEXP, NEXP], F32, parts=NEXP)
        bgrow = A.view(RT0 + 27 * K, [NEXP, 1024], F32, parts=NEXP)
        burow = A.view(RT0 + 31 * K, [NEXP, 1024], F32, parts=NEXP)
        dma("sp", kbtab, kbtab_d, writes=["kbtab"])
        dma("sp", bgrow, bg_d, writes=["bgrow"])
        dma("sp", burow, bu_d, writes=["burow"])
        memset("pool", U_f, 1.0, writes=["U_f"])
        S.add("pool", lambda h: h.affine_select(out=U_f, in_=U_f, pattern=[[1, 128]], compare_op=ALU.is_gt, fill=0.0, base=0, channel_multiplier=-1),
              reads=["U_f"], writes=["U_f"])
        cp("pool", U_b, U_f, reads=["U_f"], writes=["U_b"])
        memset("pool", on_b, 1.0, writes=["on_b"])
        S.add("pool", lambda h: h.iota(iotaE, pattern=[[1, NEXP]], base=0, channel_multiplier=0, allow_small_or_imprecise_dtypes=True), writes=["iotaE"])
        S.add("pool", lambda h: h.iota(pk, pattern=[[128, 8]], base=0, channel_multiplier=1, allow_small_or_imprecise_dtypes=True), writes=["pk"])
        memset("pool", LTm, 1.0, writes=["LTm"])
        S.add("pool", lambda h: h.affine_select(out=LTm, in_=LTm, pattern=[[1, NEXP], [-1, NEXP]], compare_op=ALU.is_gt, fill=0.0, base=0, channel_multiplier=0),
              reads=["LTm"], writes=["LTm"])
        Mkeys = [("Mall", i) for i in range(16)]
        Mflat = Mall.rearrange("p a b -> p (a b)")
        mm(bank(0), U_b, Mflat, True, True, reads=["U_b"] + Mkeys, writes=[("ps", 0)])
        mm(bank(1), on_b, Mflat, True, True, reads=["on_b"] + Mkeys, writes=[("ps", 1)])
        cp("dve", within.rearrange("p a b -> p (a b)"), bank(0), reads=[("ps", 0)], writes=["within"])
        cp("act", tot.rearrange("p a b -> p (a b)"), bank(1), reads=[("ps", 1)], writes=["tot"])
        memset("dve", base[:, 0, :], 0.0, writes=["base"])
        for ti in range(1, 16):
            tt("dve", base[:, ti, :], base[:, ti - 1, :], tot[:, ti - 1, :], ALU.add, reads=["base", "tot"], writes=["base"])
        tt("dve", cnt, base[:, 15, :], tot[:, 15, :], ALU.add, reads=["base", "tot"], writes=["cnt"])
        cA = cnt.unsqueeze(1).broadcast_to([128, NEXP, NEXP])
        cB = cnt.unsqueeze(2).broadcast_to([128, NEXP, NEXP])
        tt("dve", big1, cA, cB, ALU.is_gt, reads=["cnt"], writes=["big1"])
        tt("dve", big2, cA, cB, ALU.is_equal, reads=["cnt"], writes=["big2"])
        tt("dve", big2, big2, LTm, ALU.mult, reads=["big2", "LTm"], writes=["big2"])
        tt("dve", big1, big1, big2, ALU.add, reads=["big1", "big2"], writes=["big1"])
        S.add("dve", lambda h: h.reduce_sum(out=rank, in_=big1, axis=AX.X), reads=["big1"], writes=["rank"])
        rA = rank.unsqueeze(2).broadcast_to([128, NEXP, NEXP])
        iB = iotaE.unsqueeze(1).broadcast_to([128, NEXP, NEXP])
        tt("dve", big1, rA, iB, ALU.is_equal, reads=["rank", "iotaE"], writes=["big1"])
        tt("dve", big2, big1, kbtab.unsqueeze(1).broadcast_to([128, NEXP, NEXP]), ALU.mult, reads=["big1", "kbtab"], writes=["big2"])
        S.add("dve", lambda h: h.reduce_sum(out=kbv, in_=big2, axis=AX.X), reads=["big2"], writes=["kbv"])
        tt("dve", big1, rank.unsqueeze(1).broadcast_to([128, NEXP, NEXP]), iotaE.unsqueeze(2).broadcast_to([128, NEXP, NEXP]), ALU.is_equal,
           reads=["rank", "iotaE"], writes=["big1"])
        tt("dve", big2, big1, iB, ALU.mult, reads=["big1", "iotaE"], writes=["big2"])
        S.add("dve", lambda h: h.reduce_sum(out=sig, in_=big2, axis=AX.X), reads=["big2"], writes=["sig"])
        tt("dve", dall, base, within, ALU.add, reads=["base", "within"], writes=["dall"])
        tt("dve", dall, dall, kbv.unsqueeze(1).broadcast_to([128, 16, NEXP]), ALU.add, reads=["dall", "kbv"], writes=["dall"])
        E4keys = [("E4f", i) for i in range(16)]
        for k in range(4):
            tt("dve", oh, iotaE.unsqueeze(1).broadcast_to([128, 16, NEXP]), E4f[:, :, k:k + 1].broadcast_to([128, 16, NEXP]), ALU.is_equal,
               reads=["iotaE"] + E4keys, writes=["oh"])
            tt("dve", oh, oh, dall, ALU.mult, reads=["oh", "dall"], writes=["oh"])
            S.add("dve", lambda h, k=k: h.reduce_sum(out=destF[:, k, :], in_=oh, axis=AX.X), reads=["oh"], writes=["destF"])
        cp("dve", destI, destF, reads=["destF"], writes=["destI"])
        ts("dve", sig, sig, 1024.0, None, ALU.mult, None, reads=["sig"], writes=["sig"])
        tt("dve", idxWf, sig.unsqueeze(2).broadcast_to([128, NEXP, 8]), pk.unsqueeze(1).broadcast_to([128, NEXP, 8]), ALU.add,
           reads=["sig", "pk"], writes=["idxWf"])
        cp("dve", idxW, idxWf, reads=["idxWf"], writes=["idxW"])
        pRk = bank(2)[0:NEXP, 0:128]
        tr(pRk, rank, ident_f, reads=["rank", "ident_f"], writes=[("ps", 2)])
        cp("act", rankT, pRk[:, 0:1], reads=[("ps", 2)], writes=["rankT"])
        ts("dve", RTm, iotaE[0:NEXP, :], rankT[:, 0:1], None, ALU.is_equal, None, reads=["iotaE", "rankT"], writes=["RTm"])
        for bi_, (rows, rkey, dstR, dkey) in enumerate(((bgrow, "bgrow", bgR, "bgR"), (burow, "burow", bu1R, "bu1R"))):
            pbk = bank(3 + bi_)
            for c in range(8):
                mm(pbk[:, c * NEXP:(c + 1) * NEXP], rows[:, c * 128:(c + 1) * 128], RTm, True, True, reads=[rkey, "RTm"], writes=[("ps", 3 + bi_)])
            cp("act", dstR.rearrange("p a b -> p (a b)"), pbk[:, 0:8 * NEXP], reads=[("ps", 3 + bi_)], writes=[dkey])
        ts("pool", bu1R, bu1R, 1.0, None, ALU.add, None, reads=["bu1R"], writes=["bu1R"])
        if "route" in dbg:
            dbg_out["gates"] = (gates_all, [128, 16 * NEXP], F32, [("gates", i) for i in range(16)])
            dbg_out["destI"] = (destI, [128, 64], I32, ["destI"])
            dbg_out["idxW"] = (idxW, [128, NEXP * 8], I32, ["idxW"])
            dbg_out["rank"] = (rank, [128, NEXP], F32, ["rank"])
            dbg_out["W4"] = (W4, [128, 64], F32, [("W4", i) for i in range(16)])
            dbg_out["bgR"] = (bgR, [128, 8 * NEXP], F32, ["bgR"])

    if stop_after is None or stop_after == "moe":
        for ti in range(16):
            for k in range(4):
                S.add("pool", lambda h, ti=ti, k=k: h.indirect_dma_start(
                    out=xs_scr[:, :], out_offset=bass.IndirectOffsetOnAxis(ap=destI[:, k, ti:ti + 1], axis=0),
                    in_=xnb[:, ti, :], in_offset=None), reads=["destI", ("xnb", ti)], writes=[("xs_scr", ti, k)], dma=True)
        S.barrier()
        xrow = [A.view(R5 + i * 8 * K, [128, 4, 1024], BF16) for i in range(2)]
        yrow = [A.view(R5 + 16 * K + i * 4 * K, [128, 1024], F32) for i in range(2)]
        xeT = [A.view(R1 + i * 8 * K, [128, 8, 512], BF16) for i in range(2)] + [A.view(R5 + 24 * K, [128, 8, 512], BF16)]
        actT = [A.view(R1 + 16 * K + i * 8 * K, [128, 8, 512], BF16) for i in range(2)]
        NWB = 6
        wbufs = [A.view(R2 + i * 16 * K, [128, 8, 1024], BF16) for i in range(NWB)]
        tmp_g = [A.view(MOE_TMP + i * 2 * K, [128, 512], F32) for i in range(2)]
        tmp_s = [A.view(MOE_TMP + 4 * K + i * 2 * K, [128, 512], F32) for i in range(2)]
        tmp_u = [A.view(MOE_TMP + 8 * K + i * 2 * K, [128, 512], F32) for i in range(2)]
        assert MOE_TMP + 12 * K <= R0_END, (MOE_TMP, R0_END)
        wn = [0]

        def load_w(src, r):
            i = wn[0] % NWB
            wn[0] += 1
            rows = src.rearrange("e k n -> (e k) n")
            for kc in range(8):
                S.add("pool", lambda h, i=i, kc=kc: h.indirect_dma_start(
                    out=wbufs[i][:, kc, :], out_offset=None, in_=rows[:, :],
                    in_offset=bass.IndirectOffsetOnAxis(ap=idxW[:, r, kc:kc + 1], axis=0)),
                    reads=["idxW"], writes=[("wb", i, kc)], dma=True)
            return wbufs[i], i

        def load_rank(r):
            return (load_w(wg_d, r), load_w(wu_d, r), load_w(wd_d, r))

        itc = [0]
        ytile_n = [0]

        def blk_load(blk):
            r, sb_, bidx, W = blk
            n = min(512, KCAP[r] - sb_ * 512)
            nt = n // 128
            row0 = KBASE[r] + sb_ * 512
            xr = bidx % 2
            dma("sp", xrow[xr][:, 0:nt, :], xs_scr[row0:row0 + n, :].rearrange("(t p) d -> p t d", p=128),
                reads=["xs_scr"], writes=[("xrow", xr)])

        def blk_T(blk):
            r, sb_, bidx, W = blk
            n = min(512, KCAP[r] - sb_ * 512)
            nt = n // 128
            xr, x3 = bidx % 2, bidx % 3
            for kg in range(2):
                pT = psum_t[:, 4 * 512:6 * 512].bitcast(BF16).rearrange("p (a b) -> p a b", a=4)
                for t in range(nt):
                    for k4 in range(4):
                        kc = kg * 4 + k4
                        tr(pT[:, k4, t * 128:(t + 1) * 128], xrow[xr][:, t, kc * 128:(kc + 1) * 128], ident_b,
                           reads=[("xrow", xr), "ident_b"], writes=[("ps", 4), ("ps", 5)])
                for k4 in range(4):
                    kc = kg * 4 + k4
                    evac_mod(xeT[x3][:, kc, 0:n], pT[:, k4, 0:n], opsc2[:, kc:kc + 1], sh2[:, kc:kc + 1],
                             reads=[("ps", 4), ("ps", 5), "opsc2", "modT"], writes=[("xeT", x3, kc)], eng=("act" if k4 < 2 else "dve"))

        def blk_GU(blk):
            r, sb_, bidx, W = blk
            (Wg_, Wgk), (Wu_, Wuk), (Wd_, Wdk) = W
            n = min(512, KCAP[r] - sb_ * 512)
            x3, xs2 = bidx % 3, bidx % 2
            for c in range(8):
                s2 = itc[0] % 2
                itc[0] += 1
                bg_i, bu_i = 0 + s2, 2 + s2
                pg, pu = bank(bg_i)[:, 0:n], bank(bu_i)[:, 0:n]
                for kc in range(8):
                    mm(pg, Wg_[:, kc, c * 128:(c + 1) * 128], xeT[x3][:, kc, 0:n], kc == 0, kc == 7,
                       reads=[("wb", Wgk, kc), ("xeT", x3, kc)], writes=[("ps", bg_i)])
                for kc in range(8):
                    mm(pu, Wu_[:, kc, c * 128:(c + 1) * 128], xeT[x3][:, kc, 0:n], kc == 0, kc == 7,
                       reads=[("wb", Wuk, kc), ("xeT", x3, kc)], writes=[("ps", bu_i)])
                tg, tsg, tu = tmp_g[s2][:, 0:n], tmp_s[s2][:, 0:n], tmp_u[s2][:, 0:n]
                ts("dve", tg, pg, bgR[:, c, r:r + 1], 7.0, ALU.add, ALU.min, reads=[("ps", bg_i), "bgR"], writes=[("tg", s2)])
                act(tsg, tg, AF.Sigmoid, reads=[("tg", s2)], writes=[("tsg", s2)], scale=1.702)
                act(tu, pu, AF.Identity, reads=[("ps", bu_i), "bu1R"], writes=[("tu", s2)], bias=bu1R[:, c, r:r + 1], scale=1.0)
                ts("dve", tu, tu, 8.0, -6.0, ALU.min, ALU.max, reads=[("tu", s2)], writes=[("tu", s2)])
                tt("dve", tsg, tg, tsg, ALU.mult, reads=[("tg", s2), ("tsg", s2)], writes=[("tsg", s2)])
                tt("dve", actT[xs2][:, c, 0:n], tsg, tu, ALU.mult, reads=[("tsg", s2), ("tu", s2)], writes=[("actT", xs2)])

        def blk_back(blk):
            r, sb_, bidx, W = blk
            xs2 = bidx % 2
            (Wg_, Wgk), (Wu_, Wuk), (Wd_, Wdk) = W
            n = min(512, KCAP[r] - sb_ * 512)
            nt = n // 128
            row0 = KBASE[r] + sb_ * 512
            for t in range(nt):
                ys2 = ytile_n[0] % 2
                ytile_n[0] += 1
                for hh in range(2):
                    bi = 6 + hh
                    py = bank(bi)
                    for c in range(8):
                        mm(py, actT[xs2][:, c, t * 128:(t + 1) * 128], Wd_[:, c, hh * 512:(hh + 1) * 512], c == 0, c == 7,
                           reads=[("actT", xs2), ("wb", Wdk, c)], writes=[("ps", bi)])
                    cp("act" if hh == 0 else "dve", yrow[ys2][:, hh * 512:(hh + 1) * 512], py, reads=[("ps", bi)], writes=[("yrow", ys2, hh)])
                dma("sp", ys_scr[row0 + t * 128:row0 + (t + 1) * 128, :], yrow[ys2], reads=[("yrow", ys2, 0), ("yrow", ys2, 1)], writes=[("ys_scr", ytile_n[0])])

        blist = []
        for r in range(n_experts):
            for sb_ in range((KCAP[r] + 511) // 512):
                blist.append([r, sb_, len(blist), None])
        NBLK = len(blist)
        Wsets = {0: load_rank(0)}

        def attach_w(bi_):
            r = blist[bi_][0]
            blist[bi_][3] = Wsets[r]

        blk_load(blist[0])
        if NBLK > 1:
            blk_load(blist[1])
        blk_T(blist[0])
        if NBLK > 2:
            blk_load(blist[2])
        if NBLK > 1:
            blk_T(blist[1])
        attach_w(0)
        blk_GU(blist[0])
        if 1 < n_experts:
            Wsets[1] = load_rank(1)
        for bi_ in range(NBLK):
            if bi_ + 3 < NBLK:
                blk_load(blist[bi_ + 3])
            if bi_ + 2 < NBLK:
                blk_T(blist[bi_ + 2])
            if bi_ + 1 < NBLK:
                attach_w(bi_ + 1)
                blk_GU(blist[bi_ + 1])
            blk_back(blist[bi_])
            if bi_ + 1 < NBLK and blist[bi_ + 1][1] == 0:
                r1 = blist[bi_ + 1][0]
                if r1 + 1 < n_experts and (r1 + 1) not in Wsets:
                    Wsets[r1 + 1] = load_rank(r1 + 1)
        S.barrier()
        F7 = R2
        ln2gB = A.view(F7, [128, 1024], F32)
        ln2bB = A.view(F7 + 4 * K, [128, 1024], F32)
        x1r = [A.view(F7 + 8 * K + i * 4 * K, [128, 1024], F32) for i in range(2)]
        z2 = [A.view(F7 + 16 * K + i * 4 * K, [128, 1024], F32) for i in range(2)]
        ot = [A.view(F7 + 24 * K + i * 4 * K, [128, 1024], F32) for i in range(2)]
        acct = [A.view(F7 + 32 * K + i * 4 * K, [128, 1024], F32) for i in range(2)]
        ygt = [A.view(F7 + 40 * K + i * 4 * K, [128, 1024], F32) for i in range(4)]
        bdn_s = A.view(F7 + 56 * K, [NEXP, 1024], F32, parts=NEXP)
        gT_s = [A.view(F7 + 60 * K + i * 512, [NEXP, 128], F32, parts=NEXP) for i in range(2)]
        dma("sp", ln2gB, bcast(ln2g_d[0:1, :]), writes=["ln2gB"])
        dma("sp", ln2bB, bcast(ln2b_d[0:1, :]), writes=["ln2bB"])
        dma("sp", bdn_s, bdn_d, writes=["bdn_s"])
        outs = []
        gi = 0
        gi_ = [0]
        def fin_A(ti):
            s2 = ti % 2
            dma("sp", x1r[s2], x1s_d[ti * 128:(ti + 1) * 128, :], reads=[("x1s", ti)], writes=[("x1r", s2)])
            pG = bank(4 + s2)[0:NEXP, 0:128]
            tr(pG, gates_all[:, ti, :], ident_f, reads=[("gates", ti), "ident_f"], writes=[("ps", 4 + s2)])
            cp("act", gT_s[s2], pG, reads=[("ps", 4 + s2)], writes=[("gT_s", s2)])
            for hh in range(2):
                bi = hh + 2 * s2
                pb = bank(bi)
                mm(pb, gT_s[s2], bdn_s[:, hh * 512:(hh + 1) * 512], True, True, reads=[("gT_s", s2), "bdn_s"], writes=[("ps", bi)])
                cp("act", acct[s2][:, hh * 512:(hh + 1) * 512], pb, reads=[("ps", bi)], writes=[("acct", s2)])
            for k in range(4):
                g4 = gi_[0] % 4
                gi_[0] += 1
                S.add("pool", lambda h, ti=ti, k=k, g4=g4: h.indirect_dma_start(
                    out=ygt[g4][:, :], out_offset=None, in_=ys_scr[:, :],
                    in_offset=bass.IndirectOffsetOnAxis(ap=destI[:, k, ti:ti + 1], axis=0)),
                    reads=["destI"], writes=[("ygt", g4)], dma=True)
                stt(acct[s2], ygt[g4], W4[:, ti, k:k + 1], acct[s2], ALU.mult, ALU.add,
                    reads=[("ygt", g4), ("W4", ti), ("acct", s2)], writes=[("acct", s2)])

        def fin_B(ti):
            s2 = ti % 2
            tt("dve", z2[s2], acct[s2], G2B, ALU.mult, reads=[("acct", s2), ("GB", 1)], writes=[("z2", s2)])
            stt(z2[s2], x1r[s2], ALPHA, z2[s2], ALU.mult, ALU.add, reads=[("x1r", s2), ("z2", s2)], writes=[("z2", s2)])
            ln_stats(z2[s2], s2, 0, ("z2", s2))
            ln_finish(s2, 1)
            act(z2[s2], z2[s2], AF.Identity, reads=[("z2", s2), ("rstd", s2), ("nmr", s2)], writes=[("z2", s2)],
                bias=nmr_[s2][:, 0:1], scale=rstd_[s2][:, 0:1])
            tt("dve", z2[s2], z2[s2], ln2gB, ALU.mult, reads=[("z2", s2), "ln2gB"], writes=[("z2", s2)])
            tt("dve", ot[s2], z2[s2], ln2bB, ALU.add, reads=[("z2", s2), "ln2bB"], writes=[("ot", s2)])
            dma("sp", out_d[ti * 128:(ti + 1) * 128, :], ot[s2], reads=[("ot", s2)], writes=[("out", ti)])
            outs.append(("out", ti))

        fin_A(0)
        for ti in range(16):
            if ti + 1 < 16:
                S.zipped(lambda: fin_A(ti + 1), lambda: fin_B(ti))
            else:
                fin_B(ti)
        S.add("sp", None, reads=outs)

    dkeys = []
    for name, spec in dbg_out.items():
        if spec is None:
            continue
        ap, shape, dt, keys = spec
        dd = nc.dram_tensor("dbg_" + name, shape, dt, kind="ExternalOutput").ap()
        flat = ap
        if len(ap.shape) == 3:
            flat = ap.rearrange("p a b -> p (a b)")
        dma("sp", dd, flat, reads=keys, writes=[("dbg", name)])
        dkeys.append(("dbg", name))
    if dkeys:
        S.add("sp", None, reads=dkeys)
    S.emit()
    st.close()
    return nc


def t5_bucket_np(dist):
    d = dist.astype(np.float32)
    large = np.float32(16) + np.log(np.maximum(d, np.float32(16.0)) / np.float32(16)) / np.float32(math.log(2048 / 16)) * np.float32(16)
    large = np.minimum(large.astype(np.int32), 31)
    return np.where(dist < 16, dist, large)


def host_layout(inp):
    f = np.float32
    x = np.asarray(inp["x"], f)
    shared = {}
    shared["w_ada"] = np.ascontiguousarray(np.asarray(inp["w_ada"], f)[0])
    shared["b_adaT"] = np.ascontiguousarray(np.asarray(inp["b_ada"], f)[0].reshape(48, 128).T)
    shared["w_in"] = np.ascontiguousarray(np.asarray(inp["w_in"], f)[0])
    shared["gm_g"] = np.asarray(inp["gm_ln_g"], f).reshape(1, 512)
    shared["gm_b"] = np.asarray(inp["gm_ln_b"], f).reshape(1, 512)
    ws = np.asarray(inp["gm_w_s"], f)[0]
    shared["wsT"] = np.ascontiguousarray(ws.transpose(2, 0, 1).reshape(128, 8 * 128))
    bs = np.asarray(inp["gm_b_s"], f)[0]
    bsp = np.empty((128, 4, 128), f)
    for gp in range(4):
        bsp[0:64, gp, :] = bs[2 * gp][None, :]
        bsp[64:128, gp, :] = bs[2 * gp + 1][None, :]
    shared["bsp"] = bsp.reshape(128, 512)
    shared["wa"] = np.ascontiguousarray(np.asarray(inp["w_branch_a"], f)[0])
    shared["wb"] = np.ascontiguousarray(np.asarray(inp["w_branch_b"], f)[0])
    shared["wo"] = np.ascontiguousarray(np.asarray(inp["w_out"], f)[0])
    rel = np.asarray(inp["rel_bias"], f)
    kk = np.arange(128)[:, None]
    qq = np.arange(128)[None, :]
    d_prev = qq + 128 - kk
    d_cur = qq - kk
    biasT = np.empty((128, 24, 2, 128), f)
    for g in range(3):
        bp = t5_bucket_np(np.clip(d_prev, 0, None).astype(np.int32) * DIL[g])
        bc = t5_bucket_np(np.clip(d_cur, 0, None).astype(np.int32) * DIL[g])
        for h in range(8):
            biasT[:, g * 8 + h, 0, :] = rel[bp, g * 8 + h]
            biasT[:, g * 8 + h, 1, :] = rel[bc, g * 8 + h]
    shared["biasT"] = biasT.reshape(128, 24 * 256)
    maskT = np.zeros((128, 2, 128), f)
    maskT[:, 0, :][d_prev > 128] = -1e30
    maskT[:, 1, :][d_cur < 0] = -1e30
    shared["maskT"] = maskT.reshape(128, 256)
    shared["ln1g"] = np.asarray(inp["ln1_g"], f).reshape(1, D)
    shared["ln1b"] = np.asarray(inp["ln1_b"], f).reshape(1, D)
    shared["wr"] = np.ascontiguousarray(np.asarray(inp["w_router"], f)[0])
    shared["br"] = np.asarray(inp["b_router"], f).reshape(1, NEXP)
    shared["wg"] = np.ascontiguousarray(np.asarray(inp["w_gate"], f)[0])
    shared["wu"] = np.ascontiguousarray(np.asarray(inp["w_up"], f)[0])
    shared["wd"] = np.ascontiguousarray(np.asarray(inp["w_down"], f)[0])
    shared["bg"] = np.ascontiguousarray(np.asarray(inp["b_gate"], f)[0])
    shared["bu"] = np.ascontiguousarray(np.asarray(inp["b_up"], f)[0])
    shared["kbtab"] = np.broadcast_to(np.asarray(KBASE, f)[None, :], (128, NEXP)).copy()
    shared["bdn"] = np.ascontiguousarray(np.asarray(inp["b_down"], f)[0])
    shared["ln2g"] = np.asarray(inp["ln2_g"], f).reshape(1, D)
    shared["ln2b"] = np.asarray(inp["ln2_b"], f).reshape(1, D)
    shared["ident"] = np.eye(128, dtype=f)
    c = np.asarray(inp["c"], f)
    in_maps = []
    for k in range(NCORES):
        b, hf = k // 2, k % 2
        m = dict(shared)
        m["xo"] = np.ascontiguousarray(x[b, hf * NTOK:(hf + 1) * NTOK])
        m["xc"] = np.ascontiguousarray(x[b, 0:NTOK]) if hf == 1 else np.zeros((NTOK, D), f)
        m["flag"] = np.full((128, 1), float(hf), f)
        m["cT"] = np.ascontiguousarray(c[b].reshape(8, 128).T)
        in_maps.append(m)
    return in_maps


_NC_CACHE = {}


def kernel(**inputs):
    in_maps = host_layout(inputs)
    if "nc" not in _NC_CACHE:
        _NC_CACHE["nc"] = build()
    nc = _NC_CACHE["nc"]
    res = run_bass_kernel_spmd(nc, in_maps, core_ids=list(range(NCORES)))
    out = np.empty((4, 4096, D), np.float32)
    for k in range(NCORES):
        b, hf = k // 2, k % 2
        out[b, hf * NTOK:(hf + 1) * NTOK] = np.asarray(res.results[k]["out"], np.float32)
    return out
```
